# Optimizing a Trainium2 kernel written in Bass

```python
import jax, jax.numpy as jnp
from jax import lax
import numpy as np

D_MODEL = 2048
BATCH = 2
SEQ = 4096
DEPTH = 1

H_RET = 8
DK_RET = 128
DV_RET = 128
RET_CHUNK = 128
H_ATT = 8
H_KV = 2
D_HEAD = 128
H_IDX = 16
D_IDX = 64
MAX_TOPK = 256
Q_BLOCK = 128
D_FF = 5632
ROPE_THETA = 10000.0
NORM_EPS = 1e-6

RET_W = H_RET * DV_RET
ATT_W = H_ATT * D_HEAD
MIX_W = RET_W + ATT_W
IN_SIZES = (H_RET * DK_RET, H_RET * DK_RET, RET_W, RET_W, ATT_W, H_KV * D_HEAD, H_KV * D_HEAD, H_IDX * D_IDX, D_IDX, H_IDX)
IN_COLS = sum(IN_SIZES)

kernel_name = 'hybrid_retention_dsa_macaron'


def rmsnorm(x, g):
    xf = x.astype(jnp.float32)
    y = xf * lax.rsqrt(jnp.mean(xf * xf, axis=-1, keepdims=True) + NORM_EPS)
    return (y * g.astype(jnp.float32)).astype(x.dtype)


def rope(x, pos):
    d = x.shape[-1]
    inv = ROPE_THETA ** (-jnp.arange(0, d, 2, dtype=jnp.float32) / d)
    ang = pos.astype(jnp.float32)[..., None] * inv
    cos = jnp.cos(ang)[:, :, None, :]
    sin = jnp.sin(ang)[:, :, None, :]
    xf = x.astype(jnp.float32)
    x1, x2 = xf[..., : d // 2], xf[..., d // 2:]
    return jnp.concatenate([x1 * cos - x2 * sin, x2 * cos + x1 * sin], axis=-1).astype(x.dtype)


def swiglu(h, w_gate, w_up, w_down):
    return (jax.nn.silu(h @ w_gate) * (h @ w_up)) @ w_down


def retention(q, k, v):
    B, T, H, dk = q.shape
    dv = v.shape[-1]
    C = RET_CHUNK
    N = T // C
    lg = jnp.log1p(-jnp.exp2(-5.0 - jnp.arange(H, dtype=jnp.float32)))
    q = q.astype(jnp.float32).reshape(B, N, C, H, dk)
    k = (k.astype(jnp.float32) * dk ** -0.5).reshape(B, N, C, H, dk)
    v = v.astype(jnp.float32).reshape(B, N, C, H, dv)
    j = jnp.arange(C, dtype=jnp.float32)
    diff = j[:, None] - j[None, :]
    decay = jnp.where(diff[None] >= 0, jnp.exp(jnp.maximum(diff, 0.0)[None] * lg[:, None, None]), 0.0)
    s = jnp.einsum('bnihd,bnjhd->bnhij', q, k) * decay
    intra = jnp.einsum('bnhij,bnjhe->bnihe', s, v)
    k_dec = k * jnp.exp((C - 1 - j)[:, None] * lg[None, :])[:, :, None]
    kv = jnp.einsum('bnjhd,bnjhe->nbhde', k_dec, v)
    g_chunk = jnp.exp(C * lg)[None, :, None, None]

    def step(state, kv_n):
        return state * g_chunk + kv_n, state

    _, states = lax.scan(step, jnp.zeros((B, H, dk, dv), jnp.float32), kv)
    q_dec = q * jnp.exp((j + 1)[:, None] * lg[None, :])[:, :, None]
    cross = jnp.einsum('bnihd,nbhde->bnihe', q_dec, states)
    return (intra + cross).reshape(B, T, H, dv)


def sparse_attention(q, k, v, q_idx, k_idx, w_idx):
    B, T = q.shape[0], q.shape[1]
    top_k = min(MAX_TOPK, T // 4)
    nb = T // Q_BLOCK
    key_pos = jnp.arange(T)
    gather = jax.vmap(lambda a, i: a[i])

    def blocks(a):
        return jnp.moveaxis(a.reshape((B, nb, Q_BLOCK) + a.shape[2:]), 1, 0)

    def attend(args):
        qb, qib, wb, t0 = args
        t = t0 + jnp.arange(Q_BLOCK)
        rel = jax.nn.relu(jnp.einsum('bqhd,bsd->bqhs', qib, k_idx))
        score = jnp.einsum('bqhs,bqh->bqs', rel, wb).astype(jnp.float32)
        causal = key_pos[None, :] <= t[:, None]
        score = jnp.where(causal[None], score, -jnp.inf)
        _, idx = lax.top_k(score, top_k)
        valid = idx <= t[None, :, None]
        kg = gather(k, idx)
        vg = gather(v, idx)
        logits = jnp.einsum('bqgnd,bqkgd->bqgnk', qb, kg).astype(jnp.float32) * D_HEAD ** -0.5
        logits = jnp.where(valid[:, :, None, None, :], logits, -jnp.inf)
        p = jax.nn.softmax(logits, axis=-1).astype(vg.dtype)
        return jnp.einsum('bqgnk,bqkgd->bqgnd', p, vg)

    out = lax.map(attend, (blocks(q), blocks(q_idx), blocks(w_idx), jnp.arange(nb) * Q_BLOCK))
    return jnp.moveaxis(out, 0, 1).reshape(B, T, H_ATT * D_HEAD)


def setup_inputs(seed: int = 0) -> dict:
    key = jax.random.key(seed)
    ks = jax.random.split(key, 16)

    def dense(k, shape, fan_in):
        return jax.random.normal(k, shape, jnp.float32) * fan_in ** -0.5

    def gain(k, shape):
        return 1.0 + 0.02 * jax.random.normal(k, shape, jnp.float32)

    return {
        'x': jax.random.normal(ks[0], (BATCH, SEQ, D_MODEL), jnp.float32),
        'positions': jnp.broadcast_to(jnp.arange(SEQ, dtype=jnp.int32), (BATCH, SEQ)),
        'ffn1_norm': gain(ks[1], (DEPTH, D_MODEL)),
        'ffn1_w_gate': dense(ks[2], (DEPTH, D_MODEL, D_FF), D_MODEL),
        'ffn1_w_up': dense(ks[3], (DEPTH, D_MODEL, D_FF), D_MODEL),
        'ffn1_w_down': dense(ks[4], (DEPTH, D_FF, D_MODEL), D_FF),
        'mix_norm': gain(ks[5], (DEPTH, D_MODEL)),
        'w_in': dense(ks[6], (DEPTH, D_MODEL, IN_COLS), D_MODEL),
        'ret_norm': gain(ks[7], (DEPTH, RET_W)),
        'w_out': dense(ks[8], (DEPTH, MIX_W, D_MODEL), MIX_W),
        'ffn2_norm': gain(ks[9], (DEPTH, D_MODEL)),
        'ffn2_w_gate': dense(ks[10], (DEPTH, D_MODEL, D_FF), D_MODEL),
        'ffn2_w_up': dense(ks[11], (DEPTH, D_MODEL, D_FF), D_MODEL),
        'ffn2_w_down': dense(ks[12], (DEPTH, D_FF, D_MODEL), D_FF),
        'final_norm': gain(ks[13], (D_MODEL,)),
    }


def reference(x, positions, ffn1_norm, ffn1_w_gate, ffn1_w_up, ffn1_w_down, mix_norm, w_in, ret_norm, w_out, ffn2_norm, ffn2_w_gate, ffn2_w_up, ffn2_w_down, final_norm):
    B, T, _ = x.shape
    splits = np.cumsum(IN_SIZES)[:-1].tolist()
    for l in range(DEPTH):
        x = x + 0.5 * swiglu(rmsnorm(x, ffn1_norm[l]), ffn1_w_gate[l], ffn1_w_up[l], ffn1_w_down[l])
        h = rmsnorm(x, mix_norm[l])
        proj = h @ w_in[l]
        rq, rk, rv, rg, aq, ak, av, iq, ik, iw = jnp.split(proj, splits, axis=-1)
        rq = rope(rq.reshape(B, T, H_RET, DK_RET), positions)
        rk = rope(rk.reshape(B, T, H_RET, DK_RET), positions)
        ro = retention(rq, rk, rv.reshape(B, T, H_RET, DV_RET))
        mu = jnp.mean(ro, axis=-1, keepdims=True)
        var = jnp.mean(jnp.square(ro - mu), axis=-1, keepdims=True)
        ro = (ro - mu) * lax.rsqrt(var + NORM_EPS) * ret_norm[l].astype(jnp.float32).reshape(H_RET, DV_RET)
        ro = ro.reshape(B, T, RET_W).astype(x.dtype) * jax.nn.silu(rg)
        aq = rope(aq.reshape(B, T, H_ATT, D_HEAD), positions).reshape(B, T, H_KV, H_ATT // H_KV, D_HEAD)
        ak = rope(ak.reshape(B, T, H_KV, D_HEAD), positions)
        av = av.reshape(B, T, H_KV, D_HEAD)
        iq = rope(iq.reshape(B, T, H_IDX, D_IDX), positions) * D_IDX ** -0.5
        ik = rope(ik.reshape(B, T, 1, D_IDX), positions)[:, :, 0]
        iw = iw * H_IDX ** -0.5
        ao = sparse_attention(aq, ak, av, iq, ik, iw)
        x = x + jnp.concatenate([ro, ao], axis=-1) @ w_out[l]
        x = x + 0.5 * swiglu(rmsnorm(x, ffn2_norm[l]), ffn2_w_gate[l], ffn2_w_up[l], ffn2_w_down[l])
    return rmsnorm(x, final_norm)
```

```python
import numpy as np
import ml_dtypes
from contextlib import ExitStack
import concourse.bass as bass
import concourse.mybir as mybir
from concourse.bass_utils import run_bass_kernel_spmd

F32 = mybir.dt.float32
BF16 = mybir.dt.bfloat16
I32 = mybir.dt.int32
ALU = mybir.AluOpType
AF = mybir.ActivationFunctionType
AX = mybir.AxisListType

D = 2048
DFF = 5632
NFG = 22
NB = 8
NIT = 22
NEG_MASK = -30000.0
PI = float(np.pi)

C_INV128, C_INV64, C_GQ, C_GK, C_GQT, C_GKT, C_GC, C_POW2, C_EPS, NF = 0, 64, 96, 104, 112, 1136, 2160, 3184, 3208, 3216
NCC = 1032
NCB = 1152


class Tok:
    __slots__ = ("sem", "v", "q")

    def __init__(self, sem, v, q):
        self.sem = sem
        self.v = v
        self.q = q


class Buf:
    __slots__ = ("name", "w", "r")

    def __init__(self, name=""):
        self.name = name
        self.w = None
        self.r = {}


class DSem:
    def __init__(self, nc, es, name):
        self.sem = es.enter_context(nc.semaphore(name))
        self.n = 0


class Q:
    def __init__(self, nc, es, name, is_pe=False):
        self.name = name
        self.sem = es.enter_context(nc.semaphore("q_" + name))
        self.n = 0
        self.ops = []
        self.waited = {}
        self.is_pe = is_pe

    def wait(self, toks):
        for t in toks:
            if t is None:
                continue
            k = id(t.sem)
            if self.waited.get(k, 0) >= t.v:
                continue
            self.waited[k] = t.v
            self.ops.append(lambda e, t=t: e.wait_ge(t.sem, t.v))

    def _deps(self, reads, writes, dma):
        deps = []
        for b in reads:
            t = b.w
            if t is not None and (dma or not (t.q is self and self.is_pe)):
                deps.append(t)
        for b in writes:
            for t in list(b.r.values()) + [b.w]:
                if t is None:
                    continue
                if (not dma) and t.q is self:
                    continue
                deps.append(t)
        return deps

    @staticmethod
    def _mark(tok, reads, writes):
        k = id(tok.sem)
        for b in reads:
            o = b.r.get(k)
            if o is None or o.v < tok.v:
                b.r[k] = tok
        for b in writes:
            b.w = tok
            b.r = {}

    def op(self, fn, reads=(), writes=(), extra=()):
        self.wait(self._deps(reads, writes, False) + list(extra))
        self.n += 1
        n = self.n
        sem = self.sem
        self.ops.append(lambda e: fn(e).then_inc(sem, 1))
        tok = Tok(sem, n, self)
        self._mark(tok, reads, writes)
        return tok

    def dma(self, dsem, out, in_, reads=(), writes=(), extra=()):
        self.wait(self._deps(reads, writes, True) + list(extra))
        dsem.n += 16
        s = dsem.sem
        self.ops.append(lambda e: e.dma_start(out=out, in_=in_).then_inc(s, 16))
        tok = Tok(s, dsem.n, dsem)
        self._mark(tok, reads, writes)
        return tok

    def dma_group(self, dsem, pairs, reads=(), writes=(), extra=()):
        self.wait(self._deps(reads, writes, True) + list(extra))
        s = dsem.sem
        for (out, in_) in pairs:
            dsem.n += 16
            self.ops.append(lambda e, out=out, in_=in_: e.dma_start(out=out, in_=in_).then_inc(s, 16))
        tok = Tok(s, dsem.n, dsem)
        self._mark(tok, reads, writes)
        return tok

    def replay(self, e):
        for o in self.ops:
            o(e)


def _prod(s):
    r = 1
    for v in s:
        r *= int(v)
    return r


ARENA = 210944


class Prog:
    def __init__(self, stop="full", debug=False):
        self.stop = stop
        self.debug = debug
        nc = self.nc = bass.Bass("TRN2", target_bir_lowering=False, num_devices=8)
        es = self.es = ExitStack()
        dt = nc.dram_tensor

        def din(name, shape, d=F32):
            return dt(name, shape, d, kind="ExternalInput").ap()
        self.x_d = din("x", [NB, 128, D])
        self.pos_d = din("pos", [128, NB], I32)
        self.norms_d = din("norms", [5, D])
        self.wg_d = [din("wg1", [NFG, 128, 16 * 256]), din("wg2", [NFG, 128, 16 * 256])]
        self.wu_d = [din("wu1", [NFG, 128, 16 * 256]), din("wu2", [NFG, 128, 16 * 256])]
        self.wd_d = [din("wd1", [NFG, 128, 2 * D]), din("wd2", [NFG, 128, 2 * D])]
        self.win_d = din("win", [14, 128, 16 * 512])
        self.wout_d = din("wout", [4, 128, 16 * 512])
        self.cstf_d = din("cstf", [128, NF])
        self.cstc_d = din("cstc", [128, NCC])
        self.cstb_d = din("cstb", [128, NCB], BF16)
        self.out_d = dt("out", [NB, 128, D], F32, kind="ExternalOutput").ap()
        if debug:
            self.dbg_mix = dt("dbg_mix", [128, 16384], BF16, kind="ExternalOutput").ap()
            self.dbg_x = dt("dbg_x", [NB, 128, D], F32, kind="ExternalOutput").ap()
        self.xspill = dt("xspill", [NB, 128, D], F32, kind="Internal").ap()
        self.kv_in = dt("kv_in", [NB * 8 * 128, 128], F32, kind="Internal").ap()
        self.kv_all = dt("kv_all", [8 * NB * 8 * 128, 128], F32, kind="Internal").ap()
        self.kb_in = dt("kb_in", [128, 5120], BF16, kind="Internal").ap()
        self.kb_all = dt("kb_all", [1024, 5120], BF16, kind="Internal").ap()

        self.arena = es.enter_context(nc.sbuf_tensor("arena", [128, ARENA // 4], F32))
        self.arena_b = self.arena.bitcast(BF16)
        self.arena_i = self.arena.bitcast(I32)
        self.ps = es.enter_context(nc.psum_tensor("ps", [128, 4096], F32))
        self.psb = self.ps.bitcast(BF16)
        self.pbuf = [Buf("pb%d" % i) for i in range(8)]

        self.pe = Q(nc, es, "pe", True)
        self.act = Q(nc, es, "act")
        self.dve = Q(nc, es, "dve")
        self.pool = Q(nc, es, "pool")
        self.sp = Q(nc, es, "sp")
        self.queues = [self.pe, self.act, self.dve, self.pool, self.sp]
        self.dsems = []
        self.cc_sem = es.enter_context(nc.semaphore("cc"))
        self.cc_n = 0

        self.layout()
        self.build()

        blk = es.enter_context(nc.Block())
        blk.sync(self.sp.replay)
        blk.tensor(self.pe.replay)
        blk.scalar(self.act.replay)
        blk.vector(self.dve.replay)
        blk.gpsimd(self.pool.replay)
        es.close()

    def dsem(self, name):
        d = DSem(self.nc, self.es, name)
        self.dsems.append(d)
        return d

    def carve(self, off, shape, dtype):
        n = _prod(shape[1:])
        if dtype == BF16:
            assert off % 2 == 0
            ap = self.arena_b[:, off // 2: off // 2 + n]
        elif dtype == I32:
            ap = self.arena_i[:, off // 4: off // 4 + n]
        else:
            assert off % 4 == 0
            ap = self.arena[:, off // 4: off // 4 + n]
        if len(shape) == 3:
            ap = ap.rearrange("p (a b) -> p a b", a=shape[1], b=shape[2])
        elif len(shape) == 4:
            ap = ap.rearrange("p (a b c) -> p a b c", a=shape[1], b=shape[2], c=shape[3])
        elif len(shape) == 5:
            ap = ap.rearrange("p (a b c d) -> p a b c d", a=shape[1], b=shape[2], c=shape[3], d=shape[4])
        return ap

    def bank(self, i, n=512, off=0):
        return self.ps[:, i * 512 + off: i * 512 + off + n]

    def bankb(self, i, n=1024, off=0):
        return self.psb[:, i * 1024 + off: i * 1024 + off + n]

    def barrier(self):
        toks = [Tok(q.sem, q.n, q) for q in self.queues if q.n > 0]
        toks += [Tok(d.sem, d.n, d) for d in self.dsems if d.n > 0]
        if self.cc_n:
            toks.append(Tok(self.cc_sem, self.cc_n, None))
        for q in self.queues:
            q.wait(toks)

    def layout(self):
        c = self.carve
        R_X, R_HT, R_W, R_WD, R_GBC, R_T, R_CST = 0, 65536, 98304, 131072, 147456, 155648, 176128
        self.xs = c(R_X, [128, NB, D], F32)
        self.xs_b = [Buf("xs%d" % i) for i in range(NB)]
        self.hT = c(R_HT, [128, 16, 1024], BF16)
        self.hT_b = Buf("hT")
        self.wgu = [c(R_W + s * 16384, [128, 2, 16, 256], BF16) for s in range(2)]
        self.win = [c(R_W + s * 16384, [128, 16, 512], BF16) for s in range(2)]
        self.wg_b = [Buf("wg%d" % s) for s in range(2)]
        self.wu_b = [Buf("wu%d" % s) for s in range(2)]
        self.wdr = [c(R_WD + s * 8192, [128, 2, D], BF16) for s in range(2)]
        self.wd_b = [Buf("wd%d" % s) for s in range(2)]
        self.dw = [self.dsem("dw%d" % s) for s in range(2)]
        self.dwd = [self.dsem("dwd%d" % s) for s in range(2)]
        self.gbc = c(R_GBC, [128, D], F32)
        self.gbc_b = Buf("gbc")
        self.aT = [c(R_T + g * 4096, [128, 2, 1024], BF16) for g in range(2)]
        self.aT_b = [Buf("aT%d" % g) for g in range(2)]
        self.sg = [c(R_T + 8192 + s * 2048, [128, 512], F32) for s in range(2)]
        self.sg_b = [Buf("sg%d" % s) for s in range(2)]
        self.hb = [c(R_T + 12288 + s * 4096, [128, D], BF16) for s in range(2)]
        self.hb_b = [Buf("hb%d" % s) for s in range(2)]
        o = R_CST
        self.cstf = c(o, [128, NF], F32); o += NF * 4
        self.cstc = c(o, [128, NCC], F32); o += NCC * 4
        self.cstb = c(o, [128, NCB], BF16); o += NCB * 2
        self.cs128 = c(o, [128, NB, 64], F32); o += 2048
        self.sn128 = c(o, [128, NB, 64], F32); o += 2048
        self.cs64 = c(o, [128, NB, 32], F32); o += 1024
        self.sn64 = c(o, [128, NB, 32], F32); o += 1024
        self.wabs = c(o, [128, NB, 16], F32); o += 512
        self.wsgn = c(o, [128, NB, 16], F32); o += 512
        self.stat = c(o, [128, 256], F32); o += 1024
        self.posi = c(o, [128, NB], I32); o += 32
        self.stg = [c(o + s * 2048, [128, 512], F32) for s in range(2)]
        self.stgb = [c(o + s * 2048, [128, 1024], BF16) for s in range(2)]
        o += 4096
        assert o <= ARENA, o
        self.cst_b = Buf("cst")
        self.rope_b = Buf("ropetab")
        self.wabs_b = Buf("wabs")
        self.stat_b = Buf("stat")
        self.stg_b = [Buf("stg%d" % s) for s in range(2)]
        self.ident4 = self.cstb[:, 0:512]
        self.ident = self.cstb[:, 0:128]
        self.tri4 = self.cstb[:, 512:1024]
        self.ones = self.cstb[:, 1024:1152]
        self.cb = self.cstc[:, 0:1024]
        self.selc = self.cstc[:, 1024:1032]
        self.qT = c(R_X, [128, NB, 8, 128], BF16)
        self.kT = c(R_X + 16384, [128, NB, 8, 128], BF16)
        self.vr = c(R_X + 32768, [128, NB, 1024], BF16)
        self.sgr = c(R_X + 49152, [128, NB, 1024], BF16)
        self.aqT = c(R_WD, [128, NB, 8, 128], BF16)
        BT = R_GBC
        self.ropeA = c(BT, [128, 512], F32)
        self.ropeB = c(BT + 2048, [128, 512], F32)
        self.bout = [c(BT + 4096 + s * 2048, [128, 512], F32) for s in range(2)]
        self.boutb = [c(BT + 4096 + s * 2048, [128, 512], BF16) for s in range(2)]
        self.aux = [c(BT + 8192 + s * 1024, [128, 512], BF16) for s in range(2)]
        self.iqT = c(BT + 12288, [128, NB, 8, 128], BF16)
        self.qT_b, self.kT_b, self.vr_b, self.sgr_b = Buf("qT"), Buf("kT"), Buf("vr"), Buf("sgr")
        self.aqT_b, self.iqT_b = Buf("aqT"), Buf("iqT")
        self.ropeA_b, self.ropeB_b = Buf("ropeA"), Buf("ropeB")
        self.bout_b = [Buf("bout0"), Buf("bout1")]
        self.aux_b = [Buf("aux0"), Buf("aux1")]
        self.t0a = c(BT, [128, NB, 64], F32)
        self.t0b = c(BT + 2048, [128, NB, 64], F32)
        self.t0i = c(BT + 4096, [128, NB, 64], I32)
        self.t0c = c(BT + 6144, [128, NB, 64], F32)
        self.snap = c(R_W, [128, 2, 4, 1024], BF16)
        self.S = c(R_W + 16384, [128, 1024], F32)
        self.kvbuf = [c(R_W + 20480 + s * 4096, [128, 8, 128], F32) for s in range(2)]
        self.gnr = c(R_W + 28672, [128, 1024], F32)
        self.snap_b, self.S_b, self.gnr_b = Buf("snap"), Buf("S"), Buf("gnr")
        self.kvbuf_b = [Buf("kvb0"), Buf("kvb1")]
        self.dkv = [self.dsem("dkv%d" % s) for s in range(2)]
        self.t1 = c(BT, [128, 1024], F32)
        self.t2 = c(BT + 4096, [128, 1024], F32)
        self.ybf = c(BT + 8192, [128, 1024], BF16)
        self.sTm = [c(BT + 10240 + s * 1024, [128, 512], BF16) for s in range(2)]
        self.t1_b, self.t2_b, self.ybf_b = Buf("t1"), Buf("t2"), Buf("ybf")
        self.sTm_b = [Buf("sTm0"), Buf("sTm1")]
        self.KT = c(R_X, [128, 2, 4096], BF16)
        self.V = c(R_X + 16384, [128, 32, 2, 128], BF16)
        self.ikT = c(R_X + 32768, [128, 4096], BF16)
        self.rb = [c(R_X + 40960 + s * 2048, [128, 512], F32) for s in range(4)]
        self.pT = [c(R_X + 49152 + s * 1024, [128, 512], BF16) for s in range(2)]
        self.rcp = c(R_X + 51200, [128, 512], F32)
        self.KT_b, self.V_b, self.ikT_b = Buf("KT"), Buf("V"), Buf("ikT")
        self.rb_b = [Buf("rb%d" % s) for s in range(4)]
        self.pT_b = [Buf("pT0"), Buf("pT1")]
        self.rcp_b = Buf("rcp")
        self.score = c(R_W, [128, 4096], F32)
        self.mb = c(R_W + 16384, [128, 4096], BF16)
        self.junk = c(R_W + 24576, [128, 4096], BF16)
        self.score_b, self.mb_b, self.junk_b = Buf("score"), Buf("mb"), Buf("junk")
        self.mixT = self.hT
        self.xspill_b = [Buf("xsp%d" % i) for i in range(NB)]
        self.kv_in_b, self.kb_in_b = Buf("kv_in"), Buf("kb_in")
        self.kv_all_b, self.kb_all_b = Buf("kv_all"), Buf("kb_all")
        self.d_ld = self.dsem("d_ld")
        self.d_x = [self.dsem("d_x%d" % i) for i in range(NB)]
        self.d_st = self.dsem("d_st")
        self.d_g = self.dsem("d_g")
        self.d_kt, self.d_v, self.d_ik = self.dsem("d_kt"), self.dsem("d_v"), self.dsem("d_ik")
        self.d_stg = [self.dsem("d_stg%d" % i) for i in range(2)]

    def load_x(self, src):
        for lb in range(NB):
            self.sp.dma(self.d_x[lb], self.xs[:, lb, :], src[lb], writes=[self.xs_b[lb]])

    def sincos(self, ang_fn, shape, out_sin, shift, n):
        dve, act = self.dve, self.act
        tb = self.t0b[:, :, 0:n]
        ti = self.t0i[:, :, 0:n]
        tc_ = self.t0c[:, :, 0:n]
        B1, B2, B3 = self.ropeB_b, self.bout_b[0], self.bout_b[1]
        ang = ang_fn
        dve.op(lambda e: e.tensor_scalar(out=tc_, in0=ang, scalar1=float(shift), scalar2=None, op0=ALU.add), reads=[self.ropeA_b], writes=[B3])
        dve.op(lambda e: e.tensor_scalar(out=tb, in0=tc_, scalar1=float(1.0 / (2 * PI)), scalar2=None, op0=ALU.mult), reads=[B3], writes=[B1])
        dve.op(lambda e: e.tensor_copy(out=ti, in_=tb), reads=[B1], writes=[B2])
        dve.op(lambda e: e.tensor_copy(out=tb, in_=ti), reads=[B2], writes=[B1])
        dve.op(lambda e: e.scalar_tensor_tensor(out=tc_, in0=tb, scalar=float(-2 * PI), in1=tc_, op0=ALU.mult, op1=ALU.add), reads=[B1, B3], writes=[B3])
        dve.op(lambda e: e.tensor_scalar(out=tb, in0=tc_, scalar1=PI, scalar2=float(-2 * PI), op0=ALU.is_gt, op1=ALU.mult), reads=[B3], writes=[B1])
        dve.op(lambda e: e.tensor_tensor(out=tc_, in0=tc_, in1=tb, op=ALU.add), reads=[B1, B3], writes=[B3])
        dve.op(lambda e: e.tensor_scalar(out=tb, in0=tc_, scalar1=-PI, scalar2=float(2 * PI), op0=ALU.is_lt, op1=ALU.mult), reads=[B3], writes=[B1])
        dve.op(lambda e: e.tensor_tensor(out=tc_, in0=tc_, in1=tb, op=ALU.add), reads=[B1, B3], writes=[B3])
        act.op(lambda e: e.activation(out=out_sin, in_=tc_, func=AF.Sin), reads=[B3], writes=[self.rope_b])

    def phase0(self):
        sp, dve = self.sp, self.dve
        sp.dma(self.d_ld, self.cstf, self.cstf_d, writes=[self.cst_b])
        sp.dma(self.d_ld, self.cstc, self.cstc_d, writes=[self.cst_b])
        sp.dma(self.d_ld, self.cstb, self.cstb_d, writes=[self.cst_b])
        sp.dma(self.d_ld, self.posi, self.pos_d, writes=[self.cst_b])
        self.load_x(self.x_d)
        posf = self.stat[:, 0:NB]
        dve.op(lambda e: e.tensor_copy(out=posf, in_=self.posi), reads=[self.cst_b], writes=[self.stat_b])
        for n, inv_off, cs, sn in ((64, C_INV128, self.cs128, self.sn128), (32, C_INV64, self.cs64, self.sn64)):
            ang = self.t0a[:, :, 0:n]
            inv = self.cstf[:, inv_off:inv_off + n]
            dve.op(lambda e, ang=ang, inv=inv, n=n: e.tensor_tensor(
                out=ang, in0=inv.unsqueeze(1).to_broadcast([128, NB, n]),
                in1=posf.unsqueeze(2).to_broadcast([128, NB, n]), op=ALU.mult),
                reads=[self.cst_b, self.stat_b], writes=[self.ropeA_b])
            self.sincos(ang, None, sn, 0.0, n)
            self.sincos(ang, None, cs, PI / 2, n)

    def norm_stats(self, row):
        sp, act, dve = self.sp, self.act, self.dve
        sp.dma(self.d_g, self.gbc, self.norms_d[row:row + 1, :].partition_broadcast(128), writes=[self.gbc_b])
        ss = self.stat[:, 16:16 + NB]
        for lb in range(NB):
            act.op(lambda e, lb=lb: e.activation(out=self.hb[1], in_=self.xs[:, lb, :], func=AF.Square,
                                                 accum_out=ss[:, lb:lb + 1]),
                   reads=[self.xs_b[lb]], writes=[self.hb_b[1], self.stat_b])
        rstd = self.stat[:, 32:32 + NB]
        act.op(lambda e: e.activation(out=rstd, in_=ss, func=AF.Sqrt, scale=1.0 / D,
                                      bias=self.cstf[:, C_EPS:C_EPS + 1]),
               reads=[self.stat_b, self.cst_b], writes=[self.stat_b])
        dve.op(lambda e: e.reciprocal(out=rstd, in_=rstd), reads=[self.stat_b], writes=[self.stat_b])
        return rstd

    def norm_T(self, row):
        pe, act, dve = self.pe, self.act, self.dve
        rstd = self.norm_stats(row)
        for lb in range(NB):
            s = lb % 2
            hb = self.hb[s]
            dve.op(lambda e, lb=lb, hb=hb: e.scalar_tensor_tensor(
                out=hb, in0=self.xs[:, lb, :], scalar=rstd[:, lb:lb + 1], in1=self.gbc,
                op0=ALU.mult, op1=ALU.mult),
                reads=[self.xs_b[lb], self.stat_b, self.gbc_b], writes=[self.hb_b[s]])
            for half in range(2):
                bk = 4 + 2 * s + half
                for i in range(8):
                    kc = half * 8 + i
                    pe.op(lambda e, bk=bk, i=i, kc=kc, hb=hb: e.transpose(
                        out=self.bankb(bk, 128, i * 128), in_=hb[:, kc * 128:(kc + 1) * 128], identity=self.ident),
                        reads=[self.hb_b[s], self.cst_b], writes=[self.pbuf[bk]])
                dst = self.hT[:, half * 8:(half + 1) * 8, lb * 128:(lb + 1) * 128]
                src = self.bankb(bk).rearrange("p (a b) -> p a b", a=8, b=128)
                eng = act if half == 0 else dve
                if eng is act:
                    eng.op(lambda e, dst=dst, src=src: e.activation(out=dst, in_=src, func=AF.Copy),
                           reads=[self.pbuf[bk]], writes=[self.hT_b])
                else:
                    eng.op(lambda e, dst=dst, src=src: e.tensor_copy(out=dst, in_=src),
                           reads=[self.pbuf[bk]], writes=[self.hT_b])

    def final_norm(self):
        dve, sp = self.dve, self.sp
        rstd = self.norm_stats(3)
        last = None
        for lb in range(NB):
            s = lb % 2
            o = self.sg[s]
            dve.op(lambda e, lb=lb: e.scalar_tensor_tensor(
                out=self.xs[:, lb, :], in0=self.xs[:, lb, :], scalar=rstd[:, lb:lb + 1], in1=self.gbc,
                op0=ALU.mult, op1=ALU.mult),
                reads=[self.xs_b[lb], self.stat_b, self.gbc_b], writes=[self.xs_b[lb]])
            last = sp.dma(self.d_st, self.out_d[lb], self.xs[:, lb, :], reads=[self.xs_b[lb]])
        return last

    def ffn(self, idx):
        pe, act, dve, pool = self.pe, self.act, self.dve, self.pool
        wg_d, wu_d, wd_d = self.wg_d[idx], self.wu_d[idx], self.wd_d[idx]

        def load(fg):
            s = fg % 2
            pool.dma(self.dw[s], self.wgu[s][:, 0].rearrange("p a b -> p (a b)"), wg_d[fg], writes=[self.wg_b[s]])
            pool.dma(self.dw[s], self.wgu[s][:, 1].rearrange("p a b -> p (a b)"), wu_d[fg], writes=[self.wu_b[s]])
            pool.dma(self.dwd[s], self.wdr[s].rearrange("p a b -> p (a b)"), wd_d[fg], writes=[self.wd_b[s]])

        def gate_up(fg):
            s = fg % 2
            for f in range(2):
                for half in range(2):
                    u = f * 2 + half
                    pg, pu = (u % 2) * 2, (u % 2) * 2 + 1
                    rhs_sl = slice(half * 512, (half + 1) * 512)
                    for (bk, wi, wb) in ((pg, 0, self.wg_b[s]), (pu, 1, self.wu_b[s])):
                        for kc in range(16):
                            pe.op(lambda e, bk=bk, wi=wi, kc=kc, f=f, s=s, rhs_sl=rhs_sl: e.matmul(
                                self.bank(bk), lhsT=self.wgu[s][:, wi, kc, f * 128:(f + 1) * 128],
                                rhs=self.hT[:, kc, rhs_sl], start=(kc == 0), stop=(kc == 15)),
                                reads=[wb, self.hT_b], writes=[self.pbuf[bk]])
                    sgs = u % 2
                    act.op(lambda e, pg=pg, sgs=sgs: e.activation(out=self.sg[sgs], in_=self.bank(pg), func=AF.Silu),
                           reads=[self.pbuf[pg]], writes=[self.sg_b[sgs]])
                    dve.op(lambda e, pu=pu, sgs=sgs, fg=fg, f=f, rhs_sl=rhs_sl: e.tensor_tensor(
                        out=self.aT[fg % 2][:, f, rhs_sl], in0=self.sg[sgs], in1=self.bank(pu), op=ALU.mult),
                        reads=[self.sg_b[sgs], self.pbuf[pu]], writes=[self.aT_b[fg % 2]])

        def down(fg):
            s = fg % 2
            for tb in range(NB):
                for ch in range(2):
                    par = (tb * 2 + ch) % 2
                    for cbk in range(2):
                        bk = 4 + par * 2 + cbk
                        for f in range(2):
                            c0 = ch * 1024 + cbk * 512
                            pe.op(lambda e, bk=bk, f=f, tb=tb, c0=c0, fg=fg, s=s: e.matmul(
                                self.bank(bk), lhsT=self.aT[fg % 2][:, f, tb * 128:(tb + 1) * 128],
                                rhs=self.wdr[s][:, f, c0:c0 + 512], start=(f == 0), stop=(f == 1)),
                                reads=[self.aT_b[fg % 2], self.wd_b[s]], writes=[self.pbuf[bk]])
                    b0 = 4 + par * 2
                    xsl = self.xs[:, tb, ch * 1024:(ch + 1) * 1024]
                    dve.op(lambda e, b0=b0, xsl=xsl: e.scalar_tensor_tensor(
                        out=xsl, in0=self.bank(b0, 1024), scalar=0.5, in1=xsl, op0=ALU.mult, op1=ALU.add),
                        reads=[self.pbuf[b0], self.pbuf[b0 + 1], self.xs_b[tb]], writes=[self.xs_b[tb]])

        nfg = NFG
        load(0)
        load(1)
        for fg in range(nfg + 1):
            if fg < nfg:
                gate_up(fg)
            if fg >= 1:
                down(fg - 1)
                if fg + 1 < nfg:
                    load(fg + 1)

    def rope(self, bk, col0, H, dh, cos, sin, out, out_b, extra_reads=()):
        dve = self.dve
        hd = dh // 2
        n = H * dh
        v = self.bank(bk, n, col0).rearrange("p (h t d) -> p h t d", h=H, t=2, d=hd)
        A = self.ropeA[:, 0:n].rearrange("p (h t d) -> p h t d", h=H, t=2, d=hd)
        Bt = self.ropeB[:, 0:n].rearrange("p (h t d) -> p h t d", h=H, t=2, d=hd)
        cos_b4 = cos.unsqueeze(1).unsqueeze(1).to_broadcast([128, H, 2, hd])
        sin_b3 = sin.unsqueeze(1).to_broadcast([128, H, hd])
        rd = [self.pbuf[bk], self.rope_b] + list(extra_reads)
        dve.op(lambda e: e.tensor_tensor(out=A, in0=v, in1=cos_b4, op=ALU.mult), reads=rd, writes=[self.ropeA_b])
        dve.op(lambda e: e.scalar_tensor_tensor(out=Bt[:, :, 0, :], in0=v[:, :, 1, :], scalar=-1.0, in1=sin_b3,
                                                op0=ALU.mult, op1=ALU.mult), reads=rd, writes=[self.ropeB_b])
        self.rope_ps_tok = dve.op(lambda e: e.tensor_tensor(out=Bt[:, :, 1, :], in0=v[:, :, 0, :], in1=sin_b3, op=ALU.mult),
                                  reads=rd, writes=[self.ropeB_b])
        return dve.op(lambda e: e.tensor_tensor(out=out, in0=self.ropeA[:, 0:n], in1=self.ropeB[:, 0:n], op=ALU.add),
                      reads=[self.ropeA_b, self.ropeB_b], writes=[out_b])

    def phaseB(self):
        pe, act, dve, pool, sp = self.pe, self.act, self.dve, self.pool, self.sp
        self.norm_T(1)
        for lb in range(NB):
            sp.dma(self.d_st, self.xspill[lb], self.xs[:, lb, :], reads=[self.xs_b[lb]], writes=[self.xspill_b[lb]])
        self.barrier()

        def load(cg):
            s = cg % 2
            pool.dma(self.dw[s], self.win[s].rearrange("p a b -> p (a b)"), self.win_d[cg],
                     writes=[self.wg_b[s], self.wu_b[s]])

        fams = ["rv", "rv", "rk", "rk", "rq", "rq", "rg", "rg", "aq", "aq", "akav", "ikiw", "iq", "iq"]
        load(0)
        load(1)
        self.tpc = 0
        self.kvc = 0
        pending = []
        u = 0
        kvrows = self.kv_in.rearrange("(l h d) e -> d l h e", l=NB, h=8, d=128)
        for cg in range(14):
            s = cg % 2
            fam = fams[cg]
            hs = cg % 2 if fam != "akav" and fam != "ikiw" else 0
            if fam in ("rv", "rk", "rq", "rg", "aq", "iq"):
                hs = [c for c in range(14) if fams[c] == fam].index(cg)
            ncols = 144 if fam == "ikiw" else 512
            for lb in range(NB):
                bk = u % 4
                for kc in range(16):
                    pe.op(lambda e, bk=bk, kc=kc, lb=lb, s=s, ncols=ncols: e.matmul(
                        self.bank(bk, ncols), lhsT=self.hT[:, kc, lb * 128:(lb + 1) * 128],
                        rhs=self.win[s][:, kc, 0:ncols], start=(kc == 0), stop=(kc == 15)),
                        reads=[self.hT_b, self.wg_b[s], self.wu_b[s]], writes=[self.pbuf[bk]])
                for fn in pending:
                    fn()
                pending = []
                pending.append(self.postB(fam, hs, lb, bk, u, kvrows))
                u += 1
            if cg + 2 < 14:
                load(cg + 2)
        for fn in pending:
            fn()

    def postB(self, fam, hs, lb, bk, u, kvrows):
        pe, act, dve, sp = self.pe, self.act, self.dve, self.sp
        pb = self.pbuf[bk]
        so = u % 2
        h0 = hs * 4

        def transposes(src_ap, src_b, nblk):
            tb = 4 + (self.tpc % 2)
            self.tpc += 1
            for i in range(nblk):
                pe.op(lambda e, i=i, tb=tb: e.transpose(out=self.bankb(tb, 128, i * 128),
                                                        in_=src_ap[:, i * 128:(i + 1) * 128], identity=self.ident),
                      reads=[src_b, self.cst_b], writes=[self.pbuf[tb]])
            return tb

        if fam == "rv":
            act.op(lambda e: e.activation(out=self.vr[:, lb, hs * 512:(hs + 1) * 512], in_=self.bank(bk), func=AF.Copy),
                   reads=[pb], writes=[self.vr_b])
            return lambda: None
        if fam == "rg":
            act.op(lambda e: e.activation(out=self.sgr[:, lb, hs * 512:(hs + 1) * 512], in_=self.bank(bk), func=AF.Silu),
                   reads=[pb], writes=[self.sgr_b])
            return lambda: None
        if fam in ("rk", "rq", "aq"):
            ob = self.boutb[so]
            self.rope(bk, 0, 4, 128, self.cs128[:, lb, :], self.sn128[:, lb, :], ob, self.bout_b[so])
            if fam == "rk":
                gk_b = self.cstf[:, C_GK + h0:C_GK + h0 + 4].unsqueeze(2).to_broadcast([128, 4, 128])
                dve.op(lambda e: e.tensor_tensor(out=self.aux[so].rearrange("p (h d) -> p h d", h=4, d=128),
                                                 in0=ob.rearrange("p (h d) -> p h d", h=4, d=128), in1=gk_b, op=ALU.mult),
                       reads=[self.bout_b[so], self.cst_b], writes=[self.aux_b[so]])

            def deferred():
                tb = transposes(ob, self.bout_b[so], 4)
                if fam == "rk":
                    dst = self.kT[:, lb, h0:h0 + 4, :].rearrange("p h t -> p (h t)")
                    tab = self.cstf[:, C_GKT + h0 * 128:C_GKT + h0 * 128 + 512]
                    dve.op(lambda e: e.tensor_tensor(out=dst, in0=self.bankb(tb, 512), in1=tab, op=ALU.mult),
                           reads=[self.pbuf[tb], self.cst_b], writes=[self.kT_b])
                    kb = 6 + (self.kvc % 2)
                    self.kvc += 1
                    for h in range(4):
                        pe.op(lambda e, h=h, kb=kb: e.matmul(
                            self.bank(kb, 128, h * 128), lhsT=self.aux[so][:, h * 128:(h + 1) * 128],
                            rhs=self.vr[:, lb, (h0 + h) * 128:(h0 + h + 1) * 128], start=True, stop=True),
                            reads=[self.aux_b[so], self.vr_b], writes=[self.pbuf[kb]])
                    act.op(lambda e, kb=kb: e.activation(out=self.stg[so], in_=self.bank(kb), func=AF.Copy),
                           reads=[self.pbuf[kb]], writes=[self.stg_b[so]])
                    sp.dma(self.d_stg[so], kvrows[:, lb, h0:h0 + 4, :],
                           self.stg[so].rearrange("p (h e) -> p h e", h=4, e=128),
                           reads=[self.stg_b[so]])
                elif fam == "rq":
                    dst = self.qT[:, lb, h0:h0 + 4, :].rearrange("p h t -> p (h t)")
                    tab = self.cstf[:, C_GQT + h0 * 128:C_GQT + h0 * 128 + 512]
                    dve.op(lambda e: e.tensor_tensor(out=dst, in0=self.bankb(tb, 512), in1=tab, op=ALU.mult),
                           reads=[self.pbuf[tb], self.cst_b], writes=[self.qT_b])
                else:
                    dst = self.aqT[:, lb, h0:h0 + 4, :].rearrange("p h t -> p (h t)")
                    act.op(lambda e: e.activation(out=dst, in_=self.bankb(tb, 512), func=AF.Copy),
                           reads=[self.pbuf[tb]], writes=[self.aqT_b])
            return deferred
        if fam == "akav":
            ob = self.boutb[so][:, 0:256]
            self.rope(bk, 0, 2, 128, self.cs128[:, lb, :], self.sn128[:, lb, :], ob, self.bout_b[so])
            avst = self.stgb[so][:, 0:256]
            act.op(lambda e: e.activation(out=avst, in_=self.bank(bk, 256, 256), func=AF.Copy),
                   reads=[pb], writes=[self.stg_b[so]], extra=[self.rope_ps_tok])

            def deferred():
                tb = transposes(ob, self.bout_b[so], 2)
                akst = self.stgb[so][:, 256:512]
                act.op(lambda e: e.activation(out=akst, in_=self.bankb(tb, 256), func=AF.Copy),
                       reads=[self.pbuf[tb]], writes=[self.stg_b[so]])
                sp.dma(self.d_stg[so], self.kb_in[:, lb * 256:(lb + 1) * 256], akst, reads=[self.stg_b[so]])
                sp.dma(self.d_stg[so], self.kb_in[:, 3072 + lb * 256:3072 + (lb + 1) * 256], avst,
                       reads=[self.stg_b[so]])
            return deferred
        if fam == "ikiw":
            ob = self.boutb[so][:, 0:128]
            self.rope(bk, 0, 2, 64, self.cs64[:, lb, :], self.sn64[:, lb, :], ob, self.bout_b[so])
            act.op(lambda e: e.activation(out=self.wabs[:, lb, :], in_=self.bank(bk, 16, 128), func=AF.Abs, scale=1.0 / 32),
                   reads=[pb], writes=[self.wabs_b], extra=[self.rope_ps_tok])
            act.op(lambda e: e.activation(out=self.wsgn[:, lb, :], in_=self.bank(bk, 16, 128), func=AF.Sign),
                   reads=[pb], writes=[self.wabs_b])

            def deferred():
                tb = transposes(ob, self.bout_b[so], 1)
                ikst = self.stgb[so][:, 0:128]
                act.op(lambda e: e.activation(out=ikst, in_=self.bankb(tb, 128), func=AF.Copy),
                       reads=[self.pbuf[tb]], writes=[self.stg_b[so]])
                sp.dma(self.d_stg[so], self.kb_in[:, 2048 + lb * 128:2048 + (lb + 1) * 128], ikst,
                       reads=[self.stg_b[so]])
            return deferred
        if fam == "iq":
            of = self.bout[so]
            self.rope(bk, 0, 8, 64, self.cs64[:, lb, :], self.sn64[:, lb, :], of, self.bout_b[so])
            wb_ = self.wabs[:, lb, hs * 8:(hs + 1) * 8].unsqueeze(2).to_broadcast([128, 8, 64])
            dve.op(lambda e: e.tensor_tensor(out=self.aux[so].rearrange("p (h d) -> p h d", h=8, d=64),
                                             in0=of.rearrange("p (h d) -> p h d", h=8, d=64), in1=wb_, op=ALU.mult),
                   reads=[self.bout_b[so], self.wabs_b], writes=[self.aux_b[so]])

            def deferred():
                tb = transposes(self.aux[so], self.aux_b[so], 4)
                dst = self.iqT[:, lb, hs * 4:(hs + 1) * 4, :].rearrange("p h t -> p (h t)")
                act.op(lambda e: e.activation(out=dst, in_=self.bankb(tb, 512), func=AF.Copy),
                       reads=[self.pbuf[tb]], writes=[self.iqT_b])
            return deferred
        raise ValueError(fam)

    def allgather(self):
        self.barrier()
        pool = self.pool
        for (src, dst) in ((self.kv_in, self.kv_all), (self.kb_in, self.kb_all)):
            self.cc_n += 1
            sem = self.cc_sem
            pool.ops.append(lambda e, src=src, dst=dst: e.collective_compute(
                "AllGather", ALU.bypass, replica_groups=[list(range(8))], ins=[src], outs=[dst]).then_inc(sem, 1))
        self.barrier()

    def phaseC1(self):
        pe, act, dve, pool, sp = self.pe, self.act, self.dve, self.pool, self.sp
        sp.dma(self.d_g, self.gnr, self.norms_d[4:5, 0:1024].partition_broadcast(128), writes=[self.gnr_b])
        dve.op(lambda e: e.memset(self.snap.rearrange("p a b c -> p (a b c)"), 0.0), writes=[self.snap_b])
        kvsrc = self.kv_all.rearrange("(r l h d) e -> d r l h e", r=8, l=NB, h=8, d=128)
        gC = self.cstf[:, C_GC:C_GC + 1024]
        step = 0
        for b in range(2):
            dve.op(lambda e: e.memset(self.S, 0.0), writes=[self.S_b])
            for n in range(32):
                k, jj = divmod(n, 8)
                s = step % 2
                if n < 31:
                    sp.dma(self.dkv[s], self.kvbuf[s], kvsrc[:, jj, b * 4 + k, :, :], reads=[self.kv_all_b],
                           writes=[self.kvbuf_b[s]])
                sn = self.snap[:, b, k, :]
                dve.op(lambda e, sn=sn, jj=jj: e.scalar_tensor_tensor(
                    out=sn, in0=self.S, scalar=self.selc[:, jj:jj + 1], in1=sn, op0=ALU.mult, op1=ALU.add),
                    reads=[self.S_b, self.cst_b, self.snap_b], writes=[self.snap_b])
                if n < 31:
                    dve.op(lambda e, s=s: e.tensor_tensor(out=self.S, in0=self.S,
                                                          in1=self.kvbuf[s].rearrange("p h e -> p (h e)"), op=ALU.add),
                           reads=[self.S_b, self.kvbuf_b[s]], writes=[self.S_b])
                    dve.op(lambda e: e.tensor_tensor(out=self.S, in0=self.S, in1=gC, op=ALU.mult),
                           reads=[self.S_b, self.cst_b], writes=[self.S_b])
                    step += 1
        def block(lb):
            b, k = divmod(lb, 4)
            for hg in range(2):
                sbk, obk = hg, 2 + hg
                for h in range(4):
                    H = hg * 4 + h
                    pe.op(lambda e, h=h, H=H, sbk=sbk: e.matmul(
                        self.bank(sbk, 128, h * 128), lhsT=self.kT[:, lb, H, :], rhs=self.qT[:, lb, H, :],
                        start=True, stop=True), reads=[self.kT_b, self.qT_b], writes=[self.pbuf[sbk]])
                dve.op(lambda e, sbk=sbk, hg=hg: e.tensor_tensor(out=self.sTm[hg], in0=self.bank(sbk), in1=self.tri4,
                                                                 op=ALU.mult),
                       reads=[self.pbuf[sbk], self.cst_b], writes=[self.sTm_b[hg]])
                for h in range(4):
                    H = hg * 4 + h
                    pe.op(lambda e, h=h, H=H, obk=obk, hg=hg: e.matmul(
                        self.bank(obk, 128, h * 128), lhsT=self.sTm[hg][:, h * 128:(h + 1) * 128],
                        rhs=self.vr[:, lb, H * 128:(H + 1) * 128], start=True, stop=False),
                        reads=[self.sTm_b[hg], self.vr_b], writes=[self.pbuf[obk]])
                    pe.op(lambda e, h=h, H=H, obk=obk: e.matmul(
                        self.bank(obk, 128, h * 128), lhsT=self.qT[:, lb, H, :],
                        rhs=self.snap[:, b, k, H * 128:(H + 1) * 128], start=False, stop=True),
                        reads=[self.qT_b, self.snap_b], writes=[self.pbuf[obk]])
                act.op(lambda e, obk=obk, hg=hg: e.activation(out=self.t1[:, hg * 512:(hg + 1) * 512],
                                                              in_=self.bank(obk), func=AF.Copy),
                       reads=[self.pbuf[obk]], writes=[self.t1_b])
            t1v = self.t1.rearrange("p (h e) -> p h e", h=8, e=128)
            t2v = self.t2.rearrange("p (h e) -> p h e", h=8, e=128)
            s1 = self.stat[:, 64:72]
            s2 = self.stat[:, 72:80]
            dve.op(lambda e: e.tensor_reduce(out=s1, in_=t1v, axis=AX.X, op=ALU.add), reads=[self.t1_b], writes=[self.stat_b])
            dve.op(lambda e: e.tensor_scalar(out=s1, in0=s1, scalar1=-1.0 / 128, scalar2=None, op0=ALU.mult),
                   reads=[self.stat_b], writes=[self.stat_b])
            dve.op(lambda e: e.tensor_tensor(out=t2v, in0=t1v, in1=s1.unsqueeze(2).to_broadcast([128, 8, 128]), op=ALU.add),
                   reads=[self.t1_b, self.stat_b], writes=[self.t2_b])
            dve.op(lambda e: e.tensor_tensor(out=self.t1, in0=self.t2, in1=self.t2, op=ALU.mult),
                   reads=[self.t2_b], writes=[self.t1_b])
            dve.op(lambda e: e.tensor_reduce(out=s2, in_=t1v, axis=AX.X, op=ALU.add), reads=[self.t1_b], writes=[self.stat_b])
            act.op(lambda e: e.activation(out=s2, in_=s2, func=AF.Sqrt, scale=1.0 / 128, bias=self.cstf[:, C_EPS:C_EPS + 1]),
                   reads=[self.stat_b, self.cst_b], writes=[self.stat_b])
            dve.op(lambda e: e.reciprocal(out=s2, in_=s2), reads=[self.stat_b], writes=[self.stat_b])
            dve.op(lambda e: e.tensor_tensor(out=t1v, in0=t2v, in1=s2.unsqueeze(2).to_broadcast([128, 8, 128]), op=ALU.mult),
                   reads=[self.t2_b, self.stat_b], writes=[self.t1_b])
            dve.op(lambda e: e.tensor_tensor(out=self.t2, in0=self.t1, in1=self.gnr, op=ALU.mult),
                   reads=[self.t1_b, self.gnr_b], writes=[self.t2_b])
            dve.op(lambda e: e.tensor_tensor(out=self.ybf, in0=self.t2, in1=self.sgr[:, lb, :], op=ALU.mult),
                   reads=[self.t2_b, self.sgr_b], writes=[self.ybf_b])
            tb = 4 + lb % 2
            for h in range(8):
                pe.op(lambda e, h=h, tb=tb: e.transpose(out=self.bankb(tb, 128, h * 128),
                                                        in_=self.ybf[:, h * 128:(h + 1) * 128], identity=self.ident),
                      reads=[self.ybf_b, self.cst_b], writes=[self.pbuf[tb]])
            dst = self.mixT[:, 0:8, lb * 128:(lb + 1) * 128]
            act.op(lambda e, tb=tb, dst=dst: e.activation(out=dst, in_=self.bankb(tb).rearrange("p (a b) -> p a b", a=8, b=128),
                                                          func=AF.Copy),
                   reads=[self.pbuf[tb]], writes=[self.hT_b])

        for lb in range(NB):
            block(lb)

    def phaseC2(self):
        pe, act, dve, pool, sp = self.pe, self.act, self.dve, self.pool, self.sp
        kb = self.kb_all
        ksrc = kb[:, 0:2048].rearrange("(j p) (l g t) -> p g l j t", p=128, l=NB, g=2, t=128)
        isrc = kb[:, 2048:3072].rearrange("(j p) (l t) -> p l j t", p=128, l=NB, t=128)
        vsrc = kb[:, 3072:5120].rearrange("(j p) (l g d) -> p g l j d", p=128, l=NB, g=2, d=128)
        self.iu = 0
        self.au = 0
        scale = float(128 ** -0.5)
        for b in range(2):
            pk, pv, pi_ = [], [], []
            for k in range(4):
                for g in range(2):
                    pk.append((self.KT[:, g, k * 1024:(k + 1) * 1024].rearrange("p (j t) -> p j t", j=8, t=128),
                               ksrc[:, g, b * 4 + k, :, :]))
                    pv.append((self.V[:, k * 8:(k + 1) * 8, g, :], vsrc[:, g, b * 4 + k, :, :]))
                pi_.append((self.ikT[:, k * 1024:(k + 1) * 1024].rearrange("p (j t) -> p j t", j=8, t=128),
                            isrc[:, b * 4 + k, :, :]))
            sp.dma_group(self.d_kt, pk, reads=[self.kb_all_b], writes=[self.KT_b])
            sp.dma_group(self.d_v, pv, reads=[self.kb_all_b], writes=[self.V_b])
            sp.dma_group(self.d_ik, pi_, reads=[self.kb_all_b], writes=[self.ikT_b])
            def block(k, b=b):
                lb = b * 4 + k
                S = 1024 * (k + 1)
                for kg in range(S // 512):
                    for h in range(16):
                        hp, half = divmod(h, 2)
                        bk = self.iu % 2
                        r = self.iu % 4
                        self.iu += 1
                        pr = slice(half * 64, (half + 1) * 64)
                        pe.op(lambda e, bk=bk, pr=pr, hp=hp, kg=kg: e.matmul(
                            self.bank(bk), lhsT=self.iqT[pr, lb, hp, :], rhs=self.ikT[pr, kg * 512:(kg + 1) * 512],
                            start=True, stop=True), reads=[self.iqT_b, self.ikT_b], writes=[self.pbuf[bk]])
                        act.op(lambda e, bk=bk, r=r: e.activation(out=self.rb[r], in_=self.bank(bk), func=AF.Relu),
                               reads=[self.pbuf[bk]], writes=[self.rb_b[r]])
                        sc = self.score[:, kg * 512:(kg + 1) * 512]
                        sgn = self.wsgn[:, lb, h:h + 1]
                        if h == 0:
                            dve.op(lambda e, r=r, sc=sc, sgn=sgn: e.tensor_scalar(
                                out=sc, in0=self.rb[r], scalar1=sgn, scalar2=None, op0=ALU.mult),
                                reads=[self.rb_b[r], self.wabs_b], writes=[self.score_b])
                        else:
                            dve.op(lambda e, r=r, sc=sc, sgn=sgn: e.scalar_tensor_tensor(
                                out=sc, in0=self.rb[r], scalar=sgn, in1=sc, op0=ALU.mult, op1=ALU.add),
                                reads=[self.rb_b[r], self.wabs_b, self.score_b], writes=[self.score_b])
                st = self.stat
                rmax, rmin, w0, mid, cnt, tmp, tau = (st[:, 96:97], st[:, 97:98], st[:, 98:99], st[:, 99:100],
                                                      st[:, 100:101], st[:, 101:102], st[:, 102:103])
                wtab = st[:, 128:128 + NIT + 1]
                scv = self.score[:, 0:S]
                SB = self.stat_b
                dve.op(lambda e: e.tensor_reduce(out=rmax, in_=scv, axis=AX.X, op=ALU.max), reads=[self.score_b], writes=[SB])
                dve.op(lambda e: e.tensor_reduce(out=rmin, in_=scv, axis=AX.X, op=ALU.min), reads=[self.score_b], writes=[SB])
                dve.op(lambda e: e.tensor_tensor(out=self.score[:, S - 1024:S], in0=self.score[:, S - 1024:S], in1=self.cb,
                                                 op=ALU.add), reads=[self.score_b, self.cst_b], writes=[self.score_b])
                dve.op(lambda e: e.tensor_scalar(out=w0, in0=rmax, scalar1=rmin, scalar2=0.5, op0=ALU.subtract, op1=ALU.mult),
                       reads=[SB], writes=[SB])
                dve.op(lambda e: e.tensor_scalar(out=wtab, in0=self.cstf[:, C_POW2:C_POW2 + NIT + 1], scalar1=w0, scalar2=None,
                                                 op0=ALU.mult), reads=[SB, self.cst_b], writes=[SB])
                dve.op(lambda e: e.tensor_tensor(out=mid, in0=rmin, in1=wtab[:, 0:1], op=ALU.add), reads=[SB], writes=[SB])
                for i in range(NIT):
                    dve.op(lambda e: e.memset(cnt, 0.0), writes=[SB])
                    dve.op(lambda e: e.tensor_scalar(out=self.junk[:, 0:S], in0=scv, scalar1=mid, scalar2=0.0,
                                                     op0=ALU.is_ge, op1=ALU.add, accum_out=cnt),
                           reads=[self.score_b, SB], writes=[self.junk_b, SB])
                    dve.op(lambda e, i=i: e.tensor_scalar(out=tmp, in0=cnt, scalar1=256.0, scalar2=wtab[:, i:i + 1],
                                                          op0=ALU.is_ge, op1=ALU.mult), reads=[SB], writes=[SB])
                    dve.op(lambda e, i=i: e.scalar_tensor_tensor(out=mid, in0=tmp, scalar=wtab[:, i + 1:i + 2], in1=mid,
                                                                 op0=ALU.subtract, op1=ALU.add), reads=[SB], writes=[SB])
                dve.op(lambda e: e.tensor_tensor(out=tau, in0=mid, in1=wtab[:, NIT:NIT + 1], op=ALU.subtract),
                       reads=[SB], writes=[SB])
                dve.op(lambda e: e.tensor_scalar(out=self.mb[:, 0:S], in0=scv, scalar1=tau, scalar2=NEG_MASK,
                                                 op0=ALU.is_lt, op1=ALU.mult), reads=[self.score_b, SB], writes=[self.mb_b])
                nch = S // 128
                for c in range(nch):
                    for g in range(2):
                        lbk = 2 + self.au % 2
                        ps_ = self.au % 2
                        self.au += 1
                        pe.op(lambda e, lbk=lbk, c=c, g=g: e.matmul(
                            self.bank(lbk), lhsT=self.KT[:, g, c * 128:(c + 1) * 128],
                            rhs=self.aqT[:, lb, g * 4:(g + 1) * 4, :].rearrange("p h t -> p (h t)"), start=True, stop=False),
                            reads=[self.KT_b, self.aqT_b], writes=[self.pbuf[lbk]])
                        pe.op(lambda e, lbk=lbk, c=c: e.matmul(
                            self.bank(lbk), lhsT=self.mb[:, c * 128:(c + 1) * 128], rhs=self.ident4, start=False, stop=True),
                            reads=[self.mb_b, self.cst_b], writes=[self.pbuf[lbk]])
                        act.op(lambda e, lbk=lbk, ps_=ps_: e.activation(out=self.pT[ps_], in_=self.bank(lbk), func=AF.Exp,
                                                                        scale=scale),
                               reads=[self.pbuf[lbk]], writes=[self.pT_b[ps_]])
                        pe.op(lambda e, c=c, g=g, ps_=ps_: e.matmul(
                            self.bank(4 + g), lhsT=self.V[:, c, g, 0:128], rhs=self.pT[ps_], start=(c == 0), stop=(c == nch - 1)),
                            reads=[self.V_b, self.pT_b[ps_]], writes=[self.pbuf[4 + g]])
                        pe.op(lambda e, c=c, g=g, ps_=ps_: e.matmul(
                            self.bank(6 + g), lhsT=self.ones, rhs=self.pT[ps_], start=(c == 0), stop=(c == nch - 1)),
                            reads=[self.cst_b, self.pT_b[ps_]], writes=[self.pbuf[6 + g]])
                for g in range(2):
                    dve.op(lambda e, g=g: e.reciprocal(out=self.rcp, in_=self.bank(6 + g)), reads=[self.pbuf[6 + g]],
                           writes=[self.rcp_b])
                    dst = self.mixT[:, 8 + g * 4:8 + (g + 1) * 4, lb * 128:(lb + 1) * 128]
                    dve.op(lambda e, g=g, dst=dst: e.tensor_tensor(
                        out=dst, in0=self.bank(4 + g).rearrange("p (h t) -> p h t", h=4, t=128),
                        in1=self.rcp.rearrange("p (h t) -> p h t", h=4, t=128), op=ALU.mult),
                        reads=[self.pbuf[4 + g], self.rcp_b], writes=[self.hT_b])

            for k in range(4):
                block(k)

    def phaseD(self):
        pe, dve, pool = self.pe, self.dve, self.pool
        self.barrier()
        self.load_x(self.xspill)
        for cg in range(4):
            s = cg % 2
            pool.dma(self.dw[s], self.win[s].rearrange("p a b -> p (a b)"), self.wout_d[cg],
                     writes=[self.wg_b[s], self.wu_b[s]])
            for lb in range(NB):
                bk = (cg * NB + lb) % 4
                for kc in range(16):
                    pe.op(lambda e, bk=bk, kc=kc, lb=lb, s=s: e.matmul(
                        self.bank(bk), lhsT=self.mixT[:, kc, lb * 128:(lb + 1) * 128], rhs=self.win[s][:, kc, :],
                        start=(kc == 0), stop=(kc == 15)),
                        reads=[self.hT_b, self.wg_b[s], self.wu_b[s]], writes=[self.pbuf[bk]])
                xsl = self.xs[:, lb, cg * 512:(cg + 1) * 512]
                dve.op(lambda e, bk=bk, xsl=xsl: e.tensor_tensor(out=xsl, in0=self.bank(bk), in1=xsl, op=ALU.add),
                       reads=[self.pbuf[bk], self.xs_b[lb]], writes=[self.xs_b[lb]])
        self.barrier()

    def build(self):
        stop = self.stop
        self.phase0()
        self.barrier()
        self.norm_T(0)
        self.ffn(0)

        def finish_reload():
            self.barrier()
            self.load_x(self.xspill)
            self.final_norm()
            self.barrier()
        if stop == "ffn1":
            self.final_norm()
            self.barrier()
            return
        self.phaseB()
        if stop == "B":
            return finish_reload()
        self.allgather()
        if stop == "AG":
            return finish_reload()
        self.phaseC1()
        self.barrier()
        if stop == "C1":
            if self.debug:
                self.barrier()
                self.sp.dma(self.d_st, self.dbg_mix, self.mixT.rearrange("p a b -> p (a b)"), reads=[self.hT_b])
            return finish_reload()
        self.phaseC2()
        if self.debug:
            self.barrier()
            self.sp.dma(self.d_st, self.dbg_mix, self.mixT.rearrange("p a b -> p (a b)"), reads=[self.hT_b])
        if stop == "C2":
            return finish_reload()
        self.phaseD()
        if self.debug:
            for lb in range(NB):
                self.sp.dma(self.d_st, self.dbg_x[lb], self.xs[:, lb, :], reads=[self.xs_b[lb]])
        if stop != "D":
            self.norm_T(2)
            self.ffn(1)
        self.final_norm()
        self.barrier()


_CACHE = {}


def _consts():
    f = np.zeros((128, NF), np.float32)
    f[:, C_INV128:C_INV128 + 64] = (np.float32(10000.0) ** (-(np.arange(0, 128, 2, dtype=np.float32) / np.float32(128)))).astype(np.float32)[None]
    f[:, C_INV64:C_INV64 + 32] = (np.float32(10000.0) ** (-(np.arange(0, 64, 2, dtype=np.float32) / np.float32(64)))).astype(np.float32)[None]
    h = np.arange(8, dtype=np.float64)
    lg = np.log1p(-np.exp2(-5.0 - h))
    p1 = np.arange(1, 129, dtype=np.float64)
    f[:, C_GQ:C_GQ + 8] = np.exp(p1[:, None] * lg[None, :])
    f[:, C_GK:C_GK + 8] = np.exp(-p1[:, None] * lg[None, :]) * 128 ** -0.5
    f[:, C_GQT:C_GQT + 1024] = np.exp(lg[:, None] * p1[None, :]).reshape(1, 1024)
    f[:, C_GKT:C_GKT + 1024] = (np.exp(-lg[:, None] * p1[None, :]) * 128 ** -0.5).reshape(1, 1024)
    f[:, C_GC:C_GC + 1024] = np.repeat(np.exp(128 * lg), 128)[None]
    f[:, C_POW2:C_POW2 + 24] = (2.0 ** -np.arange(24))[None]
    f[:, C_EPS] = 1e-6
    cb = np.zeros((128, NCB), np.float32)
    eye = np.eye(128, dtype=np.float32)
    cb[:, 0:512] = np.tile(eye, (1, 4))
    tri = (np.arange(128)[None, :] >= np.arange(128)[:, None]).astype(np.float32)
    cb[:, 512:1024] = np.tile(tri, (1, 4))
    cb[:, 1024:1152] = 1.0
    return f, cb.astype(ml_dtypes.bfloat16)


def _percore_consts(j):
    c = np.zeros((128, NCC), np.float32)
    qrel = 128 * j + np.arange(128)
    c[:, 0:1024] = np.where(np.arange(1024)[None, :] > qrel[:, None], np.float32(-1e30), np.float32(0.0))
    c[:, 1024 + j] = 1.0
    return c


def _win_layout(w_in):
    O = dict(rq=0, rk=1024, rv=2048, rg=3072, aq=4096, ak=5120, av=5376, iq=5632, ik=6656, iw=6720)
    cols = []

    def rng(a, n):
        return list(range(a, a + n))
    cols += rng(O["rv"], 1024) + rng(O["rk"], 1024) + rng(O["rq"], 1024) + rng(O["rg"], 1024) + rng(O["aq"], 1024)
    cols += rng(O["ak"], 256) + rng(O["av"], 256)
    cols += rng(O["ik"], 64) + rng(O["ik"], 64) + rng(O["iw"], 16) + [-1] * 368
    cols += rng(O["iq"], 1024)
    cols = np.array(cols)
    W = np.zeros((D, 14 * 512), np.float32)
    m = cols >= 0
    W[:, m] = w_in[:, cols[m]]
    return np.ascontiguousarray(W.reshape(16, 128, 14, 512).transpose(2, 1, 0, 3)).reshape(14, 128, 16 * 512)


def _gu_layout(W):
    return np.ascontiguousarray(W.reshape(16, 128, NFG, 256).transpose(2, 1, 0, 3)).reshape(NFG, 128, 16 * 256)


def _wd_layout(W):
    return np.ascontiguousarray(W.reshape(NFG, 2, 128, D).transpose(0, 2, 1, 3)).reshape(NFG, 128, 2 * D)


def _wout_layout(W):
    return np.ascontiguousarray(W.reshape(16, 128, 4, 512).transpose(2, 1, 0, 3)).reshape(4, 128, 16 * 512)


def _chunks(j):
    return [(b, 8 * k + j) for b in range(2) for k in range(4)]


def kernel(x, positions, ffn1_norm, ffn1_w_gate, ffn1_w_up, ffn1_w_down, mix_norm, w_in, ret_norm, w_out,
           ffn2_norm, ffn2_w_gate, ffn2_w_up, ffn2_w_down, final_norm, _stop="full", _debug=False):
    x = np.asarray(x, np.float32)
    positions = np.asarray(positions, np.int32)
    key = (_stop, _debug)
    if key not in _CACHE:
        _CACHE[key] = Prog(_stop, _debug)
    prog = _CACHE[key]
    cf, cbb = _consts()
    norms = np.zeros((5, D), np.float32)
    norms[0] = np.asarray(ffn1_norm)[0]
    norms[1] = np.asarray(mix_norm)[0]
    norms[2] = np.asarray(ffn2_norm)[0]
    norms[3] = np.asarray(final_norm)
    norms[4, :1024] = np.asarray(ret_norm)[0]
    shared = dict(
        norms=norms,
        wg1=_gu_layout(np.asarray(ffn1_w_gate, np.float32)[0]), wu1=_gu_layout(np.asarray(ffn1_w_up, np.float32)[0]),
        wd1=_wd_layout(np.asarray(ffn1_w_down, np.float32)[0]),
        wg2=_gu_layout(np.asarray(ffn2_w_gate, np.float32)[0]), wu2=_gu_layout(np.asarray(ffn2_w_up, np.float32)[0]),
        wd2=_wd_layout(np.asarray(ffn2_w_down, np.float32)[0]),
        win=_win_layout(np.asarray(w_in, np.float32)[0]), wout=_wout_layout(np.asarray(w_out, np.float32)[0]),
        cstf=cf, cstb=cbb,
    )
    in_maps = []
    for j in range(8):
        ch = _chunks(j)
        xm = np.stack([x[b, n * 128:(n + 1) * 128] for (b, n) in ch])
        pm = np.stack([positions[b, n * 128:(n + 1) * 128] for (b, n) in ch], axis=1)
        m = dict(shared)
        m["x"] = np.ascontiguousarray(xm)
        m["pos"] = np.ascontiguousarray(pm.astype(np.int32))
        m["cstc"] = _percore_consts(j)
        in_maps.append(m)
    res = run_bass_kernel_spmd(prog.nc, in_maps, core_ids=list(range(8)))
    if _debug:
        kernel.dbg = [(res.results[j]["dbg_mix"], res.results[j]["dbg_x"]) for j in range(8)]
    out = np.zeros((2, 4096, D), np.float32)
    for j in range(8):
        o = res.results[j]["out"]
        for lb, (b, n) in enumerate(_chunks(j)):
            out[b, n * 128:(n + 1) * 128] = o[lb]
    return out
```

```python
import numpy as np
import ml_dtypes
from contextlib import ExitStack
import concourse.bass as bass
import concourse.mybir as mybir
from concourse.bass_utils import run_bass_kernel_spmd

F32 = mybir.dt.float32
BF16 = mybir.dt.bfloat16
I32 = mybir.dt.int32
ALU = mybir.AluOpType
AF = mybir.ActivationFunctionType
AX = mybir.AxisListType

D = 2048
DFF = 5632
NFG = 22
NB = 8
NIT = 20
NEG_MASK = -30000.0
PI = float(np.pi)

C_INV128, C_INV64, C_GQ, C_GK, C_GQT, C_GKT, C_GC, C_POW2, C_EPS, NF = 0, 64, 96, 104, 112, 1136, 2160, 3184, 3208, 3216
NCC = 1032
NCB = 1152


class Tok:
    __slots__ = ("sem", "v", "q")

    def __init__(self, sem, v, q):
        self.sem = sem
        self.v = v
        self.q = q


class Buf:
    __slots__ = ("name", "w", "r")

    def __init__(self, name=""):
        self.name = name
        self.w = None
        self.r = {}


class DSem:
    def __init__(self, nc, es, name):
        self.sem = es.enter_context(nc.semaphore(name))
        self.n = 0


class Q:
    def __init__(self, nc, es, name, is_pe=False):
        self.name = name
        self.sem = es.enter_context(nc.semaphore("q_" + name))
        self.n = 0
        self.ops = []
        self.waited = {}
        self.is_pe = is_pe

    def wait(self, toks):
        for t in toks:
            if t is None:
                continue
            k = id(t.sem)
            if self.waited.get(k, 0) >= t.v:
                continue
            self.waited[k] = t.v
            self.ops.append(lambda e, t=t: e.wait_ge(t.sem, t.v))

    def _deps(self, reads, writes, dma):
        deps = []
        for b in reads:
            t = b.w
            if t is not None and (dma or not (t.q is self and self.is_pe)):
                deps.append(t)
        for b in writes:
            for t in list(b.r.values()) + [b.w]:
                if t is None:
                    continue
                if (not dma) and t.q is self:
                    continue
                deps.append(t)
        return deps

    @staticmethod
    def _mark(tok, reads, writes):
        k = id(tok.sem)
        for b in reads:
            o = b.r.get(k)
            if o is None or o.v < tok.v:
                b.r[k] = tok
        for b in writes:
            b.w = tok
            b.r = {}

    def op(self, fn, reads=(), writes=(), extra=()):
        self.wait(self._deps(reads, writes, False) + list(extra))
        self.n += 1
        n = self.n
        sem = self.sem
        self.ops.append(lambda e: fn(e).then_inc(sem, 1))
        tok = Tok(sem, n, self)
        self._mark(tok, reads, writes)
        return tok

    def dma(self, dsem, out, in_, reads=(), writes=(), extra=()):
        self.wait(self._deps(reads, writes, True) + list(extra))
        dsem.n += 16
        s = dsem.sem
        self.ops.append(lambda e: e.dma_start(out=out, in_=in_).then_inc(s, 16))
        tok = Tok(s, dsem.n, dsem)
        self._mark(tok, reads, writes)
        return tok

    def dma_group(self, dsem, pairs, reads=(), writes=(), extra=()):
        self.wait(self._deps(reads, writes, True) + list(extra))
        s = dsem.sem
        for (out, in_) in pairs:
            dsem.n += 16
            self.ops.append(lambda e, out=out, in_=in_: e.dma_start(out=out, in_=in_).then_inc(s, 16))
        tok = Tok(s, dsem.n, dsem)
        self._mark(tok, reads, writes)
        return tok

    def replay(self, e):
        for o in self.ops:
            o(e)


def _prod(s):
    r = 1
    for v in s:
        r *= int(v)
    return r


ARENA = 210944


class Prog:
    def __init__(self, stop="full", debug=False):
        self.stop = stop
        self.debug = debug
        nc = self.nc = bass.Bass("TRN2", target_bir_lowering=False, num_devices=8)
        es = self.es = ExitStack()
        dt = nc.dram_tensor

        def din(name, shape, d=F32):
            return dt(name, shape, d, kind="ExternalInput").ap()
        self.x_d = din("x", [NB, 128, D])
        self.pos_d = din("pos", [128, NB], I32)
        self.norms_d = din("norms", [5, D])
        self.wg_d = [din("wg1", [NFG, 128, 16 * 256]), din("wg2", [NFG, 128, 16 * 256])]
        self.wu_d = [din("wu1", [NFG, 128, 16 * 256]), din("wu2", [NFG, 128, 16 * 256])]
        self.wd_d = [din("wd1", [NFG, 128, 2 * D]), din("wd2", [NFG, 128, 2 * D])]
        self.win_d = din("win", [14, 128, 16 * 512])
        self.wout_d = din("wout", [4, 128, 16 * 512])
        self.cstf_d = din("cstf", [128, NF])
        self.cstc_d = din("cstc", [128, NCC])
        self.cstb_d = din("cstb", [128, NCB], BF16)
        self.out_d = dt("out", [NB, 128, D], F32, kind="ExternalOutput").ap()
        if debug:
            self.dbg_mix = dt("dbg_mix", [128, 16384], BF16, kind="ExternalOutput").ap()
            self.dbg_x = dt("dbg_x", [NB, 128, D], F32, kind="ExternalOutput").ap()
        self.xspill = dt("xspill", [NB, 128, D], F32, kind="Internal").ap()
        self.kv_in = dt("kv_in", [NB * 8 * 128, 128], F32, kind="Internal").ap()
        self.kv_all = dt("kv_all", [8 * NB * 8 * 128, 128], F32, kind="Internal", addr_space="Shared").ap()
        self.kb_in = dt("kb_in", [128, 5120], BF16, kind="Internal").ap()
        self.kb_all = dt("kb_all", [1024, 5120], BF16, kind="Internal", addr_space="Shared").ap()

        self.arena = es.enter_context(nc.sbuf_tensor("arena", [128, ARENA // 4], F32))
        self.arena_b = self.arena.bitcast(BF16)
        self.arena_i = self.arena.bitcast(I32)
        self.ps = es.enter_context(nc.psum_tensor("ps", [128, 4096], F32))
        self.psb = self.ps.bitcast(BF16)
        self.pbuf = [Buf("pb%d" % i) for i in range(8)]

        self.pe = Q(nc, es, "pe", True)
        self.act = Q(nc, es, "act")
        self.dve = Q(nc, es, "dve")
        self.pool = Q(nc, es, "pool")
        self.sp = Q(nc, es, "sp")
        self.queues = [self.pe, self.act, self.dve, self.pool, self.sp]
        self.dsems = []
        self.cc_sem = es.enter_context(nc.semaphore("cc"))
        self.cc_n = 0

        self.layout()
        self.build()

        blk = es.enter_context(nc.Block())
        blk.sync(self.sp.replay)
        blk.tensor(self.pe.replay)
        blk.scalar(self.act.replay)
        blk.vector(self.dve.replay)
        blk.gpsimd(self.pool.replay)
        es.close()

    def dsem(self, name):
        d = DSem(self.nc, self.es, name)
        self.dsems.append(d)
        return d

    def carve(self, off, shape, dtype):
        n = _prod(shape[1:])
        if dtype == BF16:
            assert off % 2 == 0
            ap = self.arena_b[:, off // 2: off // 2 + n]
        elif dtype == I32:
            ap = self.arena_i[:, off // 4: off // 4 + n]
        else:
            assert off % 4 == 0
            ap = self.arena[:, off // 4: off // 4 + n]
        if len(shape) == 3:
            ap = ap.rearrange("p (a b) -> p a b", a=shape[1], b=shape[2])
        elif len(shape) == 4:
            ap = ap.rearrange("p (a b c) -> p a b c", a=shape[1], b=shape[2], c=shape[3])
        elif len(shape) == 5:
            ap = ap.rearrange("p (a b c d) -> p a b c d", a=shape[1], b=shape[2], c=shape[3], d=shape[4])
        return ap

    def bank(self, i, n=512, off=0):
        return self.ps[:, i * 512 + off: i * 512 + off + n]

    def bankb(self, i, n=1024, off=0):
        return self.psb[:, i * 1024 + off: i * 1024 + off + n]

    def barrier(self):
        toks = [Tok(q.sem, q.n, q) for q in self.queues if q.n > 0]
        toks += [Tok(d.sem, d.n, d) for d in self.dsems if d.n > 0]
        if self.cc_n:
            toks.append(Tok(self.cc_sem, self.cc_n, None))
        for q in self.queues:
            q.wait(toks)

    def layout(self):
        c = self.carve
        R_X, R_HT, R_W, R_WD, R_GBC, R_T, R_CST = 0, 65536, 98304, 131072, 147456, 155648, 176128
        self.xs = c(R_X, [128, NB, D], F32)
        self.xs_b = [Buf("xs%d" % i) for i in range(NB)]
        self.hT = c(R_HT, [128, 16, 1024], BF16)
        self.hT_b = Buf("hT")
        self.wgu = [c(R_W + s * 16384, [128, 2, 16, 256], BF16) for s in range(2)]
        self.win = [c(R_W + s * 16384, [128, 16, 512], BF16) for s in range(2)]
        self.wg_b = [Buf("wg%d" % s) for s in range(2)]
        self.wu_b = [Buf("wu%d" % s) for s in range(2)]
        self.wdr = [c(R_WD + s * 8192, [128, 2, D], BF16) for s in range(2)]
        self.wd_b = [Buf("wd%d" % s) for s in range(2)]
        self.dw = [self.dsem("dw%d" % s) for s in range(2)]
        self.dwu = [self.dsem("dwu%d" % s) for s in range(2)]
        self.dwd = [self.dsem("dwd%d" % s) for s in range(2)]
        self.gbc = c(R_GBC, [128, D], F32)
        self.gbc_b = Buf("gbc")
        self.aT = [c(R_T + g * 4096, [128, 2, 1024], BF16) for g in range(2)]
        self.aT_b = [Buf("aT%d" % g) for g in range(2)]
        self.sg = [c(R_T + 8192 + s * 2048, [128, 512], F32) for s in range(2)]
        self.sg_b = [Buf("sg%d" % s) for s in range(2)]
        self.hb = [c(R_T + 12288 + s * 4096, [128, D], BF16) for s in range(2)]
        self.hb_b = [Buf("hb%d" % s) for s in range(2)]
        o = R_CST
        self.cstf = c(o, [128, NF], F32); o += NF * 4
        self.cstc = c(o, [128, NCC], F32); o += NCC * 4
        self.cstb = c(o, [128, NCB], BF16); o += NCB * 2
        self.cs128 = c(o, [128, NB, 64], F32); o += 2048
        self.sn128 = c(o, [128, NB, 64], F32); o += 2048
        self.cs64 = c(o, [128, NB, 32], F32); o += 1024
        self.sn64 = c(o, [128, NB, 32], F32); o += 1024
        self.wabs = c(o, [128, NB, 16], F32); o += 512
        self.wsgn = c(o, [128, NB, 16], F32); o += 512
        self.stat = c(o, [128, 256], F32); o += 1024
        self.posi = c(o, [128, NB], I32); o += 32
        self.stg = [c(o + s * 2048, [128, 512], F32) for s in range(2)]
        self.stgb = [c(o + s * 2048, [128, 1024], BF16) for s in range(2)]
        o += 4096
        assert o <= ARENA, o
        self.cst_b = Buf("cst")
        self.rope_b = Buf("ropetab")
        self.wabs_b = Buf("wabs")
        self.stat_b = Buf("stat")
        self.stg_b = [Buf("stg%d" % s) for s in range(2)]
        self.ident4 = self.cstb[:, 0:512]
        self.ident = self.cstb[:, 0:128]
        self.tri4 = self.cstb[:, 512:1024]
        self.ones = self.cstb[:, 1024:1152]
        self.cb = self.cstc[:, 0:1024]
        self.selc = self.cstc[:, 1024:1032]
        self.qT = c(R_X, [128, NB, 8, 128], BF16)
        self.kT = c(R_X + 16384, [128, NB, 8, 128], BF16)
        self.vr = c(R_X + 32768, [128, NB, 1024], BF16)
        self.sgr = c(R_X + 49152, [128, NB, 1024], BF16)
        self.aqT = c(R_WD, [128, NB, 8, 128], BF16)
        BT = R_GBC
        self.ropeA = c(BT, [128, 512], F32)
        self.ropeB = c(BT + 2048, [128, 512], F32)
        self.bout = [c(BT + 4096 + s * 2048, [128, 512], F32) for s in range(2)]
        self.boutb = [c(BT + 4096 + s * 2048, [128, 512], BF16) for s in range(2)]
        self.aux = [c(BT + 8192 + s * 1024, [128, 512], BF16) for s in range(2)]
        self.iqT = c(BT + 12288, [128, NB, 8, 128], BF16)
        self.qT_b, self.kT_b, self.vr_b, self.sgr_b = Buf("qT"), Buf("kT"), Buf("vr"), Buf("sgr")
        self.aqT_b, self.iqT_b = Buf("aqT"), Buf("iqT")
        self.ropeA_b, self.ropeB_b = Buf("ropeA"), Buf("ropeB")
        self.bout_b = [Buf("bout0"), Buf("bout1")]
        self.aux_b = [Buf("aux0"), Buf("aux1")]
        self.t0a = c(BT, [128, NB, 64], F32)
        self.t0b = c(BT + 2048, [128, NB, 64], F32)
        self.t0i = c(BT + 4096, [128, NB, 64], I32)
        self.t0c = c(BT + 6144, [128, NB, 64], F32)
        self.snap = c(R_W, [128, 2, 4, 1024], BF16)
        self.S = c(R_W + 16384, [128, 1024], F32)
        self.kvbuf = [c(R_W + 20480 + s * 4096, [128, 8, 128], F32) for s in range(2)]
        self.gnr = c(R_W + 28672, [128, 1024], F32)
        self.snap_b, self.S_b, self.gnr_b = Buf("snap"), Buf("S"), Buf("gnr")
        self.kvbuf_b = [Buf("kvb0"), Buf("kvb1")]
        self.dkv = [self.dsem("dkv%d" % s) for s in range(2)]
        self.t1 = c(BT, [128, 1024], F32)
        self.t2 = c(BT + 4096, [128, 1024], F32)
        self.ybf = c(BT + 8192, [128, 1024], BF16)
        self.sTm = [c(BT + 10240 + s * 1024, [128, 512], BF16) for s in range(2)]
        self.t1_b, self.t2_b, self.ybf_b = Buf("t1"), Buf("t2"), Buf("ybf")
        self.sTm_b = [Buf("sTm0"), Buf("sTm1")]
        self.KT = c(R_X, [128, 2, 4096], BF16)
        self.V = c(R_X + 16384, [128, 32, 2, 128], BF16)
        self.ikT = c(R_X + 32768, [128, 4096], BF16)
        self.rb = [c(R_X + 40960 + s * 2048, [128, 512], F32) for s in range(4)]
        self.pT = [c(R_X + 49152 + s * 1024, [128, 512], BF16) for s in range(2)]
        self.rcp = c(R_X + 51200, [128, 512], F32)
        self.KT_b, self.V_b, self.ikT_b = Buf("KT"), Buf("V"), Buf("ikT")
        self.rb_b = [Buf("rb%d" % s) for s in range(4)]
        self.pT_b = [Buf("pT0"), Buf("pT1")]
        self.rcp_b = Buf("rcp")
        self.score = c(R_W, [128, 4096], F32)
        self.mb = [c(R_W + 16384 + i * 8192, [128, 4096], BF16) for i in range(2)]
        self.junk = c(R_X + 53248, [128, 4096], BF16)
        self.score_b, self.mb_b, self.junk_b = Buf("score"), [Buf("mb0"), Buf("mb1")], Buf("junk")
        self.mixT = self.hT
        self.xspill_b = [Buf("xsp%d" % i) for i in range(NB)]
        self.kv_in_b, self.kb_in_b = Buf("kv_in"), Buf("kb_in")
        self.kv_all_b, self.kb_all_b = Buf("kv_all"), Buf("kb_all")
        self.d_ld = self.dsem("d_ld")
        self.d_x = [self.dsem("d_x%d" % i) for i in range(NB)]
        self.d_st = self.dsem("d_st")
        self.d_g = self.dsem("d_g")
        self.d_kt, self.d_v, self.d_ik = self.dsem("d_kt"), self.dsem("d_v"), self.dsem("d_ik")
        self.d_stg = [self.dsem("d_stg%d" % i) for i in range(2)]

    def load_x(self, src):
        for lb in range(NB):
            self.sp.dma(self.d_x[lb], self.xs[:, lb, :], src[lb], writes=[self.xs_b[lb]])

    def sincos(self, ang_fn, shape, out_sin, shift, n):
        dve, act = self.dve, self.act
        tb = self.t0b[:, :, 0:n]
        ti = self.t0i[:, :, 0:n]
        tc_ = self.t0c[:, :, 0:n]
        B1, B2, B3 = self.ropeB_b, self.bout_b[0], self.bout_b[1]
        ang = ang_fn
        dve.op(lambda e: e.tensor_scalar(out=tc_, in0=ang, scalar1=float(shift), scalar2=None, op0=ALU.add), reads=[self.ropeA_b], writes=[B3])
        dve.op(lambda e: e.tensor_scalar(out=tb, in0=tc_, scalar1=float(1.0 / (2 * PI)), scalar2=None, op0=ALU.mult), reads=[B3], writes=[B1])
        dve.op(lambda e: e.tensor_copy(out=ti, in_=tb), reads=[B1], writes=[B2])
        dve.op(lambda e: e.tensor_copy(out=tb, in_=ti), reads=[B2], writes=[B1])
        dve.op(lambda e: e.scalar_tensor_tensor(out=tc_, in0=tb, scalar=float(-2 * PI), in1=tc_, op0=ALU.mult, op1=ALU.add), reads=[B1, B3], writes=[B3])
        dve.op(lambda e: e.tensor_scalar(out=tb, in0=tc_, scalar1=PI, scalar2=float(-2 * PI), op0=ALU.is_gt, op1=ALU.mult), reads=[B3], writes=[B1])
        dve.op(lambda e: e.tensor_tensor(out=tc_, in0=tc_, in1=tb, op=ALU.add), reads=[B1, B3], writes=[B3])
        dve.op(lambda e: e.tensor_scalar(out=tb, in0=tc_, scalar1=-PI, scalar2=float(2 * PI), op0=ALU.is_lt, op1=ALU.mult), reads=[B3], writes=[B1])
        dve.op(lambda e: e.tensor_tensor(out=tc_, in0=tc_, in1=tb, op=ALU.add), reads=[B1, B3], writes=[B3])
        act.op(lambda e: e.activation(out=out_sin, in_=tc_, func=AF.Sin), reads=[B3], writes=[self.rope_b])

    def phase0(self):
        sp, dve = self.sp, self.dve
        sp.dma(self.d_ld, self.cstf, self.cstf_d, writes=[self.cst_b])
        sp.dma(self.d_ld, self.cstc, self.cstc_d, writes=[self.cst_b])
        sp.dma(self.d_ld, self.cstb, self.cstb_d, writes=[self.cst_b])
        sp.dma(self.d_ld, self.posi, self.pos_d, writes=[self.cst_b])
        self.load_x(self.x_d)
        posf = self.stat[:, 0:NB]
        dve.op(lambda e: e.tensor_copy(out=posf, in_=self.posi), reads=[self.cst_b], writes=[self.stat_b])
        for n, inv_off, cs, sn in ((64, C_INV128, self.cs128, self.sn128), (32, C_INV64, self.cs64, self.sn64)):
            ang = self.t0a[:, :, 0:n]
            inv = self.cstf[:, inv_off:inv_off + n]
            dve.op(lambda e, ang=ang, inv=inv, n=n: e.tensor_tensor(
                out=ang, in0=inv.unsqueeze(1).to_broadcast([128, NB, n]),
                in1=posf.unsqueeze(2).to_broadcast([128, NB, n]), op=ALU.mult),
                reads=[self.cst_b, self.stat_b], writes=[self.ropeA_b])
            self.sincos(ang, None, sn, 0.0, n)
            self.sincos(ang, None, cs, PI / 2, n)

    def norm_stats(self, row):
        sp, act, dve = self.sp, self.act, self.dve
        sp.dma(self.d_g, self.gbc, self.norms_d[row:row + 1, :].partition_broadcast(128), writes=[self.gbc_b])
        ss = self.stat[:, 16:16 + NB]
        for lb in range(NB):
            act.op(lambda e, lb=lb: e.activation(out=self.hb[1], in_=self.xs[:, lb, :], func=AF.Square,
                                                 accum_out=ss[:, lb:lb + 1]),
                   reads=[self.xs_b[lb]], writes=[self.hb_b[1], self.stat_b])
        rstd = self.stat[:, 32:32 + NB]
        act.op(lambda e: e.activation(out=rstd, in_=ss, func=AF.Sqrt, scale=1.0 / D,
                                      bias=self.cstf[:, C_EPS:C_EPS + 1]),
               reads=[self.stat_b, self.cst_b], writes=[self.stat_b])
        dve.op(lambda e: e.reciprocal(out=rstd, in_=rstd), reads=[self.stat_b], writes=[self.stat_b])
        return rstd

    def norm_T(self, row):
        pe, act, dve = self.pe, self.act, self.dve
        rstd = self.norm_stats(row)
        for lb in range(NB):
            s = lb % 2
            hb = self.hb[s]
            dve.op(lambda e, lb=lb, hb=hb: e.scalar_tensor_tensor(
                out=hb, in0=self.xs[:, lb, :], scalar=rstd[:, lb:lb + 1], in1=self.gbc,
                op0=ALU.mult, op1=ALU.mult),
                reads=[self.xs_b[lb], self.stat_b, self.gbc_b], writes=[self.hb_b[s]])
            for half in range(2):
                bk = 4 + 2 * s + half
                for i in range(8):
                    kc = half * 8 + i
                    pe.op(lambda e, bk=bk, i=i, kc=kc, hb=hb: e.transpose(
                        out=self.bankb(bk, 128, i * 128), in_=hb[:, kc * 128:(kc + 1) * 128], identity=self.ident),
                        reads=[self.hb_b[s], self.cst_b], writes=[self.pbuf[bk]])
                dst = self.hT[:, half * 8:(half + 1) * 8, lb * 128:(lb + 1) * 128]
                src = self.bankb(bk).rearrange("p (a b) -> p a b", a=8, b=128)
                eng = act if half == 0 else dve
                if eng is act:
                    eng.op(lambda e, dst=dst, src=src: e.activation(out=dst, in_=src, func=AF.Copy),
                           reads=[self.pbuf[bk]], writes=[self.hT_b])
                else:
                    eng.op(lambda e, dst=dst, src=src: e.tensor_copy(out=dst, in_=src),
                           reads=[self.pbuf[bk]], writes=[self.hT_b])

    def final_norm(self):
        dve, sp = self.dve, self.sp
        rstd = self.norm_stats(3)
        last = None
        for lb in range(NB):
            s = lb % 2
            o = self.sg[s]
            dve.op(lambda e, lb=lb: e.scalar_tensor_tensor(
                out=self.xs[:, lb, :], in0=self.xs[:, lb, :], scalar=rstd[:, lb:lb + 1], in1=self.gbc,
                op0=ALU.mult, op1=ALU.mult),
                reads=[self.xs_b[lb], self.stat_b, self.gbc_b], writes=[self.xs_b[lb]])
            last = sp.dma(self.d_st, self.out_d[lb], self.xs[:, lb, :], reads=[self.xs_b[lb]])
        return last

    def ffn(self, idx):
        pe, act, dve, pool = self.pe, self.act, self.dve, self.pool
        wg_d, wu_d, wd_d = self.wg_d[idx], self.wu_d[idx], self.wd_d[idx]

        def load(fg):
            s = fg % 2
            pool.dma(self.dw[s], self.wgu[s][:, 0].rearrange("p a b -> p (a b)"), wg_d[fg], writes=[self.wg_b[s]])
            pool.dma(self.dwu[s], self.wgu[s][:, 1].rearrange("p a b -> p (a b)"), wu_d[fg], writes=[self.wu_b[s]])
            pool.dma(self.dwd[s], self.wdr[s].rearrange("p a b -> p (a b)"), wd_d[fg], writes=[self.wd_b[s]])

        def gate_up(fg):
            s = fg % 2
            for f in range(2):
                for half in range(2):
                    u = f * 2 + half
                    pg, pu = (u % 2) * 2, (u % 2) * 2 + 1
                    rhs_sl = slice(half * 512, (half + 1) * 512)
                    for (bk, wi, wb) in ((pg, 0, self.wg_b[s]), (pu, 1, self.wu_b[s])):
                        for kc in range(16):
                            pe.op(lambda e, bk=bk, wi=wi, kc=kc, f=f, s=s, rhs_sl=rhs_sl: e.matmul(
                                self.bank(bk), lhsT=self.wgu[s][:, wi, kc, f * 128:(f + 1) * 128],
                                rhs=self.hT[:, kc, rhs_sl], start=(kc == 0), stop=(kc == 15)),
                                reads=[wb, self.hT_b], writes=[self.pbuf[bk]])
                    sgs = u % 2
                    act.op(lambda e, pg=pg, sgs=sgs: e.activation(out=self.sg[sgs], in_=self.bank(pg), func=AF.Silu),
                           reads=[self.pbuf[pg]], writes=[self.sg_b[sgs]])
                    dve.op(lambda e, pu=pu, sgs=sgs, fg=fg, f=f, rhs_sl=rhs_sl: e.tensor_tensor(
                        out=self.aT[fg % 2][:, f, rhs_sl], in0=self.sg[sgs], in1=self.bank(pu), op=ALU.mult),
                        reads=[self.sg_b[sgs], self.pbuf[pu]], writes=[self.aT_b[fg % 2]])

        def down(fg):
            s = fg % 2
            for tb in range(NB):
                for ch in range(2):
                    par = (tb * 2 + ch) % 2
                    for cbk in range(2):
                        bk = 4 + par * 2 + cbk
                        for f in range(2):
                            c0 = ch * 1024 + cbk * 512
                            pe.op(lambda e, bk=bk, f=f, tb=tb, c0=c0, fg=fg, s=s: e.matmul(
                                self.bank(bk), lhsT=self.aT[fg % 2][:, f, tb * 128:(tb + 1) * 128],
                                rhs=self.wdr[s][:, f, c0:c0 + 512], start=(f == 0), stop=(f == 1)),
                                reads=[self.aT_b[fg % 2], self.wd_b[s]], writes=[self.pbuf[bk]])
                    b0 = 4 + par * 2
                    xsl = self.xs[:, tb, ch * 1024:(ch + 1) * 1024]
                    dve.op(lambda e, b0=b0, xsl=xsl: e.scalar_tensor_tensor(
                        out=xsl, in0=self.bank(b0, 1024), scalar=0.5, in1=xsl, op0=ALU.mult, op1=ALU.add),
                        reads=[self.pbuf[b0], self.pbuf[b0 + 1], self.xs_b[tb]], writes=[self.xs_b[tb]])

        nfg = NFG
        load(0)
        load(1)
        for fg in range(nfg + 1):
            if fg < nfg:
                gate_up(fg)
            if fg >= 1:
                down(fg - 1)
                if fg + 1 < nfg:
                    load(fg + 1)

    def rope(self, bk, col0, H, dh, cos, sin, out, out_b, extra_reads=()):
        dve = self.dve
        hd = dh // 2
        n = H * dh
        v = self.bank(bk, n, col0).rearrange("p (h t d) -> p h t d", h=H, t=2, d=hd)
        A = self.ropeA[:, 0:n].rearrange("p (h t d) -> p h t d", h=H, t=2, d=hd)
        Bt = self.ropeB[:, 0:n].rearrange("p (h t d) -> p h t d", h=H, t=2, d=hd)
        cos_b4 = cos.unsqueeze(1).unsqueeze(1).to_broadcast([128, H, 2, hd])
        sin_b3 = sin.unsqueeze(1).to_broadcast([128, H, hd])
        rd = [self.pbuf[bk], self.rope_b] + list(extra_reads)
        dve.op(lambda e: e.tensor_tensor(out=A, in0=v, in1=cos_b4, op=ALU.mult), reads=rd, writes=[self.ropeA_b])
        dve.op(lambda e: e.scalar_tensor_tensor(out=Bt[:, :, 0, :], in0=v[:, :, 1, :], scalar=-1.0, in1=sin_b3,
                                                op0=ALU.mult, op1=ALU.mult), reads=rd, writes=[self.ropeB_b])
        self.rope_ps_tok = dve.op(lambda e: e.tensor_tensor(out=Bt[:, :, 1, :], in0=v[:, :, 0, :], in1=sin_b3, op=ALU.mult),
                                  reads=rd, writes=[self.ropeB_b])
        return dve.op(lambda e: e.tensor_tensor(out=out, in0=self.ropeA[:, 0:n], in1=self.ropeB[:, 0:n], op=ALU.add),
                      reads=[self.ropeA_b, self.ropeB_b], writes=[out_b])

    def phaseB(self):
        pe, act, dve, pool, sp = self.pe, self.act, self.dve, self.pool, self.sp
        self.norm_T(1)
        for lb in range(NB):
            sp.dma(self.d_st, self.xspill[lb], self.xs[:, lb, :], reads=[self.xs_b[lb]], writes=[self.xspill_b[lb]])
        self.barrier()

        def load(cg):
            s = cg % 2
            pool.dma(self.dw[s], self.win[s].rearrange("p a b -> p (a b)"), self.win_d[cg],
                     writes=[self.wg_b[s], self.wu_b[s]])

        fams = ["rv", "rv", "rk", "rk", "rq", "rq", "rg", "rg", "aq", "aq", "akav", "ikiw", "iq", "iq"]
        load(0)
        load(1)
        self.tpc = 0
        self.kvc = 0
        pending = []
        u = 0
        kvrows = self.kv_in.rearrange("(l h d) e -> d l h e", l=NB, h=8, d=128)
        for cg in range(14):
            s = cg % 2
            fam = fams[cg]
            hs = cg % 2 if fam != "akav" and fam != "ikiw" else 0
            if fam in ("rv", "rk", "rq", "rg", "aq", "iq"):
                hs = [c for c in range(14) if fams[c] == fam].index(cg)
            ncols = 144 if fam == "ikiw" else 512
            for lb in range(NB):
                bk = u % 4
                for kc in range(16):
                    pe.op(lambda e, bk=bk, kc=kc, lb=lb, s=s, ncols=ncols: e.matmul(
                        self.bank(bk, ncols), lhsT=self.hT[:, kc, lb * 128:(lb + 1) * 128],
                        rhs=self.win[s][:, kc, 0:ncols], start=(kc == 0), stop=(kc == 15)),
                        reads=[self.hT_b, self.wg_b[s], self.wu_b[s]], writes=[self.pbuf[bk]])
                for fn in pending:
                    fn()
                pending = []
                pending.append(self.postB(fam, hs, lb, bk, u, kvrows))
                u += 1
            if cg + 2 < 14:
                load(cg + 2)
        for fn in pending:
            fn()

    def postB(self, fam, hs, lb, bk, u, kvrows):
        pe, act, dve, sp = self.pe, self.act, self.dve, self.sp
        pb = self.pbuf[bk]
        so = u % 2
        h0 = hs * 4

        def transposes(src_ap, src_b, nblk):
            tb = 4 + (self.tpc % 2)
            self.tpc += 1
            for i in range(nblk):
                pe.op(lambda e, i=i, tb=tb: e.transpose(out=self.bankb(tb, 128, i * 128),
                                                        in_=src_ap[:, i * 128:(i + 1) * 128], identity=self.ident),
                      reads=[src_b, self.cst_b], writes=[self.pbuf[tb]])
            return tb

        if fam == "rv":
            act.op(lambda e: e.activation(out=self.vr[:, lb, hs * 512:(hs + 1) * 512], in_=self.bank(bk), func=AF.Copy),
                   reads=[pb], writes=[self.vr_b])
            return lambda: None
        if fam == "rg":
            act.op(lambda e: e.activation(out=self.sgr[:, lb, hs * 512:(hs + 1) * 512], in_=self.bank(bk), func=AF.Silu),
                   reads=[pb], writes=[self.sgr_b])
            return lambda: None
        if fam in ("rk", "rq", "aq"):
            ob = self.boutb[so]
            self.rope(bk, 0, 4, 128, self.cs128[:, lb, :], self.sn128[:, lb, :], ob, self.bout_b[so])
            if fam == "rk":
                gk_b = self.cstf[:, C_GK + h0:C_GK + h0 + 4].unsqueeze(2).to_broadcast([128, 4, 128])
                dve.op(lambda e: e.tensor_tensor(out=self.aux[so].rearrange("p (h d) -> p h d", h=4, d=128),
                                                 in0=ob.rearrange("p (h d) -> p h d", h=4, d=128), in1=gk_b, op=ALU.mult),
                       reads=[self.bout_b[so], self.cst_b], writes=[self.aux_b[so]])

            def deferred():
                tb = transposes(ob, self.bout_b[so], 4)
                if fam == "rk":
                    dst = self.kT[:, lb, h0:h0 + 4, :].rearrange("p h t -> p (h t)")
                    tab = self.cstf[:, C_GKT + h0 * 128:C_GKT + h0 * 128 + 512]
                    dve.op(lambda e: e.tensor_tensor(out=dst, in0=self.bankb(tb, 512), in1=tab, op=ALU.mult),
                           reads=[self.pbuf[tb], self.cst_b], writes=[self.kT_b])
                    kb = 6 + (self.kvc % 2)
                    self.kvc += 1
                    for h in range(4):
                        pe.op(lambda e, h=h, kb=kb: e.matmul(
                            self.bank(kb, 128, h * 128), lhsT=self.aux[so][:, h * 128:(h + 1) * 128],
                            rhs=self.vr[:, lb, (h0 + h) * 128:(h0 + h + 1) * 128], start=True, stop=True),
                            reads=[self.aux_b[so], self.vr_b], writes=[self.pbuf[kb]])
                    act.op(lambda e, kb=kb: e.activation(out=self.stg[so], in_=self.bank(kb), func=AF.Copy),
                           reads=[self.pbuf[kb]], writes=[self.stg_b[so]])
                    sp.dma(self.d_stg[so], kvrows[:, lb, h0:h0 + 4, :],
                           self.stg[so].rearrange("p (h e) -> p h e", h=4, e=128),
                           reads=[self.stg_b[so]])
                elif fam == "rq":
                    dst = self.qT[:, lb, h0:h0 + 4, :].rearrange("p h t -> p (h t)")
                    tab = self.cstf[:, C_GQT + h0 * 128:C_GQT + h0 * 128 + 512]
                    dve.op(lambda e: e.tensor_tensor(out=dst, in0=self.bankb(tb, 512), in1=tab, op=ALU.mult),
                           reads=[self.pbuf[tb], self.cst_b], writes=[self.qT_b])
                else:
                    dst = self.aqT[:, lb, h0:h0 + 4, :].rearrange("p h t -> p (h t)")
                    act.op(lambda e: e.activation(out=dst, in_=self.bankb(tb, 512), func=AF.Copy),
                           reads=[self.pbuf[tb]], writes=[self.aqT_b])
            return deferred
        if fam == "akav":
            ob = self.boutb[so][:, 0:256]
            self.rope(bk, 0, 2, 128, self.cs128[:, lb, :], self.sn128[:, lb, :], ob, self.bout_b[so])
            avst = self.stgb[so][:, 0:256]
            act.op(lambda e: e.activation(out=avst, in_=self.bank(bk, 256, 256), func=AF.Copy),
                   reads=[pb], writes=[self.stg_b[so]], extra=[self.rope_ps_tok])

            def deferred():
                tb = transposes(ob, self.bout_b[so], 2)
                akst = self.stgb[so][:, 256:512]
                act.op(lambda e: e.activation(out=akst, in_=self.bankb(tb, 256), func=AF.Copy),
                       reads=[self.pbuf[tb]], writes=[self.stg_b[so]])
                sp.dma(self.d_stg[so], self.kb_in[:, lb * 256:(lb + 1) * 256], akst, reads=[self.stg_b[so]])
                sp.dma(self.d_stg[so], self.kb_in[:, 3072 + lb * 256:3072 + (lb + 1) * 256], avst,
                       reads=[self.stg_b[so]])
            return deferred
        if fam == "ikiw":
            ob = self.boutb[so][:, 0:128]
            self.rope(bk, 0, 2, 64, self.cs64[:, lb, :], self.sn64[:, lb, :], ob, self.bout_b[so])
            act.op(lambda e: e.activation(out=self.wabs[:, lb, :], in_=self.bank(bk, 16, 128), func=AF.Abs, scale=1.0 / 32),
                   reads=[pb], writes=[self.wabs_b], extra=[self.rope_ps_tok])
            act.op(lambda e: e.activation(out=self.wsgn[:, lb, :], in_=self.bank(bk, 16, 128), func=AF.Sign),
                   reads=[pb], writes=[self.wabs_b])

            def deferred():
                tb = transposes(ob, self.bout_b[so], 1)
                ikst = self.stgb[so][:, 0:128]
                act.op(lambda e: e.activation(out=ikst, in_=self.bankb(tb, 128), func=AF.Copy),
                       reads=[self.pbuf[tb]], writes=[self.stg_b[so]])
                sp.dma(self.d_stg[so], self.kb_in[:, 2048 + lb * 128:2048 + (lb + 1) * 128], ikst,
                       reads=[self.stg_b[so]])
            return deferred
        if fam == "iq":
            of = self.bout[so]
            self.rope(bk, 0, 8, 64, self.cs64[:, lb, :], self.sn64[:, lb, :], of, self.bout_b[so])
            wb_ = self.wabs[:, lb, hs * 8:(hs + 1) * 8].unsqueeze(2).to_broadcast([128, 8, 64])
            dve.op(lambda e: e.tensor_tensor(out=self.aux[so].rearrange("p (h d) -> p h d", h=8, d=64),
                                             in0=of.rearrange("p (h d) -> p h d", h=8, d=64), in1=wb_, op=ALU.mult),
                   reads=[self.bout_b[so], self.wabs_b], writes=[self.aux_b[so]])

            def deferred():
                tb = transposes(self.aux[so], self.aux_b[so], 4)
                dst = self.iqT[:, lb, hs * 4:(hs + 1) * 4, :].rearrange("p h t -> p (h t)")
                act.op(lambda e: e.activation(out=dst, in_=self.bankb(tb, 512), func=AF.Copy),
                       reads=[self.pbuf[tb]], writes=[self.iqT_b])
            return deferred
        raise ValueError(fam)

    def allgather(self):
        self.barrier()
        pool = self.pool
        for (src, dst) in ((self.kv_in, self.kv_all), (self.kb_in, self.kb_all)):
            self.cc_n += 1
            sem = self.cc_sem
            pool.ops.append(lambda e, src=src, dst=dst: e.collective_compute(
                "AllGather", ALU.bypass, replica_groups=[list(range(8))], ins=[src], outs=[dst]).then_inc(sem, 1))
        self.barrier()

    def phaseC1(self):
        pe, act, dve, pool, sp = self.pe, self.act, self.dve, self.pool, self.sp
        sp.dma(self.d_g, self.gnr, self.norms_d[4:5, 0:1024].partition_broadcast(128), writes=[self.gnr_b])
        dve.op(lambda e: e.memset(self.snap.rearrange("p a b c -> p (a b c)"), 0.0), writes=[self.snap_b])
        kvsrc = self.kv_all.rearrange("(r l h d) e -> d r l h e", r=8, l=NB, h=8, d=128)
        gC = self.cstf[:, C_GC:C_GC + 1024]
        step = 0
        for b in range(2):
            dve.op(lambda e: e.memset(self.S, 0.0), writes=[self.S_b])
            for n in range(32):
                k, jj = divmod(n, 8)
                s = step % 2
                if n < 31:
                    sp.dma(self.dkv[s], self.kvbuf[s], kvsrc[:, jj, b * 4 + k, :, :], reads=[self.kv_all_b],
                           writes=[self.kvbuf_b[s]])
                sn = self.snap[:, b, k, :]
                dve.op(lambda e, sn=sn, jj=jj: e.scalar_tensor_tensor(
                    out=sn, in0=self.S, scalar=self.selc[:, jj:jj + 1], in1=sn, op0=ALU.mult, op1=ALU.add),
                    reads=[self.S_b, self.cst_b, self.snap_b], writes=[self.snap_b])
                if n < 31:
                    dve.op(lambda e, s=s: e.tensor_tensor(out=self.S, in0=self.S,
                                                          in1=self.kvbuf[s].rearrange("p h e -> p (h e)"), op=ALU.add),
                           reads=[self.S_b, self.kvbuf_b[s]], writes=[self.S_b])
                    dve.op(lambda e: e.tensor_tensor(out=self.S, in0=self.S, in1=gC, op=ALU.mult),
                           reads=[self.S_b, self.cst_b], writes=[self.S_b])
                    step += 1
        def block(lb):
            b, k = divmod(lb, 4)
            for hg in range(2):
                sbk, obk = hg, 2 + hg
                for h in range(4):
                    H = hg * 4 + h
                    pe.op(lambda e, h=h, H=H, sbk=sbk: e.matmul(
                        self.bank(sbk, 128, h * 128), lhsT=self.kT[:, lb, H, :], rhs=self.qT[:, lb, H, :],
                        start=True, stop=True), reads=[self.kT_b, self.qT_b], writes=[self.pbuf[sbk]])
                dve.op(lambda e, sbk=sbk, hg=hg: e.tensor_tensor(out=self.sTm[hg], in0=self.bank(sbk), in1=self.tri4,
                                                                 op=ALU.mult),
                       reads=[self.pbuf[sbk], self.cst_b], writes=[self.sTm_b[hg]])
                for h in range(4):
                    H = hg * 4 + h
                    pe.op(lambda e, h=h, H=H, obk=obk, hg=hg: e.matmul(
                        self.bank(obk, 128, h * 128), lhsT=self.sTm[hg][:, h * 128:(h + 1) * 128],
                        rhs=self.vr[:, lb, H * 128:(H + 1) * 128], start=True, stop=False),
                        reads=[self.sTm_b[hg], self.vr_b], writes=[self.pbuf[obk]])
                    pe.op(lambda e, h=h, H=H, obk=obk: e.matmul(
                        self.bank(obk, 128, h * 128), lhsT=self.qT[:, lb, H, :],
                        rhs=self.snap[:, b, k, H * 128:(H + 1) * 128], start=False, stop=True),
                        reads=[self.qT_b, self.snap_b], writes=[self.pbuf[obk]])
                act.op(lambda e, obk=obk, hg=hg: e.activation(out=self.t1[:, hg * 512:(hg + 1) * 512],
                                                              in_=self.bank(obk), func=AF.Copy),
                       reads=[self.pbuf[obk]], writes=[self.t1_b])
            t1v = self.t1.rearrange("p (h e) -> p h e", h=8, e=128)
            t2v = self.t2.rearrange("p (h e) -> p h e", h=8, e=128)
            s1 = self.stat[:, 64:72]
            s2 = self.stat[:, 72:80]
            dve.op(lambda e: e.tensor_reduce(out=s1, in_=t1v, axis=AX.X, op=ALU.add), reads=[self.t1_b], writes=[self.stat_b])
            dve.op(lambda e: e.tensor_scalar(out=s1, in0=s1, scalar1=-1.0 / 128, scalar2=None, op0=ALU.mult),
                   reads=[self.stat_b], writes=[self.stat_b])
            dve.op(lambda e: e.tensor_tensor(out=t2v, in0=t1v, in1=s1.unsqueeze(2).to_broadcast([128, 8, 128]), op=ALU.add),
                   reads=[self.t1_b, self.stat_b], writes=[self.t2_b])
            dve.op(lambda e: e.tensor_tensor(out=self.t1, in0=self.t2, in1=self.t2, op=ALU.mult),
                   reads=[self.t2_b], writes=[self.t1_b])
            dve.op(lambda e: e.tensor_reduce(out=s2, in_=t1v, axis=AX.X, op=ALU.add), reads=[self.t1_b], writes=[self.stat_b])
            act.op(lambda e: e.activation(out=s2, in_=s2, func=AF.Sqrt, scale=1.0 / 128, bias=self.cstf[:, C_EPS:C_EPS + 1]),
                   reads=[self.stat_b, self.cst_b], writes=[self.stat_b])
            dve.op(lambda e: e.reciprocal(out=s2, in_=s2), reads=[self.stat_b], writes=[self.stat_b])
            dve.op(lambda e: e.tensor_tensor(out=t1v, in0=t2v, in1=s2.unsqueeze(2).to_broadcast([128, 8, 128]), op=ALU.mult),
                   reads=[self.t2_b, self.stat_b], writes=[self.t1_b])
            dve.op(lambda e: e.tensor_tensor(out=self.t2, in0=self.t1, in1=self.gnr, op=ALU.mult),
                   reads=[self.t1_b, self.gnr_b], writes=[self.t2_b])
            dve.op(lambda e: e.tensor_tensor(out=self.ybf, in0=self.t2, in1=self.sgr[:, lb, :], op=ALU.mult),
                   reads=[self.t2_b, self.sgr_b], writes=[self.ybf_b])
            tb = 4 + lb % 2
            for h in range(8):
                pe.op(lambda e, h=h, tb=tb: e.transpose(out=self.bankb(tb, 128, h * 128),
                                                        in_=self.ybf[:, h * 128:(h + 1) * 128], identity=self.ident),
                      reads=[self.ybf_b, self.cst_b], writes=[self.pbuf[tb]])
            dst = self.mixT[:, 0:8, lb * 128:(lb + 1) * 128]
            act.op(lambda e, tb=tb, dst=dst: e.activation(out=dst, in_=self.bankb(tb).rearrange("p (a b) -> p a b", a=8, b=128),
                                                          func=AF.Copy),
                   reads=[self.pbuf[tb]], writes=[self.hT_b])

        for lb in range(NB):
            block(lb)

    def phaseC2(self):
        pe, act, dve, pool, sp = self.pe, self.act, self.dve, self.pool, self.sp
        kb = self.kb_all
        ksrc = kb[:, 0:2048].rearrange("(j p) (l g t) -> p g l j t", p=128, l=NB, g=2, t=128)
        isrc = kb[:, 2048:3072].rearrange("(j p) (l t) -> p l j t", p=128, l=NB, t=128)
        vsrc = kb[:, 3072:5120].rearrange("(j p) (l g d) -> p g l j d", p=128, l=NB, g=2, d=128)
        self.iu = 0
        self.au = 0
        scale = float(128 ** -0.5)
        st = self.stat
        rmax, rmin, w0, mid, cnt, tmp, tau = (st[:, 96:97], st[:, 97:98], st[:, 98:99], st[:, 99:100],
                                              st[:, 100:101], st[:, 101:102], st[:, 102:103])
        wtab = st[:, 128:128 + NIT + 1]
        SB = self.stat_b

        def load_ik(b):
            pi_ = [(self.ikT[:, k * 1024:(k + 1) * 1024].rearrange("p (j t) -> p j t", j=8, t=128),
                    isrc[:, b * 4 + k, :, :]) for k in range(4)]
            sp.dma_group(self.d_ik, pi_, reads=[self.kb_all_b], writes=[self.ikT_b])

        def load_kv(b):
            pk, pv = [], []
            for k in range(4):
                for g in range(2):
                    pk.append((self.KT[:, g, k * 1024:(k + 1) * 1024].rearrange("p (j t) -> p j t", j=8, t=128),
                               ksrc[:, g, b * 4 + k, :, :]))
                    pv.append((self.V[:, k * 8:(k + 1) * 8, g, :], vsrc[:, g, b * 4 + k, :, :]))
            sp.dma_group(self.d_kt, pk, reads=[self.kb_all_b], writes=[self.KT_b])
            sp.dma_group(self.d_v, pv, reads=[self.kb_all_b], writes=[self.V_b])

        def X(lb):
            S = 1024 * (lb % 4 + 1)
            for kg in range(S // 512):
                for h in range(16):
                    hp, half = divmod(h, 2)
                    bk = self.iu % 2
                    r = self.iu % 4
                    self.iu += 1
                    pr = slice(half * 64, (half + 1) * 64)
                    pe.op(lambda e, bk=bk, pr=pr, hp=hp, kg=kg: e.matmul(
                        self.bank(bk), lhsT=self.iqT[pr, lb, hp, :], rhs=self.ikT[pr, kg * 512:(kg + 1) * 512],
                        start=True, stop=True), reads=[self.iqT_b, self.ikT_b], writes=[self.pbuf[bk]])
                    act.op(lambda e, bk=bk, r=r: e.activation(out=self.rb[r], in_=self.bank(bk), func=AF.Relu),
                           reads=[self.pbuf[bk]], writes=[self.rb_b[r]])
                    sc = self.score[:, kg * 512:(kg + 1) * 512]
                    sgn = self.wsgn[:, lb, h:h + 1]
                    if h == 0:
                        dve.op(lambda e, r=r, sc=sc, sgn=sgn: e.tensor_scalar(
                            out=sc, in0=self.rb[r], scalar1=sgn, scalar2=None, op0=ALU.mult),
                            reads=[self.rb_b[r], self.wabs_b], writes=[self.score_b])
                    else:
                        dve.op(lambda e, r=r, sc=sc, sgn=sgn: e.scalar_tensor_tensor(
                            out=sc, in0=self.rb[r], scalar=sgn, in1=sc, op0=ALU.mult, op1=ALU.add),
                            reads=[self.rb_b[r], self.wabs_b, self.score_b], writes=[self.score_b])

        def Y(lb):
            S = 1024 * (lb % 4 + 1)
            m = lb % 2
            scv = self.score[:, 0:S]
            dve.op(lambda e: e.tensor_reduce(out=rmax, in_=scv, axis=AX.X, op=ALU.max), reads=[self.score_b], writes=[SB])
            dve.op(lambda e: e.tensor_reduce(out=rmin, in_=scv, axis=AX.X, op=ALU.min), reads=[self.score_b], writes=[SB])
            dve.op(lambda e: e.tensor_tensor(out=self.score[:, S - 1024:S], in0=self.score[:, S - 1024:S], in1=self.cb,
                                             op=ALU.add), reads=[self.score_b, self.cst_b], writes=[self.score_b])
            dve.op(lambda e: e.tensor_scalar(out=w0, in0=rmax, scalar1=rmin, scalar2=0.5, op0=ALU.subtract, op1=ALU.mult),
                   reads=[SB], writes=[SB])
            dve.op(lambda e: e.tensor_scalar(out=wtab, in0=self.cstf[:, C_POW2:C_POW2 + NIT + 1], scalar1=w0, scalar2=None,
                                             op0=ALU.mult), reads=[SB, self.cst_b], writes=[SB])
            dve.op(lambda e: e.tensor_tensor(out=mid, in0=rmin, in1=wtab[:, 0:1], op=ALU.add), reads=[SB], writes=[SB])
            for i in range(NIT):
                dve.op(lambda e: e.memset(cnt, 0.0), writes=[SB])
                dve.op(lambda e: e.tensor_scalar(out=self.junk[:, 0:S], in0=scv, scalar1=mid, scalar2=0.0,
                                                 op0=ALU.is_ge, op1=ALU.add, accum_out=cnt),
                       reads=[self.score_b, SB], writes=[self.junk_b, SB])
                dve.op(lambda e, i=i: e.tensor_scalar(out=tmp, in0=cnt, scalar1=256.0, scalar2=wtab[:, i:i + 1],
                                                      op0=ALU.is_ge, op1=ALU.mult), reads=[SB], writes=[SB])
                dve.op(lambda e, i=i: e.scalar_tensor_tensor(out=mid, in0=tmp, scalar=wtab[:, i + 1:i + 2], in1=mid,
                                                             op0=ALU.subtract, op1=ALU.add), reads=[SB], writes=[SB])
            dve.op(lambda e: e.tensor_tensor(out=tau, in0=mid, in1=wtab[:, NIT:NIT + 1], op=ALU.subtract),
                   reads=[SB], writes=[SB])
            dve.op(lambda e: e.tensor_scalar(out=self.mb[m][:, 0:S], in0=scv, scalar1=tau, scalar2=NEG_MASK,
                                             op0=ALU.is_lt, op1=ALU.mult), reads=[self.score_b, SB], writes=[self.mb_b[m]])

        def Zmain(lb):
            S = 1024 * (lb % 4 + 1)
            m = lb % 2
            nch = S // 128
            for c in range(nch):
                for g in range(2):
                    lbk = 2 + self.au % 2
                    ps_ = self.au % 2
                    self.au += 1
                    pe.op(lambda e, lbk=lbk, c=c, g=g: e.matmul(
                        self.bank(lbk), lhsT=self.KT[:, g, c * 128:(c + 1) * 128],
                        rhs=self.aqT[:, lb, g * 4:(g + 1) * 4, :].rearrange("p h t -> p (h t)"), start=True, stop=False),
                        reads=[self.KT_b, self.aqT_b], writes=[self.pbuf[lbk]])
                    pe.op(lambda e, lbk=lbk, c=c: e.matmul(
                        self.bank(lbk), lhsT=self.mb[m][:, c * 128:(c + 1) * 128], rhs=self.ident4, start=False, stop=True),
                        reads=[self.mb_b[m], self.cst_b], writes=[self.pbuf[lbk]])
                    act.op(lambda e, lbk=lbk, ps_=ps_: e.activation(out=self.pT[ps_], in_=self.bank(lbk), func=AF.Exp,
                                                                    scale=scale),
                           reads=[self.pbuf[lbk]], writes=[self.pT_b[ps_]])
                    pe.op(lambda e, c=c, g=g, ps_=ps_: e.matmul(
                        self.bank(4 + g), lhsT=self.V[:, c, g, :], rhs=self.pT[ps_], start=(c == 0), stop=(c == nch - 1)),
                        reads=[self.V_b, self.pT_b[ps_]], writes=[self.pbuf[4 + g]])
                    pe.op(lambda e, c=c, g=g, ps_=ps_: e.matmul(
                        self.bank(6 + g), lhsT=self.ones, rhs=self.pT[ps_], start=(c == 0), stop=(c == nch - 1)),
                        reads=[self.cst_b, self.pT_b[ps_]], writes=[self.pbuf[6 + g]])

        def Zfin(lb):
            for g in range(2):
                dve.op(lambda e, g=g: e.reciprocal(out=self.rcp, in_=self.bank(6 + g)), reads=[self.pbuf[6 + g]],
                       writes=[self.rcp_b])
                dst = self.mixT[:, 8 + g * 4:8 + (g + 1) * 4, lb * 128:(lb + 1) * 128]
                dve.op(lambda e, g=g, dst=dst: e.tensor_tensor(
                    out=dst, in0=self.bank(4 + g).rearrange("p (h t) -> p h t", h=4, t=128),
                    in1=self.rcp.rearrange("p (h t) -> p h t", h=4, t=128), op=ALU.mult),
                    reads=[self.pbuf[4 + g], self.rcp_b], writes=[self.hT_b])

        load_ik(0)
        X(0)
        Y(0)
        for lb in range(1, NB):
            if lb == 4:
                load_ik(1)
            X(lb)
            if lb - 1 in (0, 4):
                load_kv((lb - 1) // 4)
            Zmain(lb - 1)
            Y(lb)
            Zfin(lb - 1)
        Zmain(NB - 1)
        Zfin(NB - 1)

    def phaseD(self):
        pe, dve, pool = self.pe, self.dve, self.pool
        self.barrier()
        self.load_x(self.xspill)
        for cg in range(4):
            s = cg % 2
            pool.dma(self.dw[s], self.win[s].rearrange("p a b -> p (a b)"), self.wout_d[cg],
                     writes=[self.wg_b[s], self.wu_b[s]])
            for lb in range(NB):
                bk = (cg * NB + lb) % 4
                for kc in range(16):
                    pe.op(lambda e, bk=bk, kc=kc, lb=lb, s=s: e.matmul(
                        self.bank(bk), lhsT=self.mixT[:, kc, lb * 128:(lb + 1) * 128], rhs=self.win[s][:, kc, :],
                        start=(kc == 0), stop=(kc == 15)),
                        reads=[self.hT_b, self.wg_b[s], self.wu_b[s]], writes=[self.pbuf[bk]])
                xsl = self.xs[:, lb, cg * 512:(cg + 1) * 512]
                dve.op(lambda e, bk=bk, xsl=xsl: e.tensor_tensor(out=xsl, in0=self.bank(bk), in1=xsl, op=ALU.add),
                       reads=[self.pbuf[bk], self.xs_b[lb]], writes=[self.xs_b[lb]])
        self.barrier()

    def build(self):
        stop = self.stop
        self.phase0()
        self.barrier()
        self.norm_T(0)
        self.ffn(0)

        def finish_reload():
            self.barrier()
            self.load_x(self.xspill)
            self.final_norm()
            self.barrier()
        if stop == "ffn1":
            self.final_norm()
            self.barrier()
            return
        self.phaseB()
        if stop == "B":
            return finish_reload()
        self.allgather()
        if stop == "AG":
            return finish_reload()
        self.phaseC1()
        self.barrier()
        if stop == "C1":
            if self.debug:
                self.barrier()
                self.sp.dma(self.d_st, self.dbg_mix, self.mixT.rearrange("p a b -> p (a b)"), reads=[self.hT_b])
            return finish_reload()
        self.phaseC2()
        if self.debug:
            self.barrier()
            self.sp.dma(self.d_st, self.dbg_mix, self.mixT.rearrange("p a b -> p (a b)"), reads=[self.hT_b])
        if stop == "C2":
            return finish_reload()
        self.phaseD()
        if self.debug:
            for lb in range(NB):
                self.sp.dma(self.d_st, self.dbg_x[lb], self.xs[:, lb, :], reads=[self.xs_b[lb]])
        if stop != "D":
            self.norm_T(2)
            self.ffn(1)
        self.final_norm()
        self.barrier()


_CACHE = {}


def _consts():
    f = np.zeros((128, NF), np.float32)
    f[:, C_INV128:C_INV128 + 64] = (np.float32(10000.0) ** (-(np.arange(0, 128, 2, dtype=np.float32) / np.float32(128)))).astype(np.float32)[None]
    f[:, C_INV64:C_INV64 + 32] = (np.float32(10000.0) ** (-(np.arange(0, 64, 2, dtype=np.float32) / np.float32(64)))).astype(np.float32)[None]
    h = np.arange(8, dtype=np.float64)
    lg = np.log1p(-np.exp2(-5.0 - h))
    p1 = np.arange(1, 129, dtype=np.float64)
    f[:, C_GQ:C_GQ + 8] = np.exp(p1[:, None] * lg[None, :])
    f[:, C_GK:C_GK + 8] = np.exp(-p1[:, None] * lg[None, :]) * 128 ** -0.5
    f[:, C_GQT:C_GQT + 1024] = np.exp(lg[:, None] * p1[None, :]).reshape(1, 1024)
    f[:, C_GKT:C_GKT + 1024] = (np.exp(-lg[:, None] * p1[None, :]) * 128 ** -0.5).reshape(1, 1024)
    f[:, C_GC:C_GC + 1024] = np.repeat(np.exp(128 * lg), 128)[None]
    f[:, C_POW2:C_POW2 + 24] = (2.0 ** -np.arange(24))[None]
    f[:, C_EPS] = 1e-6
    cb = np.zeros((128, NCB), np.float32)
    eye = np.eye(128, dtype=np.float32)
    cb[:, 0:512] = np.tile(eye, (1, 4))
    tri = (np.arange(128)[None, :] >= np.arange(128)[:, None]).astype(np.float32)
    cb[:, 512:1024] = np.tile(tri, (1, 4))
    cb[:, 1024:1152] = 1.0
    return f, cb.astype(ml_dtypes.bfloat16)


def _percore_consts(j):
    c = np.zeros((128, NCC), np.float32)
    qrel = 128 * j + np.arange(128)
    c[:, 0:1024] = np.where(np.arange(1024)[None, :] > qrel[:, None], np.float32(-1e30), np.float32(0.0))
    c[:, 1024 + j] = 1.0
    return c


def _win_layout(w_in):
    O = dict(rq=0, rk=1024, rv=2048, rg=3072, aq=4096, ak=5120, av=5376, iq=5632, ik=6656, iw=6720)
    cols = []

    def rng(a, n):
        return list(range(a, a + n))
    cols += rng(O["rv"], 1024) + rng(O["rk"], 1024) + rng(O["rq"], 1024) + rng(O["rg"], 1024) + rng(O["aq"], 1024)
    cols += rng(O["ak"], 256) + rng(O["av"], 256)
    cols += rng(O["ik"], 64) + rng(O["ik"], 64) + rng(O["iw"], 16) + [-1] * 368
    cols += rng(O["iq"], 1024)
    cols = np.array(cols)
    W = np.zeros((D, 14 * 512), np.float32)
    m = cols >= 0
    W[:, m] = w_in[:, cols[m]]
    return np.ascontiguousarray(W.reshape(16, 128, 14, 512).transpose(2, 1, 0, 3)).reshape(14, 128, 16 * 512)


def _gu_layout(W):
    return np.ascontiguousarray(W.reshape(16, 128, NFG, 256).transpose(2, 1, 0, 3)).reshape(NFG, 128, 16 * 256)


def _wd_layout(W):
    return np.ascontiguousarray(W.reshape(NFG, 2, 128, D).transpose(0, 2, 1, 3)).reshape(NFG, 128, 2 * D)


def _wout_layout(W):
    return np.ascontiguousarray(W.reshape(16, 128, 4, 512).transpose(2, 1, 0, 3)).reshape(4, 128, 16 * 512)


def _chunks(j):
    return [(b, 8 * k + j) for b in range(2) for k in range(4)]


def kernel(x, positions, ffn1_norm, ffn1_w_gate, ffn1_w_up, ffn1_w_down, mix_norm, w_in, ret_norm, w_out,
           ffn2_norm, ffn2_w_gate, ffn2_w_up, ffn2_w_down, final_norm, _stop="full", _debug=False):
    x = np.asarray(x, np.float32)
    positions = np.asarray(positions, np.int32)
    key = (_stop, _debug)
    if key not in _CACHE:
        _CACHE[key] = Prog(_stop, _debug)
    prog = _CACHE[key]
    cf, cbb = _consts()
    norms = np.zeros((5, D), np.float32)
    norms[0] = np.asarray(ffn1_norm)[0]
    norms[1] = np.asarray(mix_norm)[0]
    norms[2] = np.asarray(ffn2_norm)[0]
    norms[3] = np.asarray(final_norm)
    norms[4, :1024] = np.asarray(ret_norm)[0]
    shared = dict(
        norms=norms,
        wg1=_gu_layout(np.asarray(ffn1_w_gate, np.float32)[0]), wu1=_gu_layout(np.asarray(ffn1_w_up, np.float32)[0]),
        wd1=_wd_layout(np.asarray(ffn1_w_down, np.float32)[0]),
        wg2=_gu_layout(np.asarray(ffn2_w_gate, np.float32)[0]), wu2=_gu_layout(np.asarray(ffn2_w_up, np.float32)[0]),
        wd2=_wd_layout(np.asarray(ffn2_w_down, np.float32)[0]),
        win=_win_layout(np.asarray(w_in, np.float32)[0]), wout=_wout_layout(np.asarray(w_out, np.float32)[0]),
        cstf=cf, cstb=cbb,
    )
    in_maps = []
    for j in range(8):
        ch = _chunks(j)
        xm = np.stack([x[b, n * 128:(n + 1) * 128] for (b, n) in ch])
        pm = np.stack([positions[b, n * 128:(n + 1) * 128] for (b, n) in ch], axis=1)
        m = dict(shared)
        m["x"] = np.ascontiguousarray(xm)
        m["pos"] = np.ascontiguousarray(pm.astype(np.int32))
        m["cstc"] = _percore_consts(j)
        in_maps.append(m)
    res = run_bass_kernel_spmd(prog.nc, in_maps, core_ids=list(range(8)))
    if _debug:
        kernel.dbg = [(res.results[j]["dbg_mix"], res.results[j]["dbg_x"]) for j in range(8)]
    out = np.zeros((2, 4096, D), np.float32)
    for j in range(8):
        o = res.results[j]["out"]
        for lb, (b, n) in enumerate(_chunks(j)):
            out[b, n * 128:(n + 1) * 128] = o[lb]
    return out
```

```python
import numpy as np
import ml_dtypes
from contextlib import ExitStack
import concourse.bass as bass
import concourse.mybir as mybir
from concourse.bass_utils import run_bass_kernel_spmd

F32 = mybir.dt.float32
BF16 = mybir.dt.bfloat16
I32 = mybir.dt.int32
ALU = mybir.AluOpType
AF = mybir.ActivationFunctionType
AX = mybir.AxisListType

D = 2048
DFF = 5632
NFG = 22
NB = 8
NIT = 20
NEG_MASK = -30000.0
PI = float(np.pi)

C_INV128, C_INV64, C_GQ, C_GK, C_GQT, C_GKT, C_GC, C_POW2, C_EPS, NF = 0, 64, 96, 104, 112, 1136, 2160, 3184, 3208, 3216
NCC = 1032
NCB = 1152


class Tok:
    __slots__ = ("sem", "v", "q")

    def __init__(self, sem, v, q):
        self.sem = sem
        self.v = v
        self.q = q


class Buf:
    __slots__ = ("name", "w", "r")

    def __init__(self, name=""):
        self.name = name
        self.w = None
        self.r = {}


class DSem:
    def __init__(self, nc, es, name):
        self.sem = es.enter_context(nc.semaphore(name))
        self.n = 0


class Q:
    def __init__(self, nc, es, name, is_pe=False):
        self.name = name
        self.sem = es.enter_context(nc.semaphore("q_" + name))
        self.n = 0
        self.ops = []
        self.waited = {}
        self.is_pe = is_pe

    def wait(self, toks):
        for t in toks:
            if t is None:
                continue
            k = id(t.sem)
            if self.waited.get(k, 0) >= t.v:
                continue
            self.waited[k] = t.v
            self.ops.append(lambda e, t=t: e.wait_ge(t.sem, t.v))

    def _deps(self, reads, writes, dma):
        deps = []
        for b in reads:
            t = b.w
            if t is not None and (dma or not (t.q is self and self.is_pe)):
                deps.append(t)
        for b in writes:
            for t in list(b.r.values()) + [b.w]:
                if t is None:
                    continue
                if (not dma) and t.q is self:
                    continue
                deps.append(t)
        return deps

    @staticmethod
    def _mark(tok, reads, writes):
        k = id(tok.sem)
        for b in reads:
            o = b.r.get(k)
            if o is None or o.v < tok.v:
                b.r[k] = tok
        for b in writes:
            b.w = tok
            b.r = {}

    def op(self, fn, reads=(), writes=(), extra=()):
        self.wait(self._deps(reads, writes, False) + list(extra))
        self.n += 1
        n = self.n
        sem = self.sem
        self.ops.append(lambda e: fn(e).then_inc(sem, 1))
        tok = Tok(sem, n, self)
        self._mark(tok, reads, writes)
        return tok

    def dma(self, dsem, out, in_, reads=(), writes=(), extra=()):
        self.wait(self._deps(reads, writes, True) + list(extra))
        dsem.n += 16
        s = dsem.sem
        self.ops.append(lambda e: e.dma_start(out=out, in_=in_).then_inc(s, 16))
        tok = Tok(s, dsem.n, dsem)
        self._mark(tok, reads, writes)
        return tok

    def dma_group(self, dsem, pairs, reads=(), writes=(), extra=()):
        self.wait(self._deps(reads, writes, True) + list(extra))
        s = dsem.sem
        for (out, in_) in pairs:
            dsem.n += 16
            self.ops.append(lambda e, out=out, in_=in_: e.dma_start(out=out, in_=in_).then_inc(s, 16))
        tok = Tok(s, dsem.n, dsem)
        self._mark(tok, reads, writes)
        return tok

    def replay(self, e):
        for o in self.ops:
            o(e)


def _prod(s):
    r = 1
    for v in s:
        r *= int(v)
    return r


ARENA = 210944


class Prog:
    def __init__(self, stop="full", debug=False):
        self.stop = stop
        self.debug = debug
        nc = self.nc = bass.Bass("TRN2", target_bir_lowering=False, num_devices=8)
        es = self.es = ExitStack()
        dt = nc.dram_tensor

        def din(name, shape, d=F32):
            return dt(name, shape, d, kind="ExternalInput").ap()
        self.x_d = din("x", [NB, 128, D])
        self.pos_d = din("pos", [128, NB], I32)
        self.norms_d = din("norms", [5, D])
        self.wg_d = [din("wg1", [NFG, 128, 16 * 256]), din("wg2", [NFG, 128, 16 * 256])]
        self.wu_d = [din("wu1", [NFG, 128, 16 * 256]), din("wu2", [NFG, 128, 16 * 256])]
        self.wd_d = [din("wd1", [NFG, 128, 2 * D]), din("wd2", [NFG, 128, 2 * D])]
        self.win_d = din("win", [14, 128, 16 * 512])
        self.wout_d = din("wout", [4, 128, 16 * 512])
        self.cstf_d = din("cstf", [128, NF])
        self.cstc_d = din("cstc", [128, NCC])
        self.cstb_d = din("cstb", [128, NCB], BF16)
        self.out_d = dt("out", [NB, 128, D], F32, kind="ExternalOutput").ap()
        if debug:
            self.dbg_mix = dt("dbg_mix", [128, 16384], BF16, kind="ExternalOutput").ap()
            self.dbg_x = dt("dbg_x", [NB, 128, D], F32, kind="ExternalOutput").ap()
        self.xspill = dt("xspill", [NB, 128, D], F32, kind="Internal").ap()
        self.kv_in = dt("kv_in", [NB * 8 * 128, 128], F32, kind="Internal").ap()
        self.kv_all = dt("kv_all", [8 * NB * 8 * 128, 128], F32, kind="Internal", addr_space="Shared").ap()
        self.kb_in = dt("kb_in", [128, 5120], BF16, kind="Internal").ap()
        self.kb_all = dt("kb_all", [1024, 5120], BF16, kind="Internal", addr_space="Shared").ap()

        self.arena = es.enter_context(nc.sbuf_tensor("arena", [128, ARENA // 4], F32))
        self.arena_b = self.arena.bitcast(BF16)
        self.arena_i = self.arena.bitcast(I32)
        self.ps = es.enter_context(nc.psum_tensor("ps", [128, 4096], F32))
        self.psb = self.ps.bitcast(BF16)
        self.pbuf = [Buf("pb%d" % i) for i in range(8)]

        self.pe = Q(nc, es, "pe", True)
        self.act = Q(nc, es, "act")
        self.dve = Q(nc, es, "dve")
        self.pool = Q(nc, es, "pool")
        self.sp = Q(nc, es, "sp")
        self.queues = [self.pe, self.act, self.dve, self.pool, self.sp]
        self.dsems = []
        self.cc_sem = es.enter_context(nc.semaphore("cc"))
        self.cc_n = 0

        self.layout()
        self.build()

        blk = es.enter_context(nc.Block())
        blk.sync(self.sp.replay)
        blk.tensor(self.pe.replay)
        blk.scalar(self.act.replay)
        blk.vector(self.dve.replay)
        blk.gpsimd(self.pool.replay)
        es.close()

    def dsem(self, name):
        d = DSem(self.nc, self.es, name)
        self.dsems.append(d)
        return d

    def carve(self, off, shape, dtype):
        n = _prod(shape[1:])
        if dtype == BF16:
            assert off % 2 == 0
            ap = self.arena_b[:, off // 2: off // 2 + n]
        elif dtype == I32:
            ap = self.arena_i[:, off // 4: off // 4 + n]
        else:
            assert off % 4 == 0
            ap = self.arena[:, off // 4: off // 4 + n]
        if len(shape) == 3:
            ap = ap.rearrange("p (a b) -> p a b", a=shape[1], b=shape[2])
        elif len(shape) == 4:
            ap = ap.rearrange("p (a b c) -> p a b c", a=shape[1], b=shape[2], c=shape[3])
        elif len(shape) == 5:
            ap = ap.rearrange("p (a b c d) -> p a b c d", a=shape[1], b=shape[2], c=shape[3], d=shape[4])
        return ap

    def bank(self, i, n=512, off=0):
        return self.ps[:, i * 512 + off: i * 512 + off + n]

    def bankb(self, i, n=1024, off=0):
        return self.psb[:, i * 1024 + off: i * 1024 + off + n]

    def barrier(self):
        toks = [Tok(q.sem, q.n, q) for q in self.queues if q.n > 0]
        toks += [Tok(d.sem, d.n, d) for d in self.dsems if d.n > 0]
        if self.cc_n:
            toks.append(Tok(self.cc_sem, self.cc_n, None))
        for q in self.queues:
            q.wait(toks)

    def layout(self):
        c = self.carve
        R_X, R_HT, R_W, R_WD, R_GBC, R_T, R_CST = 0, 65536, 98304, 131072, 147456, 155648, 176128
        self.xs = c(R_X, [128, NB, D], F32)
        self.xs_b = [Buf("xs%d" % i) for i in range(NB)]
        self.hT = c(R_HT, [128, 16, 1024], BF16)
        self.hT_b = Buf("hT")
        self.wgu = [c(R_W + s * 16384, [128, 2, 16, 256], BF16) for s in range(2)]
        self.win = [c(R_W + s * 16384, [128, 16, 512], BF16) for s in range(2)]
        self.wg_b = [Buf("wg%d" % s) for s in range(2)]
        self.wu_b = [Buf("wu%d" % s) for s in range(2)]
        self.wdr = [c(R_WD + s * 8192, [128, 2, D], BF16) for s in range(2)]
        self.wd_b = [Buf("wd%d" % s) for s in range(2)]
        self.dw = [self.dsem("dw%d" % s) for s in range(2)]
        self.dwu = [self.dsem("dwu%d" % s) for s in range(2)]
        self.dwd = [self.dsem("dwd%d" % s) for s in range(2)]
        self.gbc = c(R_GBC, [128, D], F32)
        self.gbc_b = Buf("gbc")
        self.aT = [c(R_T + g * 4096, [128, 2, 1024], BF16) for g in range(2)]
        self.aT_b = [Buf("aT%d" % g) for g in range(2)]
        self.sg = [c(R_T + 8192 + s * 2048, [128, 512], F32) for s in range(2)]
        self.sg_b = [Buf("sg%d" % s) for s in range(2)]
        self.hb = [c(R_T + 12288 + s * 4096, [128, D], BF16) for s in range(2)]
        self.hb_b = [Buf("hb%d" % s) for s in range(2)]
        o = R_CST
        self.cstf = c(o, [128, NF], F32); o += NF * 4
        self.cstc = c(o, [128, NCC], F32); o += NCC * 4
        self.cstb = c(o, [128, NCB], BF16); o += NCB * 2
        self.cs128 = c(o, [128, NB, 64], F32); o += 2048
        self.sn128 = c(o, [128, NB, 64], F32); o += 2048
        self.cs64 = c(o, [128, NB, 32], F32); o += 1024
        self.sn64 = c(o, [128, NB, 32], F32); o += 1024
        self.wabs = c(o, [128, NB, 16], F32); o += 512
        self.wsgn = c(o, [128, NB, 16], F32); o += 512
        self.stat = c(o, [128, 256], F32); o += 1024
        self.posi = c(o, [128, NB], I32); o += 32
        self.stg = [c(o + s * 2048, [128, 512], F32) for s in range(2)]
        self.stgb = [c(o + s * 2048, [128, 1024], BF16) for s in range(2)]
        o += 4096
        assert o <= ARENA, o
        self.cst_b = Buf("cst")
        self.rope_b = Buf("ropetab")
        self.wabs_b = Buf("wabs")
        self.stat_b = Buf("stat")
        self.stg_b = [Buf("stg%d" % s) for s in range(2)]
        self.ident4 = self.cstb[:, 0:512]
        self.ident = self.cstb[:, 0:128]
        self.tri4 = self.cstb[:, 512:1024]
        self.ones = self.cstb[:, 1024:1152]
        self.cb = self.cstc[:, 0:1024]
        self.selc = self.cstc[:, 1024:1032]
        self.qT = c(R_X, [128, NB, 8, 128], BF16)
        self.kT = c(R_X + 16384, [128, NB, 8, 128], BF16)
        self.vr = c(R_X + 32768, [128, NB, 1024], BF16)
        self.sgr = c(R_X + 49152, [128, NB, 1024], BF16)
        self.aqT = c(R_WD, [128, NB, 8, 128], BF16)
        BT = R_GBC
        self.ropeA = c(BT, [128, 512], F32)
        self.ropeB = c(BT + 2048, [128, 512], F32)
        self.bout = [c(BT + 4096 + s * 2048, [128, 512], F32) for s in range(2)]
        self.boutb = [c(BT + 4096 + s * 2048, [128, 512], BF16) for s in range(2)]
        self.aux = [c(BT + 8192 + s * 1024, [128, 512], BF16) for s in range(2)]
        self.iqT = c(BT + 12288, [128, NB, 8, 128], BF16)
        self.qT_b, self.kT_b, self.vr_b, self.sgr_b = Buf("qT"), Buf("kT"), Buf("vr"), Buf("sgr")
        self.aqT_b, self.iqT_b = Buf("aqT"), Buf("iqT")
        self.ropeA_b, self.ropeB_b = Buf("ropeA"), Buf("ropeB")
        self.bout_b = [Buf("bout0"), Buf("bout1")]
        self.aux_b = [Buf("aux0"), Buf("aux1")]
        self.t0a = c(BT, [128, NB, 64], F32)
        self.t0b = c(BT + 2048, [128, NB, 64], F32)
        self.t0i = c(BT + 4096, [128, NB, 64], I32)
        self.t0c = c(BT + 6144, [128, NB, 64], F32)
        self.snap = c(R_W, [128, 2, 4, 1024], BF16)
        self.S = c(R_W + 16384, [128, 1024], F32)
        self.kvbuf = [c(R_W + 20480 + s * 4096, [128, 8, 128], F32) for s in range(2)]
        self.gnr = c(R_W + 28672, [128, 1024], F32)
        self.snap_b, self.S_b, self.gnr_b = Buf("snap"), Buf("S"), Buf("gnr")
        self.kvbuf_b = [Buf("kvb0"), Buf("kvb1")]
        self.dkv = [self.dsem("dkv%d" % s) for s in range(2)]
        self.t1 = c(BT, [128, 1024], F32)
        self.t2 = c(BT + 4096, [128, 1024], F32)
        self.ybf = c(BT + 8192, [128, 1024], BF16)
        self.sTm = [c(BT + 10240 + s * 1024, [128, 512], BF16) for s in range(2)]
        self.t1_b, self.t2_b, self.ybf_b = Buf("t1"), Buf("t2"), Buf("ybf")
        self.sTm_b = [Buf("sTm0"), Buf("sTm1")]
        self.KT = c(R_X, [128, 2, 4096], BF16)
        self.V = c(R_X + 16384, [128, 32, 2, 128], BF16)
        self.ikT = c(R_X + 32768, [128, 4096], BF16)
        self.rb = [c(R_X + 40960 + s * 2048, [128, 512], F32) for s in range(4)]
        self.pT = [c(R_X + 49152 + s * 1024, [128, 512], BF16) for s in range(2)]
        self.rcp = c(R_X + 51200, [128, 512], F32)
        self.KT_b, self.V_b, self.ikT_b = Buf("KT"), Buf("V"), Buf("ikT")
        self.rb_b = [Buf("rb%d" % s) for s in range(4)]
        self.pT_b = [Buf("pT0"), Buf("pT1")]
        self.rcp_b = Buf("rcp")
        self.rbb = [c(R_X + 40960 + s * 2048, [128, 512], BF16) for s in range(4)]
        self.dg = c(R_X + 61440, [128, 16, 128], BF16)
        self.dg_b = Buf("dg")
        self.score = c(R_W, [128, 4096], F32)
        self.mb = [c(R_W + 16384 + i * 8192, [128, 4096], BF16) for i in range(2)]
        self.junk = c(R_X + 53248, [128, 4096], BF16)
        self.score_b, self.mb_b, self.junk_b = Buf("score"), [Buf("mb0"), Buf("mb1")], Buf("junk")
        self.mixT = self.hT
        self.xspill_b = [Buf("xsp%d" % i) for i in range(NB)]
        self.kv_in_b, self.kb_in_b = Buf("kv_in"), Buf("kb_in")
        self.kv_all_b, self.kb_all_b = Buf("kv_all"), Buf("kb_all")
        self.d_ld = self.dsem("d_ld")
        self.d_x = [self.dsem("d_x%d" % i) for i in range(NB)]
        self.d_st = self.dsem("d_st")
        self.d_g = self.dsem("d_g")
        self.d_kt, self.d_v, self.d_ik = self.dsem("d_kt"), self.dsem("d_v"), self.dsem("d_ik")
        self.d_stg = [self.dsem("d_stg%d" % i) for i in range(2)]

    def load_x(self, src):
        for lb in range(NB):
            self.sp.dma(self.d_x[lb], self.xs[:, lb, :], src[lb], writes=[self.xs_b[lb]])

    def sincos(self, ang_fn, shape, out_sin, shift, n):
        dve, act = self.dve, self.act
        tb = self.t0b[:, :, 0:n]
        ti = self.t0i[:, :, 0:n]
        tc_ = self.t0c[:, :, 0:n]
        B1, B2, B3 = self.ropeB_b, self.bout_b[0], self.bout_b[1]
        ang = ang_fn
        dve.op(lambda e: e.tensor_scalar(out=tc_, in0=ang, scalar1=float(shift), scalar2=None, op0=ALU.add), reads=[self.ropeA_b], writes=[B3])
        dve.op(lambda e: e.tensor_scalar(out=tb, in0=tc_, scalar1=float(1.0 / (2 * PI)), scalar2=None, op0=ALU.mult), reads=[B3], writes=[B1])
        dve.op(lambda e: e.tensor_copy(out=ti, in_=tb), reads=[B1], writes=[B2])
        dve.op(lambda e: e.tensor_copy(out=tb, in_=ti), reads=[B2], writes=[B1])
        dve.op(lambda e: e.scalar_tensor_tensor(out=tc_, in0=tb, scalar=float(-2 * PI), in1=tc_, op0=ALU.mult, op1=ALU.add), reads=[B1, B3], writes=[B3])
        dve.op(lambda e: e.tensor_scalar(out=tb, in0=tc_, scalar1=PI, scalar2=float(-2 * PI), op0=ALU.is_gt, op1=ALU.mult), reads=[B3], writes=[B1])
        dve.op(lambda e: e.tensor_tensor(out=tc_, in0=tc_, in1=tb, op=ALU.add), reads=[B1, B3], writes=[B3])
        dve.op(lambda e: e.tensor_scalar(out=tb, in0=tc_, scalar1=-PI, scalar2=float(2 * PI), op0=ALU.is_lt, op1=ALU.mult), reads=[B3], writes=[B1])
        dve.op(lambda e: e.tensor_tensor(out=tc_, in0=tc_, in1=tb, op=ALU.add), reads=[B1, B3], writes=[B3])
        act.op(lambda e: e.activation(out=out_sin, in_=tc_, func=AF.Sin), reads=[B3], writes=[self.rope_b])

    def phase0(self):
        sp, dve = self.sp, self.dve
        sp.dma(self.d_ld, self.cstf, self.cstf_d, writes=[self.cst_b])
        sp.dma(self.d_ld, self.cstc, self.cstc_d, writes=[self.cst_b])
        sp.dma(self.d_ld, self.cstb, self.cstb_d, writes=[self.cst_b])
        sp.dma(self.d_ld, self.posi, self.pos_d, writes=[self.cst_b])
        self.load_x(self.x_d)
        posf = self.stat[:, 0:NB]
        dve.op(lambda e: e.tensor_copy(out=posf, in_=self.posi), reads=[self.cst_b], writes=[self.stat_b])
        for n, inv_off, cs, sn in ((64, C_INV128, self.cs128, self.sn128), (32, C_INV64, self.cs64, self.sn64)):
            ang = self.t0a[:, :, 0:n]
            inv = self.cstf[:, inv_off:inv_off + n]
            dve.op(lambda e, ang=ang, inv=inv, n=n: e.tensor_tensor(
                out=ang, in0=inv.unsqueeze(1).to_broadcast([128, NB, n]),
                in1=posf.unsqueeze(2).to_broadcast([128, NB, n]), op=ALU.mult),
                reads=[self.cst_b, self.stat_b], writes=[self.ropeA_b])
            self.sincos(ang, None, sn, 0.0, n)
            self.sincos(ang, None, cs, PI / 2, n)

    def norm_stats(self, row):
        sp, act, dve = self.sp, self.act, self.dve
        sp.dma(self.d_g, self.gbc, self.norms_d[row:row + 1, :].partition_broadcast(128), writes=[self.gbc_b])
        ss = self.stat[:, 16:16 + NB]
        for lb in range(NB):
            act.op(lambda e, lb=lb: e.activation(out=self.hb[1], in_=self.xs[:, lb, :], func=AF.Square,
                                                 accum_out=ss[:, lb:lb + 1]),
                   reads=[self.xs_b[lb]], writes=[self.hb_b[1], self.stat_b])
        rstd = self.stat[:, 32:32 + NB]
        act.op(lambda e: e.activation(out=rstd, in_=ss, func=AF.Sqrt, scale=1.0 / D,
                                      bias=self.cstf[:, C_EPS:C_EPS + 1]),
               reads=[self.stat_b, self.cst_b], writes=[self.stat_b])
        dve.op(lambda e: e.reciprocal(out=rstd, in_=rstd), reads=[self.stat_b], writes=[self.stat_b])
        return rstd

    def norm_T(self, row):
        pe, act, dve = self.pe, self.act, self.dve
        rstd = self.norm_stats(row)
        for lb in range(NB):
            s = lb % 2
            hb = self.hb[s]
            dve.op(lambda e, lb=lb, hb=hb: e.scalar_tensor_tensor(
                out=hb, in0=self.xs[:, lb, :], scalar=rstd[:, lb:lb + 1], in1=self.gbc,
                op0=ALU.mult, op1=ALU.mult),
                reads=[self.xs_b[lb], self.stat_b, self.gbc_b], writes=[self.hb_b[s]])
            for half in range(2):
                bk = 4 + 2 * s + half
                for i in range(8):
                    kc = half * 8 + i
                    pe.op(lambda e, bk=bk, i=i, kc=kc, hb=hb: e.transpose(
                        out=self.bankb(bk, 128, i * 128), in_=hb[:, kc * 128:(kc + 1) * 128], identity=self.ident),
                        reads=[self.hb_b[s], self.cst_b], writes=[self.pbuf[bk]])
                dst = self.hT[:, half * 8:(half + 1) * 8, lb * 128:(lb + 1) * 128]
                src = self.bankb(bk).rearrange("p (a b) -> p a b", a=8, b=128)
                eng = act if half == 0 else dve
                if eng is act:
                    eng.op(lambda e, dst=dst, src=src: e.activation(out=dst, in_=src, func=AF.Copy),
                           reads=[self.pbuf[bk]], writes=[self.hT_b])
                else:
                    eng.op(lambda e, dst=dst, src=src: e.tensor_copy(out=dst, in_=src),
                           reads=[self.pbuf[bk]], writes=[self.hT_b])

    def final_norm(self):
        dve, sp = self.dve, self.sp
        rstd = self.norm_stats(3)
        last = None
        for lb in range(NB):
            s = lb % 2
            o = self.sg[s]
            dve.op(lambda e, lb=lb: e.scalar_tensor_tensor(
                out=self.xs[:, lb, :], in0=self.xs[:, lb, :], scalar=rstd[:, lb:lb + 1], in1=self.gbc,
                op0=ALU.mult, op1=ALU.mult),
                reads=[self.xs_b[lb], self.stat_b, self.gbc_b], writes=[self.xs_b[lb]])
            last = sp.dma(self.d_st, self.out_d[lb], self.xs[:, lb, :], reads=[self.xs_b[lb]])
        return last

    def ffn(self, idx):
        pe, act, dve, pool = self.pe, self.act, self.dve, self.pool
        wg_d, wu_d, wd_d = self.wg_d[idx], self.wu_d[idx], self.wd_d[idx]

        def load(fg):
            s = fg % 2
            pool.dma(self.dw[s], self.wgu[s][:, 0].rearrange("p a b -> p (a b)"), wg_d[fg], writes=[self.wg_b[s]])
            pool.dma(self.dwu[s], self.wgu[s][:, 1].rearrange("p a b -> p (a b)"), wu_d[fg], writes=[self.wu_b[s]])
            pool.dma(self.dwd[s], self.wdr[s].rearrange("p a b -> p (a b)"), wd_d[fg], writes=[self.wd_b[s]])

        def gate_up(fg):
            s = fg % 2
            for f in range(2):
                for half in range(2):
                    u = f * 2 + half
                    pg, pu = (u % 2) * 2, (u % 2) * 2 + 1
                    rhs_sl = slice(half * 512, (half + 1) * 512)
                    for (bk, wi, wb) in ((pg, 0, self.wg_b[s]), (pu, 1, self.wu_b[s])):
                        for kc in range(16):
                            pe.op(lambda e, bk=bk, wi=wi, kc=kc, f=f, s=s, rhs_sl=rhs_sl: e.matmul(
                                self.bank(bk), lhsT=self.wgu[s][:, wi, kc, f * 128:(f + 1) * 128],
                                rhs=self.hT[:, kc, rhs_sl], start=(kc == 0), stop=(kc == 15)),
                                reads=[wb, self.hT_b], writes=[self.pbuf[bk]])
                    sgs = u % 2
                    act.op(lambda e, pg=pg, sgs=sgs: e.activation(out=self.sg[sgs], in_=self.bank(pg), func=AF.Silu),
                           reads=[self.pbuf[pg]], writes=[self.sg_b[sgs]])
                    dve.op(lambda e, pu=pu, sgs=sgs, fg=fg, f=f, rhs_sl=rhs_sl: e.tensor_tensor(
                        out=self.aT[fg % 2][:, f, rhs_sl], in0=self.sg[sgs], in1=self.bank(pu), op=ALU.mult),
                        reads=[self.sg_b[sgs], self.pbuf[pu]], writes=[self.aT_b[fg % 2]])

        def down(fg):
            s = fg % 2
            for tb in range(NB):
                for ch in range(2):
                    par = (tb * 2 + ch) % 2
                    for cbk in range(2):
                        bk = 4 + par * 2 + cbk
                        for f in range(2):
                            c0 = ch * 1024 + cbk * 512
                            pe.op(lambda e, bk=bk, f=f, tb=tb, c0=c0, fg=fg, s=s: e.matmul(
                                self.bank(bk), lhsT=self.aT[fg % 2][:, f, tb * 128:(tb + 1) * 128],
                                rhs=self.wdr[s][:, f, c0:c0 + 512], start=(f == 0), stop=(f == 1)),
                                reads=[self.aT_b[fg % 2], self.wd_b[s]], writes=[self.pbuf[bk]])
                    b0 = 4 + par * 2
                    xsl = self.xs[:, tb, ch * 1024:(ch + 1) * 1024]
                    dve.op(lambda e, b0=b0, xsl=xsl: e.scalar_tensor_tensor(
                        out=xsl, in0=self.bank(b0, 1024), scalar=0.5, in1=xsl, op0=ALU.mult, op1=ALU.add),
                        reads=[self.pbuf[b0], self.pbuf[b0 + 1], self.xs_b[tb]], writes=[self.xs_b[tb]])

        nfg = NFG
        load(0)
        load(1)
        for fg in range(nfg + 1):
            if fg < nfg:
                gate_up(fg)
            if fg >= 1:
                down(fg - 1)
                if fg + 1 < nfg:
                    load(fg + 1)

    def rope(self, bk, col0, H, dh, cos, sin, out, out_b, extra_reads=()):
        dve = self.dve
        hd = dh // 2
        n = H * dh
        v = self.bank(bk, n, col0).rearrange("p (h t d) -> p h t d", h=H, t=2, d=hd)
        A = self.ropeA[:, 0:n].rearrange("p (h t d) -> p h t d", h=H, t=2, d=hd)
        Bt = self.ropeB[:, 0:n].rearrange("p (h t d) -> p h t d", h=H, t=2, d=hd)
        cos_b4 = cos.unsqueeze(1).unsqueeze(1).to_broadcast([128, H, 2, hd])
        sin_b3 = sin.unsqueeze(1).to_broadcast([128, H, hd])
        rd = [self.pbuf[bk], self.rope_b] + list(extra_reads)
        dve.op(lambda e: e.tensor_tensor(out=A, in0=v, in1=cos_b4, op=ALU.mult), reads=rd, writes=[self.ropeA_b])
        dve.op(lambda e: e.scalar_tensor_tensor(out=Bt[:, :, 0, :], in0=v[:, :, 1, :], scalar=-1.0, in1=sin_b3,
                                                op0=ALU.mult, op1=ALU.mult), reads=rd, writes=[self.ropeB_b])
        self.rope_ps_tok = dve.op(lambda e: e.tensor_tensor(out=Bt[:, :, 1, :], in0=v[:, :, 0, :], in1=sin_b3, op=ALU.mult),
                                  reads=rd, writes=[self.ropeB_b])
        return dve.op(lambda e: e.tensor_tensor(out=out, in0=self.ropeA[:, 0:n], in1=self.ropeB[:, 0:n], op=ALU.add),
                      reads=[self.ropeA_b, self.ropeB_b], writes=[out_b])

    def phaseB(self):
        pe, act, dve, pool, sp = self.pe, self.act, self.dve, self.pool, self.sp
        self.norm_T(1)
        for lb in range(NB):
            sp.dma(self.d_st, self.xspill[lb], self.xs[:, lb, :], reads=[self.xs_b[lb]], writes=[self.xspill_b[lb]])
        self.barrier()

        def load(cg):
            s = cg % 2
            pool.dma(self.dw[s], self.win[s].rearrange("p a b -> p (a b)"), self.win_d[cg],
                     writes=[self.wg_b[s], self.wu_b[s]])

        fams = ["rv", "rv", "rk", "rk", "rq", "rq", "rg", "rg", "aq", "aq", "akav", "ikiw", "iq", "iq"]
        load(0)
        load(1)
        self.tpc = 0
        self.kvc = 0
        pending = []
        u = 0
        kvrows = self.kv_in.rearrange("(l h d) e -> d l h e", l=NB, h=8, d=128)
        for cg in range(14):
            s = cg % 2
            fam = fams[cg]
            hs = cg % 2 if fam != "akav" and fam != "ikiw" else 0
            if fam in ("rv", "rk", "rq", "rg", "aq", "iq"):
                hs = [c for c in range(14) if fams[c] == fam].index(cg)
            ncols = 144 if fam == "ikiw" else 512
            for lb in range(NB):
                bk = u % 4
                for kc in range(16):
                    pe.op(lambda e, bk=bk, kc=kc, lb=lb, s=s, ncols=ncols: e.matmul(
                        self.bank(bk, ncols), lhsT=self.hT[:, kc, lb * 128:(lb + 1) * 128],
                        rhs=self.win[s][:, kc, 0:ncols], start=(kc == 0), stop=(kc == 15)),
                        reads=[self.hT_b, self.wg_b[s], self.wu_b[s]], writes=[self.pbuf[bk]])
                for fn in pending:
                    fn()
                pending = []
                pending.append(self.postB(fam, hs, lb, bk, u, kvrows))
                u += 1
            if cg + 2 < 14:
                load(cg + 2)
            if cg == 4:
                self.issue_ag(self.kv_in, self.kv_all)
            if cg == 12:
                self.issue_ag(self.kb_in, self.kb_all)
        for fn in pending:
            fn()

    def postB(self, fam, hs, lb, bk, u, kvrows):
        pe, act, dve, sp = self.pe, self.act, self.dve, self.sp
        pb = self.pbuf[bk]
        so = u % 2
        h0 = hs * 4

        def transposes(src_ap, src_b, nblk):
            tb = 4 + (self.tpc % 2)
            self.tpc += 1
            for i in range(nblk):
                pe.op(lambda e, i=i, tb=tb: e.transpose(out=self.bankb(tb, 128, i * 128),
                                                        in_=src_ap[:, i * 128:(i + 1) * 128], identity=self.ident),
                      reads=[src_b, self.cst_b], writes=[self.pbuf[tb]])
            return tb

        if fam == "rv":
            act.op(lambda e: e.activation(out=self.vr[:, lb, hs * 512:(hs + 1) * 512], in_=self.bank(bk), func=AF.Copy),
                   reads=[pb], writes=[self.vr_b])
            return lambda: None
        if fam == "rg":
            act.op(lambda e: e.activation(out=self.sgr[:, lb, hs * 512:(hs + 1) * 512], in_=self.bank(bk), func=AF.Silu),
                   reads=[pb], writes=[self.sgr_b])
            return lambda: None
        if fam in ("rk", "rq", "aq"):
            ob = self.boutb[so]
            self.rope(bk, 0, 4, 128, self.cs128[:, lb, :], self.sn128[:, lb, :], ob, self.bout_b[so])
            if fam == "rk":
                gk_b = self.cstf[:, C_GK + h0:C_GK + h0 + 4].unsqueeze(2).to_broadcast([128, 4, 128])
                dve.op(lambda e: e.tensor_tensor(out=self.aux[so].rearrange("p (h d) -> p h d", h=4, d=128),
                                                 in0=ob.rearrange("p (h d) -> p h d", h=4, d=128), in1=gk_b, op=ALU.mult),
                       reads=[self.bout_b[so], self.cst_b], writes=[self.aux_b[so]])

            def deferred():
                tb = transposes(ob, self.bout_b[so], 4)
                if fam == "rk":
                    dst = self.kT[:, lb, h0:h0 + 4, :].rearrange("p h t -> p (h t)")
                    tab = self.cstf[:, C_GKT + h0 * 128:C_GKT + h0 * 128 + 512]
                    dve.op(lambda e: e.tensor_tensor(out=dst, in0=self.bankb(tb, 512), in1=tab, op=ALU.mult),
                           reads=[self.pbuf[tb], self.cst_b], writes=[self.kT_b])
                    kb = 6 + (self.kvc % 2)
                    self.kvc += 1
                    for h in range(4):
                        pe.op(lambda e, h=h, kb=kb: e.matmul(
                            self.bank(kb, 128, h * 128), lhsT=self.aux[so][:, h * 128:(h + 1) * 128],
                            rhs=self.vr[:, lb, (h0 + h) * 128:(h0 + h + 1) * 128], start=True, stop=True),
                            reads=[self.aux_b[so], self.vr_b], writes=[self.pbuf[kb]])
                    act.op(lambda e, kb=kb: e.activation(out=self.stg[so], in_=self.bank(kb), func=AF.Copy),
                           reads=[self.pbuf[kb]], writes=[self.stg_b[so]])
                    sp.dma(self.d_stg[so], kvrows[:, lb, h0:h0 + 4, :],
                           self.stg[so].rearrange("p (h e) -> p h e", h=4, e=128),
                           reads=[self.stg_b[so]])
                elif fam == "rq":
                    dst = self.qT[:, lb, h0:h0 + 4, :].rearrange("p h t -> p (h t)")
                    tab = self.cstf[:, C_GQT + h0 * 128:C_GQT + h0 * 128 + 512]
                    dve.op(lambda e: e.tensor_tensor(out=dst, in0=self.bankb(tb, 512), in1=tab, op=ALU.mult),
                           reads=[self.pbuf[tb], self.cst_b], writes=[self.qT_b])
                else:
                    dst = self.aqT[:, lb, h0:h0 + 4, :].rearrange("p h t -> p (h t)")
                    act.op(lambda e: e.activation(out=dst, in_=self.bankb(tb, 512), func=AF.Copy),
                           reads=[self.pbuf[tb]], writes=[self.aqT_b])
            return deferred
        if fam == "akav":
            ob = self.boutb[so][:, 0:256]
            self.rope(bk, 0, 2, 128, self.cs128[:, lb, :], self.sn128[:, lb, :], ob, self.bout_b[so])
            avst = self.stgb[so][:, 0:256]
            act.op(lambda e: e.activation(out=avst, in_=self.bank(bk, 256, 256), func=AF.Copy),
                   reads=[pb], writes=[self.stg_b[so]], extra=[self.rope_ps_tok])

            def deferred():
                tb = transposes(ob, self.bout_b[so], 2)
                akst = self.stgb[so][:, 256:512]
                act.op(lambda e: e.activation(out=akst, in_=self.bankb(tb, 256), func=AF.Copy),
                       reads=[self.pbuf[tb]], writes=[self.stg_b[so]])
                sp.dma(self.d_stg[so], self.kb_in[:, lb * 256:(lb + 1) * 256], akst, reads=[self.stg_b[so]])
                sp.dma(self.d_stg[so], self.kb_in[:, 3072 + lb * 256:3072 + (lb + 1) * 256], avst,
                       reads=[self.stg_b[so]])
            return deferred
        if fam == "ikiw":
            ob = self.boutb[so][:, 0:128]
            self.rope(bk, 0, 2, 64, self.cs64[:, lb, :], self.sn64[:, lb, :], ob, self.bout_b[so])
            act.op(lambda e: e.activation(out=self.wabs[:, lb, :], in_=self.bank(bk, 16, 128), func=AF.Abs, scale=1.0 / 32),
                   reads=[pb], writes=[self.wabs_b], extra=[self.rope_ps_tok])
            act.op(lambda e: e.activation(out=self.wsgn[:, lb, :], in_=self.bank(bk, 16, 128), func=AF.Sign),
                   reads=[pb], writes=[self.wabs_b])

            def deferred():
                tb = transposes(ob, self.bout_b[so], 1)
                ikst = self.stgb[so][:, 0:128]
                act.op(lambda e: e.activation(out=ikst, in_=self.bankb(tb, 128), func=AF.Copy),
                       reads=[self.pbuf[tb]], writes=[self.stg_b[so]])
                sp.dma(self.d_stg[so], self.kb_in[:, 2048 + lb * 128:2048 + (lb + 1) * 128], ikst,
                       reads=[self.stg_b[so]])
            return deferred
        if fam == "iq":
            of = self.bout[so]
            self.rope(bk, 0, 8, 64, self.cs64[:, lb, :], self.sn64[:, lb, :], of, self.bout_b[so])
            wb_ = self.wabs[:, lb, hs * 8:(hs + 1) * 8].unsqueeze(2).to_broadcast([128, 8, 64])
            dve.op(lambda e: e.tensor_tensor(out=self.aux[so].rearrange("p (h d) -> p h d", h=8, d=64),
                                             in0=of.rearrange("p (h d) -> p h d", h=8, d=64), in1=wb_, op=ALU.mult),
                   reads=[self.bout_b[so], self.wabs_b], writes=[self.aux_b[so]])

            def deferred():
                tb = transposes(self.aux[so], self.aux_b[so], 4)
                dst = self.iqT[:, lb, hs * 4:(hs + 1) * 4, :].rearrange("p h t -> p (h t)")
                act.op(lambda e: e.activation(out=dst, in_=self.bankb(tb, 512), func=AF.Copy),
                       reads=[self.pbuf[tb]], writes=[self.iqT_b])
            return deferred
        raise ValueError(fam)

    def issue_ag(self, src, dst):
        pool = self.pool
        toks = [Tok(d.sem, d.n, d) for d in self.d_stg if d.n > 0]
        pool.wait(toks)
        self.cc_n += 1
        sem = self.cc_sem
        pool.ops.append(lambda e, src=src, dst=dst: e.collective_compute(
            "AllGather", ALU.bypass, replica_groups=[list(range(8))], ins=[src], outs=[dst]).then_inc(sem, 1))

    def allgather(self):
        assert self.cc_n == 2
        self.barrier()

    def phaseC1(self):
        pe, act, dve, pool, sp = self.pe, self.act, self.dve, self.pool, self.sp
        sp.dma(self.d_g, self.gnr, self.norms_d[4:5, 0:1024].partition_broadcast(128), writes=[self.gnr_b])
        dve.op(lambda e: e.memset(self.snap.rearrange("p a b c -> p (a b c)"), 0.0), writes=[self.snap_b])
        kvsrc = self.kv_all.rearrange("(r l h d) e -> d r l h e", r=8, l=NB, h=8, d=128)
        gC = self.cstf[:, C_GC:C_GC + 1024]
        step = 0
        for b in range(2):
            dve.op(lambda e: e.memset(self.S, 0.0), writes=[self.S_b])
            for n in range(32):
                k, jj = divmod(n, 8)
                s = step % 2
                if n < 31:
                    sp.dma(self.dkv[s], self.kvbuf[s], kvsrc[:, jj, b * 4 + k, :, :], reads=[self.kv_all_b],
                           writes=[self.kvbuf_b[s]])
                sn = self.snap[:, b, k, :]
                dve.op(lambda e, sn=sn, jj=jj: e.scalar_tensor_tensor(
                    out=sn, in0=self.S, scalar=self.selc[:, jj:jj + 1], in1=sn, op0=ALU.mult, op1=ALU.add),
                    reads=[self.S_b, self.cst_b, self.snap_b], writes=[self.snap_b])
                if n < 31:
                    dve.op(lambda e, s=s: e.tensor_tensor(out=self.S, in0=self.S,
                                                          in1=self.kvbuf[s].rearrange("p h e -> p (h e)"), op=ALU.add),
                           reads=[self.S_b, self.kvbuf_b[s]], writes=[self.S_b])
                    dve.op(lambda e: e.tensor_tensor(out=self.S, in0=self.S, in1=gC, op=ALU.mult),
                           reads=[self.S_b, self.cst_b], writes=[self.S_b])
                    step += 1
        def block(lb):
            b, k = divmod(lb, 4)
            for hg in range(2):
                sbk, obk = hg, 2 + hg
                for h in range(4):
                    H = hg * 4 + h
                    pe.op(lambda e, h=h, H=H, sbk=sbk: e.matmul(
                        self.bank(sbk, 128, h * 128), lhsT=self.kT[:, lb, H, :], rhs=self.qT[:, lb, H, :],
                        start=True, stop=True), reads=[self.kT_b, self.qT_b], writes=[self.pbuf[sbk]])
                dve.op(lambda e, sbk=sbk, hg=hg: e.tensor_tensor(out=self.sTm[hg], in0=self.bank(sbk), in1=self.tri4,
                                                                 op=ALU.mult),
                       reads=[self.pbuf[sbk], self.cst_b], writes=[self.sTm_b[hg]])
                for h in range(4):
                    H = hg * 4 + h
                    pe.op(lambda e, h=h, H=H, obk=obk, hg=hg: e.matmul(
                        self.bank(obk, 128, h * 128), lhsT=self.sTm[hg][:, h * 128:(h + 1) * 128],
                        rhs=self.vr[:, lb, H * 128:(H + 1) * 128], start=True, stop=False),
                        reads=[self.sTm_b[hg], self.vr_b], writes=[self.pbuf[obk]])
                    pe.op(lambda e, h=h, H=H, obk=obk: e.matmul(
                        self.bank(obk, 128, h * 128), lhsT=self.qT[:, lb, H, :],
                        rhs=self.snap[:, b, k, H * 128:(H + 1) * 128], start=False, stop=True),
                        reads=[self.qT_b, self.snap_b], writes=[self.pbuf[obk]])
                act.op(lambda e, obk=obk, hg=hg: e.activation(out=self.t1[:, hg * 512:(hg + 1) * 512],
                                                              in_=self.bank(obk), func=AF.Copy),
                       reads=[self.pbuf[obk]], writes=[self.t1_b])
            t1v = self.t1.rearrange("p (h e) -> p h e", h=8, e=128)
            t2v = self.t2.rearrange("p (h e) -> p h e", h=8, e=128)
            s1 = self.stat[:, 64:72]
            s2 = self.stat[:, 72:80]
            dve.op(lambda e: e.tensor_reduce(out=s1, in_=t1v, axis=AX.X, op=ALU.add), reads=[self.t1_b], writes=[self.stat_b])
            dve.op(lambda e: e.tensor_scalar(out=s1, in0=s1, scalar1=-1.0 / 128, scalar2=None, op0=ALU.mult),
                   reads=[self.stat_b], writes=[self.stat_b])
            dve.op(lambda e: e.tensor_tensor(out=t2v, in0=t1v, in1=s1.unsqueeze(2).to_broadcast([128, 8, 128]), op=ALU.add),
                   reads=[self.t1_b, self.stat_b], writes=[self.t2_b])
            dve.op(lambda e: e.tensor_tensor(out=self.t1, in0=self.t2, in1=self.t2, op=ALU.mult),
                   reads=[self.t2_b], writes=[self.t1_b])
            dve.op(lambda e: e.tensor_reduce(out=s2, in_=t1v, axis=AX.X, op=ALU.add), reads=[self.t1_b], writes=[self.stat_b])
            act.op(lambda e: e.activation(out=s2, in_=s2, func=AF.Sqrt, scale=1.0 / 128, bias=self.cstf[:, C_EPS:C_EPS + 1]),
                   reads=[self.stat_b, self.cst_b], writes=[self.stat_b])
            dve.op(lambda e: e.reciprocal(out=s2, in_=s2), reads=[self.stat_b], writes=[self.stat_b])
            dve.op(lambda e: e.tensor_tensor(out=t1v, in0=t2v, in1=s2.unsqueeze(2).to_broadcast([128, 8, 128]), op=ALU.mult),
                   reads=[self.t2_b, self.stat_b], writes=[self.t1_b])
            dve.op(lambda e: e.tensor_tensor(out=self.t2, in0=self.t1, in1=self.gnr, op=ALU.mult),
                   reads=[self.t1_b, self.gnr_b], writes=[self.t2_b])
            dve.op(lambda e: e.tensor_tensor(out=self.ybf, in0=self.t2, in1=self.sgr[:, lb, :], op=ALU.mult),
                   reads=[self.t2_b, self.sgr_b], writes=[self.ybf_b])
            tb = 4 + lb % 2
            for h in range(8):
                pe.op(lambda e, h=h, tb=tb: e.transpose(out=self.bankb(tb, 128, h * 128),
                                                        in_=self.ybf[:, h * 128:(h + 1) * 128], identity=self.ident),
                      reads=[self.ybf_b, self.cst_b], writes=[self.pbuf[tb]])
            dst = self.mixT[:, 0:8, lb * 128:(lb + 1) * 128]
            act.op(lambda e, tb=tb, dst=dst: e.activation(out=dst, in_=self.bankb(tb).rearrange("p (a b) -> p a b", a=8, b=128),
                                                          func=AF.Copy),
                   reads=[self.pbuf[tb]], writes=[self.hT_b])

        for lb in range(NB):
            block(lb)

    def phaseC2(self):
        pe, act, dve, pool, sp = self.pe, self.act, self.dve, self.pool, self.sp
        kb = self.kb_all
        ksrc = kb[:, 0:2048].rearrange("(j p) (l g t) -> p g l j t", p=128, l=NB, g=2, t=128)
        isrc = kb[:, 2048:3072].rearrange("(j p) (l t) -> p l j t", p=128, l=NB, t=128)
        vsrc = kb[:, 3072:5120].rearrange("(j p) (l g d) -> p g l j d", p=128, l=NB, g=2, d=128)
        self.iu = 0
        self.au = 0
        scale = float(128 ** -0.5)
        st = self.stat
        rmax, rmin, w0, mid, cnt, tmp, tau = (st[:, 96:97], st[:, 97:98], st[:, 98:99], st[:, 99:100],
                                              st[:, 100:101], st[:, 101:102], st[:, 102:103])
        wtab = st[:, 128:128 + NIT + 1]
        SB = self.stat_b

        def load_ik(b):
            pi_ = [(self.ikT[:, k * 1024:(k + 1) * 1024].rearrange("p (j t) -> p j t", j=8, t=128),
                    isrc[:, b * 4 + k, :, :]) for k in range(4)]
            sp.dma_group(self.d_ik, pi_, reads=[self.kb_all_b], writes=[self.ikT_b])

        def load_kv(b):
            pk, pv = [], []
            for k in range(4):
                for g in range(2):
                    pk.append((self.KT[:, g, k * 1024:(k + 1) * 1024].rearrange("p (j t) -> p j t", j=8, t=128),
                               ksrc[:, g, b * 4 + k, :, :]))
                    pv.append((self.V[:, k * 8:(k + 1) * 8, g, :], vsrc[:, g, b * 4 + k, :, :]))
            sp.dma_group(self.d_kt, pk, reads=[self.kb_all_b], writes=[self.KT_b])
            sp.dma_group(self.d_v, pv, reads=[self.kb_all_b], writes=[self.V_b])

        def X(lb):
            S = 1024 * (lb % 4 + 1)
            dve.op(lambda e: e.tensor_tensor(
                out=self.dg, in0=self.ident.unsqueeze(1).to_broadcast([128, 16, 128]),
                in1=self.wsgn[:, lb, :].unsqueeze(2).to_broadcast([128, 16, 128]), op=ALU.mult),
                reads=[self.cst_b, self.wabs_b], writes=[self.dg_b])
            units = [(kg, h) for kg in range(S // 512) for h in range(16)]
            prev = None

            def headsum(kg, h, r):
                pe.op(lambda e: e.matmul(self.bank(2), lhsT=self.dg[:, h, :], rhs=self.rbb[r],
                                         start=(h == 0), stop=(h == 15)),
                      reads=[self.dg_b, self.rb_b[r]], writes=[self.pbuf[2]])
                if h == 15:
                    sc = self.score[:, kg * 512:(kg + 1) * 512]
                    dve.op(lambda e: e.tensor_copy(out=sc, in_=self.bank(2)), reads=[self.pbuf[2]], writes=[self.score_b])
            for (kg, h) in units:
                hp, half = divmod(h, 2)
                bk = self.iu % 2
                r = self.iu % 4
                self.iu += 1
                pr = slice(half * 64, (half + 1) * 64)
                pe.op(lambda e, bk=bk, pr=pr, hp=hp, kg=kg: e.matmul(
                    self.bank(bk), lhsT=self.iqT[pr, lb, hp, :], rhs=self.ikT[pr, kg * 512:(kg + 1) * 512],
                    start=True, stop=True), reads=[self.iqT_b, self.ikT_b], writes=[self.pbuf[bk]])
                act.op(lambda e, bk=bk, r=r: e.activation(out=self.rbb[r], in_=self.bank(bk), func=AF.Relu),
                       reads=[self.pbuf[bk]], writes=[self.rb_b[r]])
                if prev is not None:
                    headsum(*prev)
                prev = (kg, h, r)
            headsum(*prev)

        def Y(lb):
            S = 1024 * (lb % 4 + 1)
            m = lb % 2
            scv = self.score[:, 0:S]
            dve.op(lambda e: e.tensor_reduce(out=rmax, in_=scv, axis=AX.X, op=ALU.max), reads=[self.score_b], writes=[SB])
            dve.op(lambda e: e.tensor_reduce(out=rmin, in_=scv, axis=AX.X, op=ALU.min), reads=[self.score_b], writes=[SB])
            dve.op(lambda e: e.tensor_tensor(out=self.score[:, S - 1024:S], in0=self.score[:, S - 1024:S], in1=self.cb,
                                             op=ALU.add), reads=[self.score_b, self.cst_b], writes=[self.score_b])
            dve.op(lambda e: e.tensor_scalar(out=w0, in0=rmax, scalar1=rmin, scalar2=0.5, op0=ALU.subtract, op1=ALU.mult),
                   reads=[SB], writes=[SB])
            dve.op(lambda e: e.tensor_scalar(out=wtab, in0=self.cstf[:, C_POW2:C_POW2 + NIT + 1], scalar1=w0, scalar2=None,
                                             op0=ALU.mult), reads=[SB, self.cst_b], writes=[SB])
            dve.op(lambda e: e.tensor_tensor(out=mid, in0=rmin, in1=wtab[:, 0:1], op=ALU.add), reads=[SB], writes=[SB])
            for i in range(NIT):
                dve.op(lambda e: e.tensor_scalar(out=self.junk[:, 0:S], in0=scv, scalar1=mid, scalar2=0.0,
                                                 op0=ALU.is_ge, op1=ALU.add, accum_out=cnt),
                       reads=[self.score_b, SB], writes=[self.junk_b, SB])
                dve.op(lambda e, i=i: e.tensor_scalar(out=tmp, in0=cnt, scalar1=256.0, scalar2=wtab[:, i:i + 1],
                                                      op0=ALU.is_ge, op1=ALU.mult), reads=[SB], writes=[SB])
                dve.op(lambda e, i=i: e.scalar_tensor_tensor(out=mid, in0=tmp, scalar=wtab[:, i + 1:i + 2], in1=mid,
                                                             op0=ALU.subtract, op1=ALU.add), reads=[SB], writes=[SB])
            dve.op(lambda e: e.tensor_tensor(out=tau, in0=mid, in1=wtab[:, NIT:NIT + 1], op=ALU.subtract),
                   reads=[SB], writes=[SB])
            dve.op(lambda e: e.tensor_scalar(out=self.mb[m][:, 0:S], in0=scv, scalar1=tau, scalar2=NEG_MASK,
                                             op0=ALU.is_lt, op1=ALU.mult), reads=[self.score_b, SB], writes=[self.mb_b[m]])

        def Zmain(lb):
            S = 1024 * (lb % 4 + 1)
            m = lb % 2
            nch = S // 128
            prev = None

            def pv(c, g, ps_):
                pe.op(lambda e: e.matmul(
                    self.bank(4 + g), lhsT=self.V[:, c, g, :], rhs=self.pT[ps_], start=(c == 0), stop=(c == nch - 1)),
                    reads=[self.V_b, self.pT_b[ps_]], writes=[self.pbuf[4 + g]])
                pe.op(lambda e: e.matmul(
                    self.bank(6 + g), lhsT=self.ones, rhs=self.pT[ps_], start=(c == 0), stop=(c == nch - 1)),
                    reads=[self.cst_b, self.pT_b[ps_]], writes=[self.pbuf[6 + g]])
            for c in range(nch):
                for g in range(2):
                    ps_ = self.au % 2
                    self.au += 1
                    pe.op(lambda e, c=c, g=g: e.matmul(
                        self.bank(3), lhsT=self.KT[:, g, c * 128:(c + 1) * 128],
                        rhs=self.aqT[:, lb, g * 4:(g + 1) * 4, :].rearrange("p h t -> p (h t)"), start=True, stop=False),
                        reads=[self.KT_b, self.aqT_b], writes=[self.pbuf[3]])
                    pe.op(lambda e, c=c: e.matmul(
                        self.bank(3), lhsT=self.mb[m][:, c * 128:(c + 1) * 128], rhs=self.ident4, start=False, stop=True),
                        reads=[self.mb_b[m], self.cst_b], writes=[self.pbuf[3]])
                    if prev is not None:
                        pv(*prev)
                    act.op(lambda e, ps_=ps_: e.activation(out=self.pT[ps_], in_=self.bank(3), func=AF.Exp, scale=scale),
                           reads=[self.pbuf[3]], writes=[self.pT_b[ps_]])
                    prev = (c, g, ps_)
            pv(*prev)

        def Zfin(lb):
            for g in range(2):
                dve.op(lambda e, g=g: e.reciprocal(out=self.rcp, in_=self.bank(6 + g)), reads=[self.pbuf[6 + g]],
                       writes=[self.rcp_b])
                dst = self.mixT[:, 8 + g * 4:8 + (g + 1) * 4, lb * 128:(lb + 1) * 128]
                dve.op(lambda e, g=g, dst=dst: e.tensor_tensor(
                    out=dst, in0=self.bank(4 + g).rearrange("p (h t) -> p h t", h=4, t=128),
                    in1=self.rcp.rearrange("p (h t) -> p h t", h=4, t=128), op=ALU.mult),
                    reads=[self.pbuf[4 + g], self.rcp_b], writes=[self.hT_b])

        load_ik(0)
        X(0)
        Y(0)
        for lb in range(1, NB):
            if lb == 4:
                load_ik(1)
            X(lb)
            if lb - 1 in (0, 4):
                load_kv((lb - 1) // 4)
            Zmain(lb - 1)
            Y(lb)
            Zfin(lb - 1)
        Zmain(NB - 1)
        Zfin(NB - 1)

    def phaseD(self):
        pe, dve, pool = self.pe, self.dve, self.pool
        self.barrier()
        self.load_x(self.xspill)
        for cg in range(4):
            s = cg % 2
            pool.dma(self.dw[s], self.win[s].rearrange("p a b -> p (a b)"), self.wout_d[cg],
                     writes=[self.wg_b[s], self.wu_b[s]])
            for lb in range(NB):
                bk = (cg * NB + lb) % 4
                for kc in range(16):
                    pe.op(lambda e, bk=bk, kc=kc, lb=lb, s=s: e.matmul(
                        self.bank(bk), lhsT=self.mixT[:, kc, lb * 128:(lb + 1) * 128], rhs=self.win[s][:, kc, :],
                        start=(kc == 0), stop=(kc == 15)),
                        reads=[self.hT_b, self.wg_b[s], self.wu_b[s]], writes=[self.pbuf[bk]])
                xsl = self.xs[:, lb, cg * 512:(cg + 1) * 512]
                dve.op(lambda e, bk=bk, xsl=xsl: e.tensor_tensor(out=xsl, in0=self.bank(bk), in1=xsl, op=ALU.add),
                       reads=[self.pbuf[bk], self.xs_b[lb]], writes=[self.xs_b[lb]])
        self.barrier()

    def build(self):
        stop = self.stop
        self.phase0()
        self.barrier()
        self.norm_T(0)
        self.ffn(0)

        def finish_reload():
            self.barrier()
            self.load_x(self.xspill)
            self.final_norm()
            self.barrier()
        if stop == "ffn1":
            self.final_norm()
            self.barrier()
            return
        self.phaseB()
        if stop == "B":
            return finish_reload()
        self.allgather()
        if stop == "AG":
            return finish_reload()
        self.phaseC1()
        self.barrier()
        if stop == "C1":
            if self.debug:
                self.barrier()
                self.sp.dma(self.d_st, self.dbg_mix, self.mixT.rearrange("p a b -> p (a b)"), reads=[self.hT_b])
            return finish_reload()
        self.phaseC2()
        if self.debug:
            self.barrier()
            self.sp.dma(self.d_st, self.dbg_mix, self.mixT.rearrange("p a b -> p (a b)"), reads=[self.hT_b])
        if stop == "C2":
            return finish_reload()
        self.phaseD()
        if self.debug:
            for lb in range(NB):
                self.sp.dma(self.d_st, self.dbg_x[lb], self.xs[:, lb, :], reads=[self.xs_b[lb]])
        if stop != "D":
            self.norm_T(2)
            self.ffn(1)
        self.final_norm()
        self.barrier()


_CACHE = {}


def _consts():
    f = np.zeros((128, NF), np.float32)
    f[:, C_INV128:C_INV128 + 64] = (np.float32(10000.0) ** (-(np.arange(0, 128, 2, dtype=np.float32) / np.float32(128)))).astype(np.float32)[None]
    f[:, C_INV64:C_INV64 + 32] = (np.float32(10000.0) ** (-(np.arange(0, 64, 2, dtype=np.float32) / np.float32(64)))).astype(np.float32)[None]
    h = np.arange(8, dtype=np.float64)
    lg = np.log1p(-np.exp2(-5.0 - h))
    p1 = np.arange(1, 129, dtype=np.float64)
    f[:, C_GQ:C_GQ + 8] = np.exp(p1[:, None] * lg[None, :])
    f[:, C_GK:C_GK + 8] = np.exp(-p1[:, None] * lg[None, :]) * 128 ** -0.5
    f[:, C_GQT:C_GQT + 1024] = np.exp(lg[:, None] * p1[None, :]).reshape(1, 1024)
    f[:, C_GKT:C_GKT + 1024] = (np.exp(-lg[:, None] * p1[None, :]) * 128 ** -0.5).reshape(1, 1024)
    f[:, C_GC:C_GC + 1024] = np.repeat(np.exp(128 * lg), 128)[None]
    f[:, C_POW2:C_POW2 + 24] = (2.0 ** -np.arange(24))[None]
    f[:, C_EPS] = 1e-6
    cb = np.zeros((128, NCB), np.float32)
    eye = np.eye(128, dtype=np.float32)
    cb[:, 0:512] = np.tile(eye, (1, 4))
    tri = (np.arange(128)[None, :] >= np.arange(128)[:, None]).astype(np.float32)
    cb[:, 512:1024] = np.tile(tri, (1, 4))
    cb[:, 1024:1152] = 1.0
    return f, cb.astype(ml_dtypes.bfloat16)


def _percore_consts(j):
    c = np.zeros((128, NCC), np.float32)
    qrel = 128 * j + np.arange(128)
    c[:, 0:1024] = np.where(np.arange(1024)[None, :] > qrel[:, None], np.float32(-1e30), np.float32(0.0))
    c[:, 1024 + j] = 1.0
    return c


def _win_layout(w_in):
    O = dict(rq=0, rk=1024, rv=2048, rg=3072, aq=4096, ak=5120, av=5376, iq=5632, ik=6656, iw=6720)
    cols = []

    def rng(a, n):
        return list(range(a, a + n))
    cols += rng(O["rv"], 1024) + rng(O["rk"], 1024) + rng(O["rq"], 1024) + rng(O["rg"], 1024) + rng(O["aq"], 1024)
    cols += rng(O["ak"], 256) + rng(O["av"], 256)
    cols += rng(O["ik"], 64) + rng(O["ik"], 64) + rng(O["iw"], 16) + [-1] * 368
    cols += rng(O["iq"], 1024)
    cols = np.array(cols)
    W = np.zeros((D, 14 * 512), np.float32)
    m = cols >= 0
    W[:, m] = w_in[:, cols[m]]
    return np.ascontiguousarray(W.reshape(16, 128, 14, 512).transpose(2, 1, 0, 3)).reshape(14, 128, 16 * 512)


def _gu_layout(W):
    return np.ascontiguousarray(W.reshape(16, 128, NFG, 256).transpose(2, 1, 0, 3)).reshape(NFG, 128, 16 * 256)


def _wd_layout(W):
    return np.ascontiguousarray(W.reshape(NFG, 2, 128, D).transpose(0, 2, 1, 3)).reshape(NFG, 128, 2 * D)


def _wout_layout(W):
    return np.ascontiguousarray(W.reshape(16, 128, 4, 512).transpose(2, 1, 0, 3)).reshape(4, 128, 16 * 512)


def _chunks(j):
    return [(b, 8 * k + j) for b in range(2) for k in range(4)]


def kernel(x, positions, ffn1_norm, ffn1_w_gate, ffn1_w_up, ffn1_w_down, mix_norm, w_in, ret_norm, w_out,
           ffn2_norm, ffn2_w_gate, ffn2_w_up, ffn2_w_down, final_norm, _stop="full", _debug=False):
    x = np.asarray(x, np.float32)
    positions = np.asarray(positions, np.int32)
    key = (_stop, _debug)
    if key not in _CACHE:
        _CACHE[key] = Prog(_stop, _debug)
    prog = _CACHE[key]
    cf, cbb = _consts()
    norms = np.zeros((5, D), np.float32)
    norms[0] = np.asarray(ffn1_norm)[0]
    norms[1] = np.asarray(mix_norm)[0]
    norms[2] = np.asarray(ffn2_norm)[0]
    norms[3] = np.asarray(final_norm)
    norms[4, :1024] = np.asarray(ret_norm)[0]
    shared = dict(
        norms=norms,
        wg1=_gu_layout(np.asarray(ffn1_w_gate, np.float32)[0]), wu1=_gu_layout(np.asarray(ffn1_w_up, np.float32)[0]),
        wd1=_wd_layout(np.asarray(ffn1_w_down, np.float32)[0]),
        wg2=_gu_layout(np.asarray(ffn2_w_gate, np.float32)[0]), wu2=_gu_layout(np.asarray(ffn2_w_up, np.float32)[0]),
        wd2=_wd_layout(np.asarray(ffn2_w_down, np.float32)[0]),
        win=_win_layout(np.asarray(w_in, np.float32)[0]), wout=_wout_layout(np.asarray(w_out, np.float32)[0]),
        cstf=cf, cstb=cbb,
    )
    in_maps = []
    for j in range(8):
        ch = _chunks(j)
        xm = np.stack([x[b, n * 128:(n + 1) * 128] for (b, n) in ch])
        pm = np.stack([positions[b, n * 128:(n + 1) * 128] for (b, n) in ch], axis=1)
        m = dict(shared)
        m["x"] = np.ascontiguousarray(xm)
        m["pos"] = np.ascontiguousarray(pm.astype(np.int32))
        m["cstc"] = _percore_consts(j)
        in_maps.append(m)
    res = run_bass_kernel_spmd(prog.nc, in_maps, core_ids=list(range(8)))
    if _debug:
        kernel.dbg = [(res.results[j]["dbg_mix"], res.results[j]["dbg_x"]) for j in range(8)]
    out = np.zeros((2, 4096, D), np.float32)
    for j in range(8):
        o = res.results[j]["out"]
        for lb, (b, n) in enumerate(_chunks(j)):
            out[b, n * 128:(n + 1) * 128] = o[lb]
    return out
```

```python
import numpy as np
import ml_dtypes
from contextlib import ExitStack
import concourse.bass as bass
import concourse.mybir as mybir
from concourse.bass_utils import run_bass_kernel_spmd

F32 = mybir.dt.float32
BF16 = mybir.dt.bfloat16
I32 = mybir.dt.int32
ALU = mybir.AluOpType
AF = mybir.ActivationFunctionType
AX = mybir.AxisListType

D = 2048
DFF = 5632
NFG = 22
NB = 8
NIT = 20
NEG_MASK = -30000.0
PI = float(np.pi)

C_INV128, C_INV64, C_GQ, C_GK, C_GQT, C_GKT, C_GC, C_POW2, C_EPS, NF = 0, 64, 96, 104, 112, 1136, 2160, 3184, 3208, 3216
NCC = 1032
NCB = 1152


class Tok:
    __slots__ = ("sem", "v", "q")

    def __init__(self, sem, v, q):
        self.sem = sem
        self.v = v
        self.q = q


class Buf:
    __slots__ = ("name", "w", "r")

    def __init__(self, name=""):
        self.name = name
        self.w = None
        self.r = {}


class DSem:
    def __init__(self, nc, es, name):
        self.sem = es.enter_context(nc.semaphore(name))
        self.n = 0


class Q:
    def __init__(self, nc, es, name, is_pe=False):
        self.name = name
        self.sem = es.enter_context(nc.semaphore("q_" + name))
        self.n = 0
        self.ops = []
        self.waited = {}
        self.is_pe = is_pe

    def wait(self, toks):
        for t in toks:
            if t is None:
                continue
            k = id(t.sem)
            if self.waited.get(k, 0) >= t.v:
                continue
            self.waited[k] = t.v
            self.ops.append(lambda e, t=t: e.wait_ge(t.sem, t.v))

    def _deps(self, reads, writes, dma):
        deps = []
        for b in reads:
            t = b.w
            if t is not None and (dma or not (t.q is self and self.is_pe)):
                deps.append(t)
        for b in writes:
            for t in list(b.r.values()) + [b.w]:
                if t is None:
                    continue
                if (not dma) and t.q is self:
                    continue
                deps.append(t)
        return deps

    @staticmethod
    def _mark(tok, reads, writes):
        k = id(tok.sem)
        for b in reads:
            o = b.r.get(k)
            if o is None or o.v < tok.v:
                b.r[k] = tok
        for b in writes:
            b.w = tok
            b.r = {}

    def op(self, fn, reads=(), writes=(), extra=()):
        self.wait(self._deps(reads, writes, False) + list(extra))
        self.n += 1
        n = self.n
        sem = self.sem
        self.ops.append(lambda e: fn(e).then_inc(sem, 1))
        tok = Tok(sem, n, self)
        self._mark(tok, reads, writes)
        return tok

    def dma(self, dsem, out, in_, reads=(), writes=(), extra=()):
        self.wait(self._deps(reads, writes, True) + list(extra))
        dsem.n += 16
        s = dsem.sem
        self.ops.append(lambda e: e.dma_start(out=out, in_=in_).then_inc(s, 16))
        tok = Tok(s, dsem.n, dsem)
        self._mark(tok, reads, writes)
        return tok

    def dma_group(self, dsem, pairs, reads=(), writes=(), extra=()):
        self.wait(self._deps(reads, writes, True) + list(extra))
        s = dsem.sem
        for (out, in_) in pairs:
            dsem.n += 16
            self.ops.append(lambda e, out=out, in_=in_: e.dma_start(out=out, in_=in_).then_inc(s, 16))
        tok = Tok(s, dsem.n, dsem)
        self._mark(tok, reads, writes)
        return tok

    def replay(self, e):
        for o in self.ops:
            o(e)


def _prod(s):
    r = 1
    for v in s:
        r *= int(v)
    return r


ARENA = 210944


class Prog:
    def __init__(self, stop="full", debug=False):
        self.stop = stop
        self.debug = debug
        nc = self.nc = bass.Bass("TRN2", target_bir_lowering=False, num_devices=8)
        es = self.es = ExitStack()
        dt = nc.dram_tensor

        def din(name, shape, d=F32):
            return dt(name, shape, d, kind="ExternalInput").ap()
        self.x_d = din("x", [NB, 128, D])
        self.pos_d = din("pos", [128, NB], I32)
        self.norms_d = din("norms", [5, D])
        self.wg_d = [din("wg1", [NFG, 128, 16 * 256]), din("wg2", [NFG, 128, 16 * 256])]
        self.wu_d = [din("wu1", [NFG, 128, 16 * 256]), din("wu2", [NFG, 128, 16 * 256])]
        self.wd_d = [din("wd1", [NFG, 128, 2 * D]), din("wd2", [NFG, 128, 2 * D])]
        self.win_d = din("win", [14, 128, 16 * 512])
        self.wout_d = din("wout", [4, 128, 16 * 512])
        self.cstf_d = din("cstf", [128, NF])
        self.cstc_d = din("cstc", [128, NCC])
        self.cstb_d = din("cstb", [128, NCB], BF16)
        self.out_d = dt("out", [NB, 128, D], F32, kind="ExternalOutput").ap()
        if debug:
            self.dbg_mix = dt("dbg_mix", [128, 16384], BF16, kind="ExternalOutput").ap()
            self.dbg_x = dt("dbg_x", [NB, 128, D], F32, kind="ExternalOutput").ap()
        self.xspill = dt("xspill", [NB, 128, D], F32, kind="Internal").ap()
        self.kv_in = dt("kv_in", [NB * 8 * 128, 128], F32, kind="Internal").ap()
        self.kv_all = dt("kv_all", [8 * NB * 8 * 128, 128], F32, kind="Internal", addr_space="Shared").ap()
        self.kb_in = dt("kb_in", [128, 5120], BF16, kind="Internal").ap()
        self.kb_all = dt("kb_all", [1024, 5120], BF16, kind="Internal", addr_space="Shared").ap()

        self.arena = es.enter_context(nc.sbuf_tensor("arena", [128, ARENA // 4], F32))
        self.arena_b = self.arena.bitcast(BF16)
        self.arena_i = self.arena.bitcast(I32)
        self.ps = es.enter_context(nc.psum_tensor("ps", [128, 4096], F32))
        self.psb = self.ps.bitcast(BF16)
        self.pbuf = [Buf("pb%d" % i) for i in range(8)]

        self.pe = Q(nc, es, "pe", True)
        self.act = Q(nc, es, "act")
        self.dve = Q(nc, es, "dve")
        self.pool = Q(nc, es, "pool")
        self.sp = Q(nc, es, "sp")
        self.queues = [self.pe, self.act, self.dve, self.pool, self.sp]
        self.dsems = []
        self.cc_sem = es.enter_context(nc.semaphore("cc"))
        self.cc_n = 0

        self.layout()
        self.build()

        blk = es.enter_context(nc.Block())
        blk.sync(self.sp.replay)
        blk.tensor(self.pe.replay)
        blk.scalar(self.act.replay)
        blk.vector(self.dve.replay)
        blk.gpsimd(self.pool.replay)
        es.close()

    def dsem(self, name):
        d = DSem(self.nc, self.es, name)
        self.dsems.append(d)
        return d

    def carve(self, off, shape, dtype):
        n = _prod(shape[1:])
        if dtype == BF16:
            assert off % 2 == 0
            ap = self.arena_b[:, off // 2: off // 2 + n]
        elif dtype == I32:
            ap = self.arena_i[:, off // 4: off // 4 + n]
        else:
            assert off % 4 == 0
            ap = self.arena[:, off // 4: off // 4 + n]
        if len(shape) == 3:
            ap = ap.rearrange("p (a b) -> p a b", a=shape[1], b=shape[2])
        elif len(shape) == 4:
            ap = ap.rearrange("p (a b c) -> p a b c", a=shape[1], b=shape[2], c=shape[3])
        elif len(shape) == 5:
            ap = ap.rearrange("p (a b c d) -> p a b c d", a=shape[1], b=shape[2], c=shape[3], d=shape[4])
        return ap

    def bank(self, i, n=512, off=0):
        return self.ps[:, i * 512 + off: i * 512 + off + n]

    def bankb(self, i, n=1024, off=0):
        return self.psb[:, i * 1024 + off: i * 1024 + off + n]

    def barrier(self):
        toks = [Tok(q.sem, q.n, q) for q in self.queues if q.n > 0]
        toks += [Tok(d.sem, d.n, d) for d in self.dsems if d.n > 0]
        if self.cc_n:
            toks.append(Tok(self.cc_sem, self.cc_n, None))
        for q in self.queues:
            q.wait(toks)

    def layout(self):
        c = self.carve
        R_X, R_HT, R_W, R_WD, R_GBC, R_T, R_CST = 0, 65536, 98304, 131072, 147456, 155648, 176128
        self.xs = c(R_X, [128, NB, D], F32)
        self.xs_b = [Buf("xs%d" % i) for i in range(NB)]
        self.hT = c(R_HT, [128, 16, 1024], BF16)
        self.hT_b = Buf("hT")
        self.wgu = [c(R_W + s * 16384, [128, 2, 16, 256], BF16) for s in range(2)]
        self.win = [c(R_W + s * 16384, [128, 16, 512], BF16) for s in range(2)]
        self.wg_b = [Buf("wg%d" % s) for s in range(2)]
        self.wu_b = [Buf("wu%d" % s) for s in range(2)]
        self.wdr = [c(R_WD + s * 8192, [128, 2, D], BF16) for s in range(2)]
        self.wd_b = [Buf("wd%d" % s) for s in range(2)]
        self.dw = [self.dsem("dw%d" % s) for s in range(2)]
        self.dwu = [self.dsem("dwu%d" % s) for s in range(2)]
        self.dwd = [self.dsem("dwd%d" % s) for s in range(2)]
        self.gbc = c(R_GBC, [128, D], F32)
        self.gbc_b = Buf("gbc")
        self.aT = [c(R_T + g * 4096, [128, 2, 1024], BF16) for g in range(2)]
        self.aT_b = [Buf("aT%d" % g) for g in range(2)]
        self.sg = [c(R_T + 8192 + s * 2048, [128, 512], F32) for s in range(2)]
        self.sg_b = [Buf("sg%d" % s) for s in range(2)]
        self.hb = [c(R_T + 12288 + s * 4096, [128, D], BF16) for s in range(2)]
        self.hb_b = [Buf("hb%d" % s) for s in range(2)]
        o = R_CST
        self.cstf = c(o, [128, NF], F32); o += NF * 4
        self.cstc = c(o, [128, NCC], F32); o += NCC * 4
        self.cstb = c(o, [128, NCB], BF16); o += NCB * 2
        self.cs128 = c(o, [128, NB, 64], F32); o += 2048
        self.sn128 = c(o, [128, NB, 64], F32); o += 2048
        self.cs64 = c(o, [128, NB, 32], F32); o += 1024
        self.sn64 = c(o, [128, NB, 32], F32); o += 1024
        self.wabs = c(o, [128, NB, 16], F32); o += 512
        self.wsgn = c(o, [128, NB, 16], F32); o += 512
        self.stat = c(o, [128, 256], F32); o += 1024
        self.posi = c(o, [128, NB], I32); o += 32
        self.stg = [c(o + s * 2048, [128, 512], F32) for s in range(2)]
        self.stgb = [c(o + s * 2048, [128, 1024], BF16) for s in range(2)]
        o += 4096
        self.wsgnb = c(o, [128, NB, 16], BF16); o += 256
        assert o <= ARENA, o
        self.cst_b = Buf("cst")
        self.rope_b = Buf("ropetab")
        self.wabs_b = Buf("wabs")
        self.stat_b = Buf("stat")
        self.stg_b = [Buf("stg%d" % s) for s in range(2)]
        self.ident4 = self.cstb[:, 0:512]
        self.ident = self.cstb[:, 0:128]
        self.tri4 = self.cstb[:, 512:1024]
        self.ones = self.cstb[:, 1024:1152]
        self.cb = self.cstc[:, 0:1024]
        self.selc = self.cstc[:, 1024:1032]
        self.qT = c(R_X, [128, NB, 8, 128], BF16)
        self.kT = c(R_X + 16384, [128, NB, 8, 128], BF16)
        self.vr = c(R_X + 32768, [128, NB, 1024], BF16)
        self.sgr = c(R_X + 49152, [128, NB, 1024], BF16)
        self.aqT = c(R_WD, [128, NB, 8, 128], BF16)
        BT = R_GBC
        self.ropeA = c(BT, [128, 512], F32)
        self.ropeB = c(BT + 2048, [128, 512], F32)
        self.bout = [c(BT + 4096 + s * 2048, [128, 512], F32) for s in range(2)]
        self.boutb = [c(BT + 4096 + s * 2048, [128, 512], BF16) for s in range(2)]
        self.aux = [c(BT + 8192 + s * 1024, [128, 512], BF16) for s in range(2)]
        self.iqT = c(BT + 12288, [128, NB, 8, 128], BF16)
        self.qT_b, self.kT_b, self.vr_b, self.sgr_b = Buf("qT"), Buf("kT"), Buf("vr"), Buf("sgr")
        self.aqT_b, self.iqT_b = Buf("aqT"), Buf("iqT")
        self.ropeA_b, self.ropeB_b = Buf("ropeA"), Buf("ropeB")
        self.bout_b = [Buf("bout0"), Buf("bout1")]
        self.aux_b = [Buf("aux0"), Buf("aux1")]
        self.t0a = c(BT, [128, NB, 64], F32)
        self.t0b = c(BT + 2048, [128, NB, 64], F32)
        self.t0i = c(BT + 4096, [128, NB, 64], I32)
        self.t0c = c(BT + 6144, [128, NB, 64], F32)
        self.snap = c(R_W, [128, 2, 4, 1024], BF16)
        self.S = c(R_W + 16384, [128, 1024], F32)
        self.kvbuf = [c(R_W + 20480 + s * 4096, [128, 8, 128], F32) for s in range(2)]
        self.gnr = c(R_W + 28672, [128, 1024], F32)
        self.snap_b, self.S_b, self.gnr_b = Buf("snap"), Buf("S"), Buf("gnr")
        self.kvbuf_b = [Buf("kvb0"), Buf("kvb1")]
        self.dkv = [self.dsem("dkv%d" % s) for s in range(2)]
        self.t1 = c(BT, [128, 1024], F32)
        self.t2 = c(BT + 4096, [128, 1024], F32)
        self.ybf = c(BT + 8192, [128, 1024], BF16)
        self.sTm = [c(BT + 10240 + s * 1024, [128, 512], BF16) for s in range(2)]
        self.t1_b, self.t2_b, self.ybf_b = Buf("t1"), Buf("t2"), Buf("ybf")
        self.sTm_b = [Buf("sTm0"), Buf("sTm1")]
        self.KT = c(R_X, [128, 2, 4096], BF16)
        self.V = c(R_X + 16384, [128, 32, 2, 128], BF16)
        self.ikT = c(R_X + 32768, [128, 4096], BF16)
        self.rb = [c(R_X + 40960 + s * 2048, [128, 512], F32) for s in range(4)]
        self.pT = [c(R_X + 49152 + s * 1024, [128, 512], BF16) for s in range(2)]
        self.rcp = c(R_X + 51200, [128, 512], F32)
        self.KT_b, self.V_b, self.ikT_b = Buf("KT"), Buf("V"), Buf("ikT")
        self.rb_b = [Buf("rb%d" % s) for s in range(4)]
        self.pT_b = [Buf("pT0"), Buf("pT1")]
        self.rcp_b = Buf("rcp")
        self.rbb = [c(R_X + 40960 + s * 2048, [128, 512], BF16) for s in range(4)]
        self.dg = c(R_X + 61440, [128, 16, 128], BF16)
        self.dg_b = Buf("dg")
        self.score = [c(R_W + i * 16384, [128, 4096], F32) for i in range(2)]
        self.mb = [c(R_X + 53248, [128, 4096], BF16), c(BT, [128, 4096], BF16)]
        self.score_b, self.mb_b = [Buf("score0"), Buf("score1")], [Buf("mb0"), Buf("mb1")]
        self.mixT = self.hT
        self.xspill_b = [Buf("xsp%d" % i) for i in range(NB)]
        self.kv_in_b, self.kb_in_b = Buf("kv_in"), Buf("kb_in")
        self.kv_all_b, self.kb_all_b = Buf("kv_all"), Buf("kb_all")
        self.d_ld = self.dsem("d_ld")
        self.d_x = [self.dsem("d_x%d" % i) for i in range(NB)]
        self.d_st = self.dsem("d_st")
        self.d_g = self.dsem("d_g")
        self.d_kt, self.d_v, self.d_ik = self.dsem("d_kt"), self.dsem("d_v"), self.dsem("d_ik")
        self.d_stg = [self.dsem("d_stg%d" % i) for i in range(2)]

    def load_x(self, src):
        for lb in range(NB):
            self.sp.dma(self.d_x[lb], self.xs[:, lb, :], src[lb], writes=[self.xs_b[lb]])

    def sincos(self, ang_fn, shape, out_sin, shift, n):
        dve, act = self.dve, self.act
        tb = self.t0b[:, :, 0:n]
        ti = self.t0i[:, :, 0:n]
        tc_ = self.t0c[:, :, 0:n]
        B1, B2, B3 = self.ropeB_b, self.bout_b[0], self.bout_b[1]
        ang = ang_fn
        dve.op(lambda e: e.tensor_scalar(out=tc_, in0=ang, scalar1=float(shift), scalar2=None, op0=ALU.add), reads=[self.ropeA_b], writes=[B3])
        dve.op(lambda e: e.tensor_scalar(out=tb, in0=tc_, scalar1=float(1.0 / (2 * PI)), scalar2=None, op0=ALU.mult), reads=[B3], writes=[B1])
        dve.op(lambda e: e.tensor_copy(out=ti, in_=tb), reads=[B1], writes=[B2])
        dve.op(lambda e: e.tensor_copy(out=tb, in_=ti), reads=[B2], writes=[B1])
        dve.op(lambda e: e.scalar_tensor_tensor(out=tc_, in0=tb, scalar=float(-2 * PI), in1=tc_, op0=ALU.mult, op1=ALU.add), reads=[B1, B3], writes=[B3])
        dve.op(lambda e: e.tensor_scalar(out=tb, in0=tc_, scalar1=PI, scalar2=float(-2 * PI), op0=ALU.is_gt, op1=ALU.mult), reads=[B3], writes=[B1])
        dve.op(lambda e: e.tensor_tensor(out=tc_, in0=tc_, in1=tb, op=ALU.add), reads=[B1, B3], writes=[B3])
        dve.op(lambda e: e.tensor_scalar(out=tb, in0=tc_, scalar1=-PI, scalar2=float(2 * PI), op0=ALU.is_lt, op1=ALU.mult), reads=[B3], writes=[B1])
        dve.op(lambda e: e.tensor_tensor(out=tc_, in0=tc_, in1=tb, op=ALU.add), reads=[B1, B3], writes=[B3])
        act.op(lambda e: e.activation(out=out_sin, in_=tc_, func=AF.Sin), reads=[B3], writes=[self.rope_b])

    def phase0(self):
        sp, dve = self.sp, self.dve
        sp.dma(self.d_ld, self.cstf, self.cstf_d, writes=[self.cst_b])
        sp.dma(self.d_ld, self.cstc, self.cstc_d, writes=[self.cst_b])
        sp.dma(self.d_ld, self.cstb, self.cstb_d, writes=[self.cst_b])
        sp.dma(self.d_ld, self.posi, self.pos_d, writes=[self.cst_b])
        self.load_x(self.x_d)
        posf = self.stat[:, 0:NB]
        dve.op(lambda e: e.tensor_copy(out=posf, in_=self.posi), reads=[self.cst_b], writes=[self.stat_b])
        for n, inv_off, cs, sn in ((64, C_INV128, self.cs128, self.sn128), (32, C_INV64, self.cs64, self.sn64)):
            ang = self.t0a[:, :, 0:n]
            inv = self.cstf[:, inv_off:inv_off + n]
            dve.op(lambda e, ang=ang, inv=inv, n=n: e.tensor_tensor(
                out=ang, in0=inv.unsqueeze(1).to_broadcast([128, NB, n]),
                in1=posf.unsqueeze(2).to_broadcast([128, NB, n]), op=ALU.mult),
                reads=[self.cst_b, self.stat_b], writes=[self.ropeA_b])
            self.sincos(ang, None, sn, 0.0, n)
            self.sincos(ang, None, cs, PI / 2, n)

    def norm_stats(self, row):
        sp, act, dve = self.sp, self.act, self.dve
        sp.dma(self.d_g, self.gbc, self.norms_d[row:row + 1, :].partition_broadcast(128), writes=[self.gbc_b])
        ss = self.stat[:, 16:16 + NB]
        for lb in range(NB):
            act.op(lambda e, lb=lb: e.activation(out=self.hb[1], in_=self.xs[:, lb, :], func=AF.Square,
                                                 accum_out=ss[:, lb:lb + 1]),
                   reads=[self.xs_b[lb]], writes=[self.hb_b[1], self.stat_b])
        rstd = self.stat[:, 32:32 + NB]
        act.op(lambda e: e.activation(out=rstd, in_=ss, func=AF.Sqrt, scale=1.0 / D,
                                      bias=self.cstf[:, C_EPS:C_EPS + 1]),
               reads=[self.stat_b, self.cst_b], writes=[self.stat_b])
        dve.op(lambda e: e.reciprocal(out=rstd, in_=rstd), reads=[self.stat_b], writes=[self.stat_b])
        return rstd

    def norm_T(self, row):
        pe, act, dve = self.pe, self.act, self.dve
        rstd = self.norm_stats(row)
        for lb in range(NB):
            s = lb % 2
            hb = self.hb[s]
            dve.op(lambda e, lb=lb, hb=hb: e.scalar_tensor_tensor(
                out=hb, in0=self.xs[:, lb, :], scalar=rstd[:, lb:lb + 1], in1=self.gbc,
                op0=ALU.mult, op1=ALU.mult),
                reads=[self.xs_b[lb], self.stat_b, self.gbc_b], writes=[self.hb_b[s]])
            for half in range(2):
                bk = 4 + 2 * s + half
                for i in range(8):
                    kc = half * 8 + i
                    pe.op(lambda e, bk=bk, i=i, kc=kc, hb=hb: e.transpose(
                        out=self.bankb(bk, 128, i * 128), in_=hb[:, kc * 128:(kc + 1) * 128], identity=self.ident),
                        reads=[self.hb_b[s], self.cst_b], writes=[self.pbuf[bk]])
                dst = self.hT[:, half * 8:(half + 1) * 8, lb * 128:(lb + 1) * 128]
                src = self.bankb(bk).rearrange("p (a b) -> p a b", a=8, b=128)
                eng = act if half == 0 else dve
                if eng is act:
                    eng.op(lambda e, dst=dst, src=src: e.activation(out=dst, in_=src, func=AF.Copy),
                           reads=[self.pbuf[bk]], writes=[self.hT_b])
                else:
                    eng.op(lambda e, dst=dst, src=src: e.tensor_copy(out=dst, in_=src),
                           reads=[self.pbuf[bk]], writes=[self.hT_b])

    def final_norm(self):
        dve, sp = self.dve, self.sp
        rstd = self.norm_stats(3)
        last = None
        for lb in range(NB):
            s = lb % 2
            o = self.sg[s]
            dve.op(lambda e, lb=lb: e.scalar_tensor_tensor(
                out=self.xs[:, lb, :], in0=self.xs[:, lb, :], scalar=rstd[:, lb:lb + 1], in1=self.gbc,
                op0=ALU.mult, op1=ALU.mult),
                reads=[self.xs_b[lb], self.stat_b, self.gbc_b], writes=[self.xs_b[lb]])
            last = sp.dma(self.d_st, self.out_d[lb], self.xs[:, lb, :], reads=[self.xs_b[lb]])
        return last

    def ffn(self, idx):
        pe, act, dve, pool = self.pe, self.act, self.dve, self.pool
        wg_d, wu_d, wd_d = self.wg_d[idx], self.wu_d[idx], self.wd_d[idx]

        def load(fg):
            s = fg % 2
            pool.dma(self.dw[s], self.wgu[s][:, 0].rearrange("p a b -> p (a b)"), wg_d[fg], writes=[self.wg_b[s]])
            pool.dma(self.dwu[s], self.wgu[s][:, 1].rearrange("p a b -> p (a b)"), wu_d[fg], writes=[self.wu_b[s]])
            pool.dma(self.dwd[s], self.wdr[s].rearrange("p a b -> p (a b)"), wd_d[fg], writes=[self.wd_b[s]])

        def gate_up(fg):
            s = fg % 2
            for f in range(2):
                for half in range(2):
                    u = f * 2 + half
                    pg, pu = (u % 2) * 2, (u % 2) * 2 + 1
                    rhs_sl = slice(half * 512, (half + 1) * 512)
                    for (bk, wi, wb) in ((pg, 0, self.wg_b[s]), (pu, 1, self.wu_b[s])):
                        for kc in range(16):
                            pe.op(lambda e, bk=bk, wi=wi, kc=kc, f=f, s=s, rhs_sl=rhs_sl: e.matmul(
                                self.bank(bk), lhsT=self.wgu[s][:, wi, kc, f * 128:(f + 1) * 128],
                                rhs=self.hT[:, kc, rhs_sl], start=(kc == 0), stop=(kc == 15)),
                                reads=[wb, self.hT_b], writes=[self.pbuf[bk]])
                    sgs = u % 2
                    act.op(lambda e, pg=pg, sgs=sgs: e.activation(out=self.sg[sgs], in_=self.bank(pg), func=AF.Silu),
                           reads=[self.pbuf[pg]], writes=[self.sg_b[sgs]])
                    dve.op(lambda e, pu=pu, sgs=sgs, fg=fg, f=f, rhs_sl=rhs_sl: e.tensor_tensor(
                        out=self.aT[fg % 2][:, f, rhs_sl], in0=self.sg[sgs], in1=self.bank(pu), op=ALU.mult),
                        reads=[self.sg_b[sgs], self.pbuf[pu]], writes=[self.aT_b[fg % 2]])

        def down(fg):
            s = fg % 2
            for tb in range(NB):
                for ch in range(2):
                    par = (tb * 2 + ch) % 2
                    for cbk in range(2):
                        bk = 4 + par * 2 + cbk
                        for f in range(2):
                            c0 = ch * 1024 + cbk * 512
                            pe.op(lambda e, bk=bk, f=f, tb=tb, c0=c0, fg=fg, s=s: e.matmul(
                                self.bank(bk), lhsT=self.aT[fg % 2][:, f, tb * 128:(tb + 1) * 128],
                                rhs=self.wdr[s][:, f, c0:c0 + 512], start=(f == 0), stop=(f == 1)),
                                reads=[self.aT_b[fg % 2], self.wd_b[s]], writes=[self.pbuf[bk]])
                    b0 = 4 + par * 2
                    xsl = self.xs[:, tb, ch * 1024:(ch + 1) * 1024]
                    dve.op(lambda e, b0=b0, xsl=xsl: e.scalar_tensor_tensor(
                        out=xsl, in0=self.bank(b0, 1024), scalar=0.5, in1=xsl, op0=ALU.mult, op1=ALU.add),
                        reads=[self.pbuf[b0], self.pbuf[b0 + 1], self.xs_b[tb]], writes=[self.xs_b[tb]])

        nfg = NFG
        load(0)
        load(1)
        for fg in range(nfg + 1):
            if fg < nfg:
                gate_up(fg)
            if fg >= 1:
                down(fg - 1)
                if fg + 1 < nfg:
                    load(fg + 1)

    def rope(self, bk, col0, H, dh, cos, sin, out, out_b, extra_reads=()):
        dve = self.dve
        hd = dh // 2
        n = H * dh
        v = self.bank(bk, n, col0).rearrange("p (h t d) -> p h t d", h=H, t=2, d=hd)
        A = self.ropeA[:, 0:n].rearrange("p (h t d) -> p h t d", h=H, t=2, d=hd)
        Bt = self.ropeB[:, 0:n].rearrange("p (h t d) -> p h t d", h=H, t=2, d=hd)
        cos_b4 = cos.unsqueeze(1).unsqueeze(1).to_broadcast([128, H, 2, hd])
        sin_b3 = sin.unsqueeze(1).to_broadcast([128, H, hd])
        rd = [self.pbuf[bk], self.rope_b] + list(extra_reads)
        dve.op(lambda e: e.tensor_tensor(out=A, in0=v, in1=cos_b4, op=ALU.mult), reads=rd, writes=[self.ropeA_b])
        dve.op(lambda e: e.scalar_tensor_tensor(out=Bt[:, :, 0, :], in0=v[:, :, 1, :], scalar=-1.0, in1=sin_b3,
                                                op0=ALU.mult, op1=ALU.mult), reads=rd, writes=[self.ropeB_b])
        self.rope_ps_tok = dve.op(lambda e: e.tensor_tensor(out=Bt[:, :, 1, :], in0=v[:, :, 0, :], in1=sin_b3, op=ALU.mult),
                                  reads=rd, writes=[self.ropeB_b])
        return dve.op(lambda e: e.tensor_tensor(out=out, in0=self.ropeA[:, 0:n], in1=self.ropeB[:, 0:n], op=ALU.add),
                      reads=[self.ropeA_b, self.ropeB_b], writes=[out_b])

    def phaseB(self):
        pe, act, dve, pool, sp = self.pe, self.act, self.dve, self.pool, self.sp
        self.norm_T(1)
        for lb in range(NB):
            sp.dma(self.d_st, self.xspill[lb], self.xs[:, lb, :], reads=[self.xs_b[lb]], writes=[self.xspill_b[lb]])
        self.barrier()

        def load(cg):
            s = cg % 2
            pool.dma(self.dw[s], self.win[s].rearrange("p a b -> p (a b)"), self.win_d[cg],
                     writes=[self.wg_b[s], self.wu_b[s]])

        fams = ["rv", "rv", "rk", "rk", "rq", "rq", "rg", "rg", "aq", "aq", "akav", "ikiw", "iq", "iq"]
        load(0)
        load(1)
        self.tpc = 0
        self.kvc = 0
        pending = []
        u = 0
        kvrows = self.kv_in.rearrange("(l h d) e -> d l h e", l=NB, h=8, d=128)
        for cg in range(14):
            s = cg % 2
            fam = fams[cg]
            hs = cg % 2 if fam != "akav" and fam != "ikiw" else 0
            if fam in ("rv", "rk", "rq", "rg", "aq", "iq"):
                hs = [c for c in range(14) if fams[c] == fam].index(cg)
            ncols = 144 if fam == "ikiw" else 512
            for lb in range(NB):
                bk = u % 4
                for kc in range(16):
                    pe.op(lambda e, bk=bk, kc=kc, lb=lb, s=s, ncols=ncols: e.matmul(
                        self.bank(bk, ncols), lhsT=self.hT[:, kc, lb * 128:(lb + 1) * 128],
                        rhs=self.win[s][:, kc, 0:ncols], start=(kc == 0), stop=(kc == 15)),
                        reads=[self.hT_b, self.wg_b[s], self.wu_b[s]], writes=[self.pbuf[bk]])
                for fn in pending:
                    fn()
                pending = []
                pending.append(self.postB(fam, hs, lb, bk, u, kvrows))
                u += 1
            if cg + 2 < 14:
                load(cg + 2)
            if cg == 4:
                self.issue_ag(self.kv_in, self.kv_all)
            if cg == 12:
                self.issue_ag(self.kb_in, self.kb_all)
        for fn in pending:
            fn()

    def postB(self, fam, hs, lb, bk, u, kvrows):
        pe, act, dve, sp = self.pe, self.act, self.dve, self.sp
        pb = self.pbuf[bk]
        so = u % 2
        h0 = hs * 4

        def transposes(src_ap, src_b, nblk):
            tb = 4 + (self.tpc % 2)
            self.tpc += 1
            for i in range(nblk):
                pe.op(lambda e, i=i, tb=tb: e.transpose(out=self.bankb(tb, 128, i * 128),
                                                        in_=src_ap[:, i * 128:(i + 1) * 128], identity=self.ident),
                      reads=[src_b, self.cst_b], writes=[self.pbuf[tb]])
            return tb

        if fam == "rv":
            act.op(lambda e: e.activation(out=self.vr[:, lb, hs * 512:(hs + 1) * 512], in_=self.bank(bk), func=AF.Copy),
                   reads=[pb], writes=[self.vr_b])
            return lambda: None
        if fam == "rg":
            act.op(lambda e: e.activation(out=self.sgr[:, lb, hs * 512:(hs + 1) * 512], in_=self.bank(bk), func=AF.Silu),
                   reads=[pb], writes=[self.sgr_b])
            return lambda: None
        if fam in ("rk", "rq", "aq"):
            ob = self.boutb[so]
            self.rope(bk, 0, 4, 128, self.cs128[:, lb, :], self.sn128[:, lb, :], ob, self.bout_b[so])
            if fam == "rk":
                gk_b = self.cstf[:, C_GK + h0:C_GK + h0 + 4].unsqueeze(2).to_broadcast([128, 4, 128])
                dve.op(lambda e: e.tensor_tensor(out=self.aux[so].rearrange("p (h d) -> p h d", h=4, d=128),
                                                 in0=ob.rearrange("p (h d) -> p h d", h=4, d=128), in1=gk_b, op=ALU.mult),
                       reads=[self.bout_b[so], self.cst_b], writes=[self.aux_b[so]])

            def deferred():
                tb = transposes(ob, self.bout_b[so], 4)
                if fam == "rk":
                    dst = self.kT[:, lb, h0:h0 + 4, :].rearrange("p h t -> p (h t)")
                    tab = self.cstf[:, C_GKT + h0 * 128:C_GKT + h0 * 128 + 512]
                    dve.op(lambda e: e.tensor_tensor(out=dst, in0=self.bankb(tb, 512), in1=tab, op=ALU.mult),
                           reads=[self.pbuf[tb], self.cst_b], writes=[self.kT_b])
                    kb = 6 + (self.kvc % 2)
                    self.kvc += 1
                    for h in range(4):
                        pe.op(lambda e, h=h, kb=kb: e.matmul(
                            self.bank(kb, 128, h * 128), lhsT=self.aux[so][:, h * 128:(h + 1) * 128],
                            rhs=self.vr[:, lb, (h0 + h) * 128:(h0 + h + 1) * 128], start=True, stop=True),
                            reads=[self.aux_b[so], self.vr_b], writes=[self.pbuf[kb]])
                    act.op(lambda e, kb=kb: e.activation(out=self.stg[so], in_=self.bank(kb), func=AF.Copy),
                           reads=[self.pbuf[kb]], writes=[self.stg_b[so]])
                    sp.dma(self.d_stg[so], kvrows[:, lb, h0:h0 + 4, :],
                           self.stg[so].rearrange("p (h e) -> p h e", h=4, e=128),
                           reads=[self.stg_b[so]])
                elif fam == "rq":
                    dst = self.qT[:, lb, h0:h0 + 4, :].rearrange("p h t -> p (h t)")
                    tab = self.cstf[:, C_GQT + h0 * 128:C_GQT + h0 * 128 + 512]
                    dve.op(lambda e: e.tensor_tensor(out=dst, in0=self.bankb(tb, 512), in1=tab, op=ALU.mult),
                           reads=[self.pbuf[tb], self.cst_b], writes=[self.qT_b])
                else:
                    dst = self.aqT[:, lb, h0:h0 + 4, :].rearrange("p h t -> p (h t)")
                    act.op(lambda e: e.activation(out=dst, in_=self.bankb(tb, 512), func=AF.Copy),
                           reads=[self.pbuf[tb]], writes=[self.aqT_b])
            return deferred
        if fam == "akav":
            ob = self.boutb[so][:, 0:256]
            self.rope(bk, 0, 2, 128, self.cs128[:, lb, :], self.sn128[:, lb, :], ob, self.bout_b[so])
            avst = self.stgb[so][:, 0:256]
            act.op(lambda e: e.activation(out=avst, in_=self.bank(bk, 256, 256), func=AF.Copy),
                   reads=[pb], writes=[self.stg_b[so]], extra=[self.rope_ps_tok])

            def deferred():
                tb = transposes(ob, self.bout_b[so], 2)
                akst = self.stgb[so][:, 256:512]
                act.op(lambda e: e.activation(out=akst, in_=self.bankb(tb, 256), func=AF.Copy),
                       reads=[self.pbuf[tb]], writes=[self.stg_b[so]])
                sp.dma(self.d_stg[so], self.kb_in[:, lb * 256:(lb + 1) * 256], akst, reads=[self.stg_b[so]])
                sp.dma(self.d_stg[so], self.kb_in[:, 3072 + lb * 256:3072 + (lb + 1) * 256], avst,
                       reads=[self.stg_b[so]])
            return deferred
        if fam == "ikiw":
            ob = self.boutb[so][:, 0:128]
            self.rope(bk, 0, 2, 64, self.cs64[:, lb, :], self.sn64[:, lb, :], ob, self.bout_b[so])
            act.op(lambda e: e.activation(out=self.wabs[:, lb, :], in_=self.bank(bk, 16, 128), func=AF.Abs, scale=1.0 / 32),
                   reads=[pb], writes=[self.wabs_b], extra=[self.rope_ps_tok])
            act.op(lambda e: e.activation(out=self.wsgn[:, lb, :], in_=self.bank(bk, 16, 128), func=AF.Sign),
                   reads=[pb], writes=[self.wabs_b])
            act.op(lambda e: e.activation(out=self.wsgnb[:, lb, :], in_=self.bank(bk, 16, 128), func=AF.Sign),
                   reads=[pb], writes=[self.wabs_b])

            def deferred():
                tb = transposes(ob, self.bout_b[so], 1)
                ikst = self.stgb[so][:, 0:128]
                act.op(lambda e: e.activation(out=ikst, in_=self.bankb(tb, 128), func=AF.Copy),
                       reads=[self.pbuf[tb]], writes=[self.stg_b[so]])
                sp.dma(self.d_stg[so], self.kb_in[:, 2048 + lb * 128:2048 + (lb + 1) * 128], ikst,
                       reads=[self.stg_b[so]])
            return deferred
        if fam == "iq":
            of = self.bout[so]
            self.rope(bk, 0, 8, 64, self.cs64[:, lb, :], self.sn64[:, lb, :], of, self.bout_b[so])
            wb_ = self.wabs[:, lb, hs * 8:(hs + 1) * 8].unsqueeze(2).to_broadcast([128, 8, 64])
            dve.op(lambda e: e.tensor_tensor(out=self.aux[so].rearrange("p (h d) -> p h d", h=8, d=64),
                                             in0=of.rearrange("p (h d) -> p h d", h=8, d=64), in1=wb_, op=ALU.mult),
                   reads=[self.bout_b[so], self.wabs_b], writes=[self.aux_b[so]])

            def deferred():
                tb = transposes(self.aux[so], self.aux_b[so], 4)
                dst = self.iqT[:, lb, hs * 4:(hs + 1) * 4, :].rearrange("p h t -> p (h t)")
                act.op(lambda e: e.activation(out=dst, in_=self.bankb(tb, 512), func=AF.Copy),
                       reads=[self.pbuf[tb]], writes=[self.iqT_b])
            return deferred
        raise ValueError(fam)

    def issue_ag(self, src, dst):
        pool = self.pool
        toks = [Tok(d.sem, d.n, d) for d in self.d_stg if d.n > 0]
        pool.wait(toks)
        self.cc_n += 1
        sem = self.cc_sem
        pool.ops.append(lambda e, src=src, dst=dst: e.collective_compute(
            "AllGather", ALU.bypass, replica_groups=[list(range(8))], ins=[src], outs=[dst]).then_inc(sem, 1))

    def allgather(self):
        assert self.cc_n == 2
        self.barrier()

    def phaseC1(self):
        pe, act, dve, pool, sp = self.pe, self.act, self.dve, self.pool, self.sp
        sp.dma(self.d_g, self.gnr, self.norms_d[4:5, 0:1024].partition_broadcast(128), writes=[self.gnr_b])
        dve.op(lambda e: e.memset(self.snap.rearrange("p a b c -> p (a b c)"), 0.0), writes=[self.snap_b])
        kvsrc = self.kv_all.rearrange("(r l h d) e -> d r l h e", r=8, l=NB, h=8, d=128)
        gC = self.cstf[:, C_GC:C_GC + 1024]
        step = 0
        for b in range(2):
            dve.op(lambda e: e.memset(self.S, 0.0), writes=[self.S_b])
            for n in range(32):
                k, jj = divmod(n, 8)
                s = step % 2
                if n < 31:
                    sp.dma(self.dkv[s], self.kvbuf[s], kvsrc[:, jj, b * 4 + k, :, :], reads=[self.kv_all_b],
                           writes=[self.kvbuf_b[s]])
                sn = self.snap[:, b, k, :]
                dve.op(lambda e, sn=sn, jj=jj: e.scalar_tensor_tensor(
                    out=sn, in0=self.S, scalar=self.selc[:, jj:jj + 1], in1=sn, op0=ALU.mult, op1=ALU.add),
                    reads=[self.S_b, self.cst_b, self.snap_b], writes=[self.snap_b])
                if n < 31:
                    dve.op(lambda e, s=s: e.tensor_tensor(out=self.S, in0=self.S,
                                                          in1=self.kvbuf[s].rearrange("p h e -> p (h e)"), op=ALU.add),
                           reads=[self.S_b, self.kvbuf_b[s]], writes=[self.S_b])
                    dve.op(lambda e: e.tensor_tensor(out=self.S, in0=self.S, in1=gC, op=ALU.mult),
                           reads=[self.S_b, self.cst_b], writes=[self.S_b])
                    step += 1
        def block(lb):
            b, k = divmod(lb, 4)
            for hg in range(2):
                sbk, obk = hg, 2 + hg
                for h in range(4):
                    H = hg * 4 + h
                    pe.op(lambda e, h=h, H=H, sbk=sbk: e.matmul(
                        self.bank(sbk, 128, h * 128), lhsT=self.kT[:, lb, H, :], rhs=self.qT[:, lb, H, :],
                        start=True, stop=True), reads=[self.kT_b, self.qT_b], writes=[self.pbuf[sbk]])
                dve.op(lambda e, sbk=sbk, hg=hg: e.tensor_tensor(out=self.sTm[hg], in0=self.bank(sbk), in1=self.tri4,
                                                                 op=ALU.mult),
                       reads=[self.pbuf[sbk], self.cst_b], writes=[self.sTm_b[hg]])
                for h in range(4):
                    H = hg * 4 + h
                    pe.op(lambda e, h=h, H=H, obk=obk, hg=hg: e.matmul(
                        self.bank(obk, 128, h * 128), lhsT=self.sTm[hg][:, h * 128:(h + 1) * 128],
                        rhs=self.vr[:, lb, H * 128:(H + 1) * 128], start=True, stop=False),
                        reads=[self.sTm_b[hg], self.vr_b], writes=[self.pbuf[obk]])
                    pe.op(lambda e, h=h, H=H, obk=obk: e.matmul(
                        self.bank(obk, 128, h * 128), lhsT=self.qT[:, lb, H, :],
                        rhs=self.snap[:, b, k, H * 128:(H + 1) * 128], start=False, stop=True),
                        reads=[self.qT_b, self.snap_b], writes=[self.pbuf[obk]])
                act.op(lambda e, obk=obk, hg=hg: e.activation(out=self.t1[:, hg * 512:(hg + 1) * 512],
                                                              in_=self.bank(obk), func=AF.Copy),
                       reads=[self.pbuf[obk]], writes=[self.t1_b])
            t1v = self.t1.rearrange("p (h e) -> p h e", h=8, e=128)
            t2v = self.t2.rearrange("p (h e) -> p h e", h=8, e=128)
            s1 = self.stat[:, 64:72]
            s2 = self.stat[:, 72:80]
            dve.op(lambda e: e.tensor_reduce(out=s1, in_=t1v, axis=AX.X, op=ALU.add), reads=[self.t1_b], writes=[self.stat_b])
            dve.op(lambda e: e.tensor_scalar(out=s1, in0=s1, scalar1=-1.0 / 128, scalar2=None, op0=ALU.mult),
                   reads=[self.stat_b], writes=[self.stat_b])
            dve.op(lambda e: e.tensor_tensor(out=t2v, in0=t1v, in1=s1.unsqueeze(2).to_broadcast([128, 8, 128]), op=ALU.add),
                   reads=[self.t1_b, self.stat_b], writes=[self.t2_b])
            dve.op(lambda e: e.tensor_tensor(out=self.t1, in0=self.t2, in1=self.t2, op=ALU.mult),
                   reads=[self.t2_b], writes=[self.t1_b])
            dve.op(lambda e: e.tensor_reduce(out=s2, in_=t1v, axis=AX.X, op=ALU.add), reads=[self.t1_b], writes=[self.stat_b])
            act.op(lambda e: e.activation(out=s2, in_=s2, func=AF.Sqrt, scale=1.0 / 128, bias=self.cstf[:, C_EPS:C_EPS + 1]),
                   reads=[self.stat_b, self.cst_b], writes=[self.stat_b])
            dve.op(lambda e: e.reciprocal(out=s2, in_=s2), reads=[self.stat_b], writes=[self.stat_b])
            dve.op(lambda e: e.tensor_tensor(out=t1v, in0=t2v, in1=s2.unsqueeze(2).to_broadcast([128, 8, 128]), op=ALU.mult),
                   reads=[self.t2_b, self.stat_b], writes=[self.t1_b])
            dve.op(lambda e: e.tensor_tensor(out=self.t2, in0=self.t1, in1=self.gnr, op=ALU.mult),
                   reads=[self.t1_b, self.gnr_b], writes=[self.t2_b])
            dve.op(lambda e: e.tensor_tensor(out=self.ybf, in0=self.t2, in1=self.sgr[:, lb, :], op=ALU.mult),
                   reads=[self.t2_b, self.sgr_b], writes=[self.ybf_b])
            tb = 4 + lb % 2
            for h in range(8):
                pe.op(lambda e, h=h, tb=tb: e.transpose(out=self.bankb(tb, 128, h * 128),
                                                        in_=self.ybf[:, h * 128:(h + 1) * 128], identity=self.ident),
                      reads=[self.ybf_b, self.cst_b], writes=[self.pbuf[tb]])
            dst = self.mixT[:, 0:8, lb * 128:(lb + 1) * 128]
            act.op(lambda e, tb=tb, dst=dst: e.activation(out=dst, in_=self.bankb(tb).rearrange("p (a b) -> p a b", a=8, b=128),
                                                          func=AF.Copy),
                   reads=[self.pbuf[tb]], writes=[self.hT_b])

        for lb in range(NB):
            block(lb)

    def phaseC2(self):
        pe, act, dve, pool, sp = self.pe, self.act, self.dve, self.pool, self.sp
        kb = self.kb_all
        ksrc = kb[:, 0:2048].rearrange("(j p) (l g t) -> p g l j t", p=128, l=NB, g=2, t=128)
        isrc = kb[:, 2048:3072].rearrange("(j p) (l t) -> p l j t", p=128, l=NB, t=128)
        vsrc = kb[:, 3072:5120].rearrange("(j p) (l g d) -> p g l j d", p=128, l=NB, g=2, d=128)
        self.iu = 0
        self.au = 0
        scale = float(128 ** -0.5)
        st = self.stat
        rmax, rmin, w0, mid, cnt, tmp, tau = (st[:, 96:97], st[:, 97:98], st[:, 98:99], st[:, 99:100],
                                              st[:, 100:101], st[:, 101:102], st[:, 102:103])
        wtab = st[:, 128:128 + NIT + 1]
        SB = self.stat_b

        def load_ik(b):
            pi_ = [(self.ikT[:, k * 1024:(k + 1) * 1024].rearrange("p (j t) -> p j t", j=8, t=128),
                    isrc[:, b * 4 + k, :, :]) for k in range(4)]
            sp.dma_group(self.d_ik, pi_, reads=[self.kb_all_b], writes=[self.ikT_b])

        def load_kv(b):
            pk, pv = [], []
            for k in range(4):
                for g in range(2):
                    pk.append((self.KT[:, g, k * 1024:(k + 1) * 1024].rearrange("p (j t) -> p j t", j=8, t=128),
                               ksrc[:, g, b * 4 + k, :, :]))
                    pv.append((self.V[:, k * 8:(k + 1) * 8, g, :], vsrc[:, g, b * 4 + k, :, :]))
            sp.dma_group(self.d_kt, pk, reads=[self.kb_all_b], writes=[self.KT_b])
            sp.dma_group(self.d_v, pv, reads=[self.kb_all_b], writes=[self.V_b])

        def X(lb):
            S = 1024 * (lb % 4 + 1)
            pool.op(lambda e: e.tensor_tensor(
                out=self.dg, in0=self.ident.unsqueeze(1).to_broadcast([128, 16, 128]),
                in1=self.wsgnb[:, lb, :].unsqueeze(2).to_broadcast([128, 16, 128]), op=ALU.mult),
                reads=[self.cst_b, self.wabs_b], writes=[self.dg_b])
            units = [(kg, h) for kg in range(S // 512) for h in range(16)]
            prev = None

            def headsum(kg, h, r):
                pe.op(lambda e: e.matmul(self.bank(2), lhsT=self.dg[:, h, :], rhs=self.rbb[r],
                                         start=(h == 0), stop=(h == 15)),
                      reads=[self.dg_b, self.rb_b[r]], writes=[self.pbuf[2]])
                if h == 15:
                    sc = self.score[lb % 2][:, kg * 512:(kg + 1) * 512]
                    act.op(lambda e: e.activation(out=sc, in_=self.bank(2), func=AF.Copy), reads=[self.pbuf[2]],
                           writes=[self.score_b[lb % 2]])
            for (kg, h) in units:
                hp, half = divmod(h, 2)
                bk = self.iu % 2
                r = self.iu % 4
                self.iu += 1
                pr = slice(half * 64, (half + 1) * 64)
                pe.op(lambda e, bk=bk, pr=pr, hp=hp, kg=kg: e.matmul(
                    self.bank(bk), lhsT=self.iqT[pr, lb, hp, :], rhs=self.ikT[pr, kg * 512:(kg + 1) * 512],
                    start=True, stop=True), reads=[self.iqT_b, self.ikT_b], writes=[self.pbuf[bk]])
                act.op(lambda e, bk=bk, r=r: e.activation(out=self.rbb[r], in_=self.bank(bk), func=AF.Relu),
                       reads=[self.pbuf[bk]], writes=[self.rb_b[r]])
                if prev is not None:
                    headsum(*prev)
                prev = (kg, h, r)
            headsum(*prev)

        def Y(lb):
            S = 1024 * (lb % 4 + 1)
            m = lb % 2
            scb = self.score_b[m]
            scv = self.score[m][:, 0:S]
            dve.op(lambda e: e.tensor_reduce(out=rmax, in_=scv, axis=AX.X, op=ALU.max), reads=[scb], writes=[SB])
            dve.op(lambda e: e.tensor_reduce(out=rmin, in_=scv, axis=AX.X, op=ALU.min), reads=[scb], writes=[SB])
            dve.op(lambda e: e.tensor_tensor(out=self.score[m][:, S - 1024:S], in0=self.score[m][:, S - 1024:S], in1=self.cb,
                                             op=ALU.add), reads=[scb, self.cst_b], writes=[scb])
            dve.op(lambda e: e.tensor_scalar(out=w0, in0=rmax, scalar1=rmin, scalar2=0.5, op0=ALU.subtract, op1=ALU.mult),
                   reads=[SB], writes=[SB])
            dve.op(lambda e: e.tensor_scalar(out=wtab, in0=self.cstf[:, C_POW2:C_POW2 + NIT + 1], scalar1=w0, scalar2=None,
                                             op0=ALU.mult), reads=[SB, self.cst_b], writes=[SB])
            dve.op(lambda e: e.tensor_tensor(out=mid, in0=rmin, in1=wtab[:, 0:1], op=ALU.add), reads=[SB], writes=[SB])
            for i in range(NIT):
                dve.op(lambda e: e.tensor_scalar(out=self.mb[m][:, 0:S], in0=scv, scalar1=mid, scalar2=0.0,
                                                 op0=ALU.is_ge, op1=ALU.add, accum_out=cnt),
                       reads=[scb, SB], writes=[self.mb_b[m], SB])
                dve.op(lambda e, i=i: e.tensor_scalar(out=tmp, in0=cnt, scalar1=256.0, scalar2=wtab[:, i:i + 1],
                                                      op0=ALU.is_ge, op1=ALU.mult), reads=[SB], writes=[SB])
                dve.op(lambda e, i=i: e.scalar_tensor_tensor(out=mid, in0=tmp, scalar=wtab[:, i + 1:i + 2], in1=mid,
                                                             op0=ALU.subtract, op1=ALU.add), reads=[SB], writes=[SB])
            dve.op(lambda e: e.tensor_tensor(out=tau, in0=mid, in1=wtab[:, NIT:NIT + 1], op=ALU.subtract),
                   reads=[SB], writes=[SB])
            dve.op(lambda e: e.tensor_scalar(out=self.mb[m][:, 0:S], in0=scv, scalar1=tau, scalar2=NEG_MASK,
                                             op0=ALU.is_lt, op1=ALU.mult), reads=[scb, SB], writes=[self.mb_b[m]])

        def Zmain(lb):
            S = 1024 * (lb % 4 + 1)
            m = lb % 2
            nch = S // 128
            prev = None

            def pv(c, g, ps_):
                pe.op(lambda e: e.matmul(
                    self.bank(4 + g), lhsT=self.V[:, c, g, :], rhs=self.pT[ps_], start=(c == 0), stop=(c == nch - 1)),
                    reads=[self.V_b, self.pT_b[ps_]], writes=[self.pbuf[4 + g]])
                pe.op(lambda e: e.matmul(
                    self.bank(6 + g), lhsT=self.ones, rhs=self.pT[ps_], start=(c == 0), stop=(c == nch - 1)),
                    reads=[self.cst_b, self.pT_b[ps_]], writes=[self.pbuf[6 + g]])
            for c in range(nch):
                for g in range(2):
                    ps_ = self.au % 2
                    self.au += 1
                    pe.op(lambda e, c=c, g=g: e.matmul(
                        self.bank(3), lhsT=self.KT[:, g, c * 128:(c + 1) * 128],
                        rhs=self.aqT[:, lb, g * 4:(g + 1) * 4, :].rearrange("p h t -> p (h t)"), start=True, stop=False),
                        reads=[self.KT_b, self.aqT_b], writes=[self.pbuf[3]])
                    pe.op(lambda e, c=c: e.matmul(
                        self.bank(3), lhsT=self.mb[m][:, c * 128:(c + 1) * 128], rhs=self.ident4, start=False, stop=True),
                        reads=[self.mb_b[m], self.cst_b], writes=[self.pbuf[3]])
                    if prev is not None:
                        pv(*prev)
                    act.op(lambda e, ps_=ps_: e.activation(out=self.pT[ps_], in_=self.bank(3), func=AF.Exp, scale=scale),
                           reads=[self.pbuf[3]], writes=[self.pT_b[ps_]])
                    prev = (c, g, ps_)
            pv(*prev)

        def Zfin(lb):
            for g in range(2):
                dve.op(lambda e, g=g: e.reciprocal(out=self.rcp, in_=self.bank(6 + g)), reads=[self.pbuf[6 + g]],
                       writes=[self.rcp_b])
                dst = self.mixT[:, 8 + g * 4:8 + (g + 1) * 4, lb * 128:(lb + 1) * 128]
                dve.op(lambda e, g=g, dst=dst: e.tensor_tensor(
                    out=dst, in0=self.bank(4 + g).rearrange("p (h t) -> p h t", h=4, t=128),
                    in1=self.rcp.rearrange("p (h t) -> p h t", h=4, t=128), op=ALU.mult),
                    reads=[self.pbuf[4 + g], self.rcp_b], writes=[self.hT_b])

        load_ik(0)
        X(0)
        Y(0)
        for lb in range(1, NB):
            if lb == 4:
                load_ik(1)
            X(lb)
            if lb - 1 in (0, 4):
                load_kv((lb - 1) // 4)
            Zmain(lb - 1)
            Y(lb)
            Zfin(lb - 1)
        Zmain(NB - 1)
        Zfin(NB - 1)

    def phaseD(self):
        pe, dve, pool = self.pe, self.dve, self.pool
        self.barrier()
        self.load_x(self.xspill)
        for cg in range(4):
            s = cg % 2
            pool.dma(self.dw[s], self.win[s].rearrange("p a b -> p (a b)"), self.wout_d[cg],
                     writes=[self.wg_b[s], self.wu_b[s]])
            for lb in range(NB):
                bk = (cg * NB + lb) % 4
                for kc in range(16):
                    pe.op(lambda e, bk=bk, kc=kc, lb=lb, s=s: e.matmul(
                        self.bank(bk), lhsT=self.mixT[:, kc, lb * 128:(lb + 1) * 128], rhs=self.win[s][:, kc, :],
                        start=(kc == 0), stop=(kc == 15)),
                        reads=[self.hT_b, self.wg_b[s], self.wu_b[s]], writes=[self.pbuf[bk]])
                xsl = self.xs[:, lb, cg * 512:(cg + 1) * 512]
                dve.op(lambda e, bk=bk, xsl=xsl: e.tensor_tensor(out=xsl, in0=self.bank(bk), in1=xsl, op=ALU.add),
                       reads=[self.pbuf[bk], self.xs_b[lb]], writes=[self.xs_b[lb]])
        self.barrier()

    def build(self):
        stop = self.stop
        self.phase0()
        self.barrier()
        self.norm_T(0)
        self.ffn(0)

        def finish_reload():
            self.barrier()
            self.load_x(self.xspill)
            self.final_norm()
            self.barrier()
        if stop == "ffn1":
            self.final_norm()
            self.barrier()
            return
        self.phaseB()
        if stop == "B":
            return finish_reload()
        self.allgather()
        if stop == "AG":
            return finish_reload()
        self.phaseC1()
        self.barrier()
        if stop == "C1":
            if self.debug:
                self.barrier()
                self.sp.dma(self.d_st, self.dbg_mix, self.mixT.rearrange("p a b -> p (a b)"), reads=[self.hT_b])
            return finish_reload()
        self.phaseC2()
        if self.debug:
            self.barrier()
            self.sp.dma(self.d_st, self.dbg_mix, self.mixT.rearrange("p a b -> p (a b)"), reads=[self.hT_b])
        if stop == "C2":
            return finish_reload()
        self.phaseD()
        if self.debug:
            for lb in range(NB):
                self.sp.dma(self.d_st, self.dbg_x[lb], self.xs[:, lb, :], reads=[self.xs_b[lb]])
        if stop != "D":
            self.norm_T(2)
            self.ffn(1)
        self.final_norm()
        self.barrier()


_CACHE = {}


def _consts():
    f = np.zeros((128, NF), np.float32)
    f[:, C_INV128:C_INV128 + 64] = (np.float32(10000.0) ** (-(np.arange(0, 128, 2, dtype=np.float32) / np.float32(128)))).astype(np.float32)[None]
    f[:, C_INV64:C_INV64 + 32] = (np.float32(10000.0) ** (-(np.arange(0, 64, 2, dtype=np.float32) / np.float32(64)))).astype(np.float32)[None]
    h = np.arange(8, dtype=np.float64)
    lg = np.log1p(-np.exp2(-5.0 - h))
    p1 = np.arange(1, 129, dtype=np.float64)
    f[:, C_GQ:C_GQ + 8] = np.exp(p1[:, None] * lg[None, :])
    f[:, C_GK:C_GK + 8] = np.exp(-p1[:, None] * lg[None, :]) * 128 ** -0.5
    f[:, C_GQT:C_GQT + 1024] = np.exp(lg[:, None] * p1[None, :]).reshape(1, 1024)
    f[:, C_GKT:C_GKT + 1024] = (np.exp(-lg[:, None] * p1[None, :]) * 128 ** -0.5).reshape(1, 1024)
    f[:, C_GC:C_GC + 1024] = np.repeat(np.exp(128 * lg), 128)[None]
    f[:, C_POW2:C_POW2 + 24] = (2.0 ** -np.arange(24))[None]
    f[:, C_EPS] = 1e-6
    cb = np.zeros((128, NCB), np.float32)
    eye = np.eye(128, dtype=np.float32)
    cb[:, 0:512] = np.tile(eye, (1, 4))
    tri = (np.arange(128)[None, :] >= np.arange(128)[:, None]).astype(np.float32)
    cb[:, 512:1024] = np.tile(tri, (1, 4))
    cb[:, 1024:1152] = 1.0
    return f, cb.astype(ml_dtypes.bfloat16)


def _percore_consts(j):
    c = np.zeros((128, NCC), np.float32)
    qrel = 128 * j + np.arange(128)
    c[:, 0:1024] = np.where(np.arange(1024)[None, :] > qrel[:, None], np.float32(-1e30), np.float32(0.0))
    c[:, 1024 + j] = 1.0
    return c


def _win_layout(w_in):
    O = dict(rq=0, rk=1024, rv=2048, rg=3072, aq=4096, ak=5120, av=5376, iq=5632, ik=6656, iw=6720)
    cols = []

    def rng(a, n):
        return list(range(a, a + n))
    cols += rng(O["rv"], 1024) + rng(O["rk"], 1024) + rng(O["rq"], 1024) + rng(O["rg"], 1024) + rng(O["aq"], 1024)
    cols += rng(O["ak"], 256) + rng(O["av"], 256)
    cols += rng(O["ik"], 64) + rng(O["ik"], 64) + rng(O["iw"], 16) + [-1] * 368
    cols += rng(O["iq"], 1024)
    cols = np.array(cols)
    W = np.zeros((D, 14 * 512), np.float32)
    m = cols >= 0
    W[:, m] = w_in[:, cols[m]]
    return np.ascontiguousarray(W.reshape(16, 128, 14, 512).transpose(2, 1, 0, 3)).reshape(14, 128, 16 * 512)


def _gu_layout(W):
    return np.ascontiguousarray(W.reshape(16, 128, NFG, 256).transpose(2, 1, 0, 3)).reshape(NFG, 128, 16 * 256)


def _wd_layout(W):
    return np.ascontiguousarray(W.reshape(NFG, 2, 128, D).transpose(0, 2, 1, 3)).reshape(NFG, 128, 2 * D)


def _wout_layout(W):
    return np.ascontiguousarray(W.reshape(16, 128, 4, 512).transpose(2, 1, 0, 3)).reshape(4, 128, 16 * 512)


def _chunks(j):
    return [(b, 8 * k + j) for b in range(2) for k in range(4)]


def kernel(x, positions, ffn1_norm, ffn1_w_gate, ffn1_w_up, ffn1_w_down, mix_norm, w_in, ret_norm, w_out,
           ffn2_norm, ffn2_w_gate, ffn2_w_up, ffn2_w_down, final_norm, _stop="full", _debug=False):
    x = np.asarray(x, np.float32)
    positions = np.asarray(positions, np.int32)
    key = (_stop, _debug)
    if key not in _CACHE:
        _CACHE[key] = Prog(_stop, _debug)
    prog = _CACHE[key]
    cf, cbb = _consts()
    norms = np.zeros((5, D), np.float32)
    norms[0] = np.asarray(ffn1_norm)[0]
    norms[1] = np.asarray(mix_norm)[0]
    norms[2] = np.asarray(ffn2_norm)[0]
    norms[3] = np.asarray(final_norm)
    norms[4, :1024] = np.asarray(ret_norm)[0]
    shared = dict(
        norms=norms,
        wg1=_gu_layout(np.asarray(ffn1_w_gate, np.float32)[0]), wu1=_gu_layout(np.asarray(ffn1_w_up, np.float32)[0]),
        wd1=_wd_layout(np.asarray(ffn1_w_down, np.float32)[0]),
        wg2=_gu_layout(np.asarray(ffn2_w_gate, np.float32)[0]), wu2=_gu_layout(np.asarray(ffn2_w_up, np.float32)[0]),
        wd2=_wd_layout(np.asarray(ffn2_w_down, np.float32)[0]),
        win=_win_layout(np.asarray(w_in, np.float32)[0]), wout=_wout_layout(np.asarray(w_out, np.float32)[0]),
        cstf=cf, cstb=cbb,
    )
    in_maps = []
    for j in range(8):
        ch = _chunks(j)
        xm = np.stack([x[b, n * 128:(n + 1) * 128] for (b, n) in ch])
        pm = np.stack([positions[b, n * 128:(n + 1) * 128] for (b, n) in ch], axis=1)
        m = dict(shared)
        m["x"] = np.ascontiguousarray(xm)
        m["pos"] = np.ascontiguousarray(pm.astype(np.int32))
        m["cstc"] = _percore_consts(j)
        in_maps.append(m)
    res = run_bass_kernel_spmd(prog.nc, in_maps, core_ids=list(range(8)))
    if _debug:
        kernel.dbg = [(res.results[j]["dbg_mix"], res.results[j]["dbg_x"]) for j in range(8)]
    out = np.zeros((2, 4096, D), np.float32)
    for j in range(8):
        o = res.results[j]["out"]
        for lb, (b, n) in enumerate(_chunks(j)):
            out[b, n * 128:(n + 1) * 128] = o[lb]
    return out
```

```python
import numpy as np
import ml_dtypes
from contextlib import ExitStack
import concourse.bass as bass
import concourse.mybir as mybir
from concourse.bass_utils import run_bass_kernel_spmd

F32 = mybir.dt.float32
BF16 = mybir.dt.bfloat16
I32 = mybir.dt.int32
ALU = mybir.AluOpType
AF = mybir.ActivationFunctionType
AX = mybir.AxisListType

D = 2048
DFF = 5632
NFG = 22
NB = 8
NIT = 20
NEG_MASK = -30000.0
PI = float(np.pi)

C_INV128, C_INV64, C_GQ, C_GK, C_GQT, C_GKT, C_GC, C_POW2, C_EPS, NF = 0, 64, 96, 104, 112, 1136, 2160, 3184, 3208, 3216
NCC = 1032
NCB = 1152


class Tok:
    __slots__ = ("sem", "v", "q")

    def __init__(self, sem, v, q):
        self.sem = sem
        self.v = v
        self.q = q


class Buf:
    __slots__ = ("name", "w", "r")

    def __init__(self, name=""):
        self.name = name
        self.w = None
        self.r = {}


class DSem:
    def __init__(self, nc, es, name):
        self.sem = es.enter_context(nc.semaphore(name))
        self.n = 0


class Q:
    def __init__(self, nc, es, name, is_pe=False):
        self.name = name
        self.sem = es.enter_context(nc.semaphore("q_" + name))
        self.n = 0
        self.ops = []
        self.waited = {}
        self.is_pe = is_pe

    def wait(self, toks):
        for t in toks:
            if t is None:
                continue
            k = id(t.sem)
            if self.waited.get(k, 0) >= t.v:
                continue
            self.waited[k] = t.v
            self.ops.append(lambda e, t=t: e.wait_ge(t.sem, t.v))

    def _deps(self, reads, writes, dma):
        deps = []
        for b in reads:
            t = b.w
            if t is not None and (dma or not (t.q is self and self.is_pe)):
                deps.append(t)
        for b in writes:
            for t in list(b.r.values()) + [b.w]:
                if t is None:
                    continue
                if (not dma) and t.q is self:
                    continue
                deps.append(t)
        return deps

    @staticmethod
    def _mark(tok, reads, writes):
        k = id(tok.sem)
        for b in reads:
            o = b.r.get(k)
            if o is None or o.v < tok.v:
                b.r[k] = tok
        for b in writes:
            b.w = tok
            b.r = {}

    def op(self, fn, reads=(), writes=(), extra=()):
        self.wait(self._deps(reads, writes, False) + list(extra))
        self.n += 1
        n = self.n
        sem = self.sem
        self.ops.append(lambda e: fn(e).then_inc(sem, 1))
        tok = Tok(sem, n, self)
        self._mark(tok, reads, writes)
        return tok

    def dma(self, dsem, out, in_, reads=(), writes=(), extra=()):
        self.wait(self._deps(reads, writes, True) + list(extra))
        dsem.n += 16
        s = dsem.sem
        self.ops.append(lambda e: e.dma_start(out=out, in_=in_).then_inc(s, 16))
        tok = Tok(s, dsem.n, dsem)
        self._mark(tok, reads, writes)
        return tok

    def dma_group(self, dsem, pairs, reads=(), writes=(), extra=()):
        self.wait(self._deps(reads, writes, True) + list(extra))
        s = dsem.sem
        for (out, in_) in pairs:
            dsem.n += 16
            self.ops.append(lambda e, out=out, in_=in_: e.dma_start(out=out, in_=in_).then_inc(s, 16))
        tok = Tok(s, dsem.n, dsem)
        self._mark(tok, reads, writes)
        return tok

    def replay(self, e):
        for o in self.ops:
            o(e)


def _prod(s):
    r = 1
    for v in s:
        r *= int(v)
    return r


ARENA = 210944


class Prog:
    def __init__(self, stop="full", debug=False):
        self.stop = stop
        self.debug = debug
        nc = self.nc = bass.Bass("TRN2", target_bir_lowering=False, num_devices=8)
        es = self.es = ExitStack()
        dt = nc.dram_tensor

        def din(name, shape, d=F32):
            return dt(name, shape, d, kind="ExternalInput").ap()
        self.x_d = din("x", [NB, 128, D])
        self.pos_d = din("pos", [128, NB], I32)
        self.norms_d = din("norms", [5, D])
        self.wg_d = [din("wg1", [NFG, 128, 16 * 256]), din("wg2", [NFG, 128, 16 * 256])]
        self.wu_d = [din("wu1", [NFG, 128, 16 * 256]), din("wu2", [NFG, 128, 16 * 256])]
        self.wd_d = [din("wd1", [NFG, 128, 2 * D]), din("wd2", [NFG, 128, 2 * D])]
        self.win_d = din("win", [14, 128, 16 * 512])
        self.wout_d = din("wout", [4, 128, 16 * 512])
        self.cstf_d = din("cstf", [128, NF])
        self.cstc_d = din("cstc", [128, NCC])
        self.cstb_d = din("cstb", [128, NCB], BF16)
        self.out_d = dt("out", [NB, 128, D], F32, kind="ExternalOutput").ap()
        if debug:
            self.dbg_mix = dt("dbg_mix", [128, 16384], BF16, kind="ExternalOutput").ap()
            self.dbg_x = dt("dbg_x", [NB, 128, D], F32, kind="ExternalOutput").ap()
        self.xspill = dt("xspill", [NB, 128, D], F32, kind="Internal").ap()
        self.kv_in = dt("kv_in", [NB * 8 * 128, 128], F32, kind="Internal").ap()
        self.kv_all = dt("kv_all", [8 * NB * 8 * 128, 128], F32, kind="Internal", addr_space="Shared").ap()
        self.kb_in = dt("kb_in", [128, 5120], BF16, kind="Internal").ap()
        self.kb_all = dt("kb_all", [1024, 5120], BF16, kind="Internal", addr_space="Shared").ap()

        self.arena = es.enter_context(nc.sbuf_tensor("arena", [128, ARENA // 4], F32))
        self.arena_b = self.arena.bitcast(BF16)
        self.arena_i = self.arena.bitcast(I32)
        self.ps = es.enter_context(nc.psum_tensor("ps", [128, 4096], F32))
        self.psb = self.ps.bitcast(BF16)
        self.pbuf = [Buf("pb%d" % i) for i in range(8)]

        self.pe = Q(nc, es, "pe", True)
        self.act = Q(nc, es, "act")
        self.dve = Q(nc, es, "dve")
        self.pool = Q(nc, es, "pool")
        self.sp = Q(nc, es, "sp")
        self.queues = [self.pe, self.act, self.dve, self.pool, self.sp]
        self.dsems = []
        self.cc_sem = es.enter_context(nc.semaphore("cc"))
        self.cc_n = 0

        self.layout()
        self.build()

        blk = es.enter_context(nc.Block())
        blk.sync(self.sp.replay)
        blk.tensor(self.pe.replay)
        blk.scalar(self.act.replay)
        blk.vector(self.dve.replay)
        blk.gpsimd(self.pool.replay)
        es.close()

    def dsem(self, name):
        d = DSem(self.nc, self.es, name)
        self.dsems.append(d)
        return d

    def carve(self, off, shape, dtype):
        n = _prod(shape[1:])
        if dtype == BF16:
            assert off % 2 == 0
            ap = self.arena_b[:, off // 2: off // 2 + n]
        elif dtype == I32:
            ap = self.arena_i[:, off // 4: off // 4 + n]
        else:
            assert off % 4 == 0
            ap = self.arena[:, off // 4: off // 4 + n]
        if len(shape) == 3:
            ap = ap.rearrange("p (a b) -> p a b", a=shape[1], b=shape[2])
        elif len(shape) == 4:
            ap = ap.rearrange("p (a b c) -> p a b c", a=shape[1], b=shape[2], c=shape[3])
        elif len(shape) == 5:
            ap = ap.rearrange("p (a b c d) -> p a b c d", a=shape[1], b=shape[2], c=shape[3], d=shape[4])
        return ap

    def bank(self, i, n=512, off=0):
        return self.ps[:, i * 512 + off: i * 512 + off + n]

    def bankb(self, i, n=1024, off=0):
        return self.psb[:, i * 1024 + off: i * 1024 + off + n]

    def barrier(self):
        toks = [Tok(q.sem, q.n, q) for q in self.queues if q.n > 0]
        toks += [Tok(d.sem, d.n, d) for d in self.dsems if d.n > 0]
        if self.cc_n:
            toks.append(Tok(self.cc_sem, self.cc_n, None))
        for q in self.queues:
            q.wait(toks)

    def layout(self):
        c = self.carve
        R_X, R_HT, R_W, R_WD, R_GBC, R_T, R_CST = 0, 65536, 98304, 131072, 147456, 155648, 176128
        self.xs = c(R_X, [128, NB, D], F32)
        self.xs_b = [Buf("xs%d" % i) for i in range(NB)]
        self.hT = c(R_HT, [128, 16, 1024], BF16)
        self.hT_b = [Buf("hT0"), Buf("hT1")]
        self.wgu = [c(R_W + s * 16384, [128, 2, 16, 256], BF16) for s in range(2)]
        self.win = [c(R_W + s * 16384, [128, 16, 512], BF16) for s in range(2)]
        self.wg_b = [Buf("wg%d" % s) for s in range(2)]
        self.wu_b = [Buf("wu%d" % s) for s in range(2)]
        self.wdr = [c(R_WD + s * 8192, [128, 2, D], BF16) for s in range(2)]
        self.wd_b = [Buf("wd%d" % s) for s in range(2)]
        self.dw = [self.dsem("dw%d" % s) for s in range(2)]
        self.dwu = [self.dsem("dwu%d" % s) for s in range(2)]
        self.dwd = [self.dsem("dwd%d" % s) for s in range(2)]
        self.gbc = c(R_GBC, [128, D], F32)
        self.gbc_b = Buf("gbc")
        self.aT = [c(R_T + g * 4096, [128, 2, 1024], BF16) for g in range(2)]
        self.aT_b = [Buf("aT%d" % g) for g in range(2)]
        self.sg = [c(R_T + 8192 + s * 2048, [128, 512], F32) for s in range(2)]
        self.sg_b = [Buf("sg%d" % s) for s in range(2)]
        self.hb = [c(R_T + 12288 + s * 4096, [128, D], BF16) for s in range(2)]
        self.hb_b = [Buf("hb%d" % s) for s in range(2)]
        o = R_CST
        self.cstf = c(o, [128, NF], F32); o += NF * 4
        self.cstc = c(o, [128, NCC], F32); o += NCC * 4
        self.cstb = c(o, [128, NCB], BF16); o += NCB * 2
        self.cs128 = c(o, [128, NB, 64], F32); o += 2048
        self.sn128 = c(o, [128, NB, 64], F32); o += 2048
        self.cs64 = c(o, [128, NB, 32], F32); o += 1024
        self.sn64 = c(o, [128, NB, 32], F32); o += 1024
        self.wabs = c(o, [128, NB, 16], F32); o += 512
        self.wsgn = c(o, [128, NB, 16], F32); o += 512
        self.stat = c(o, [128, 256], F32); o += 1024
        self.posi = c(o, [128, NB], I32); o += 32
        self.stg = [c(o + s * 2048, [128, 512], F32) for s in range(2)]
        self.stgb = [c(o + s * 2048, [128, 1024], BF16) for s in range(2)]
        o += 4096
        self.wsgnb = c(o, [128, NB, 16], BF16); o += 256
        assert o <= ARENA, o
        self.cst_b = Buf("cst")
        self.rope_b = Buf("ropetab")
        self.wabs_b = Buf("wabs")
        self.stat_b = Buf("stat")
        self.stg_b = [Buf("stg%d" % s) for s in range(2)]
        self.ident4 = self.cstb[:, 0:512]
        self.ident = self.cstb[:, 0:128]
        self.tri4 = self.cstb[:, 512:1024]
        self.ones = self.cstb[:, 1024:1152]
        self.cb = self.cstc[:, 0:1024]
        self.selc = self.cstc[:, 1024:1032]
        self.qT = c(R_X, [128, NB, 8, 128], BF16)
        self.kT = c(R_X + 16384, [128, NB, 8, 128], BF16)
        self.vr = c(R_X + 32768, [128, NB, 1024], BF16)
        self.sgr = c(R_X + 49152, [128, NB, 1024], BF16)
        self.aqT = c(R_WD, [128, NB, 8, 128], BF16)
        BT = R_GBC
        self.ropeA = c(BT, [128, 512], F32)
        self.ropeB = c(BT + 2048, [128, 512], F32)
        self.bout = [c(BT + 4096 + s * 2048, [128, 512], F32) for s in range(2)]
        self.boutb = [c(BT + 4096 + s * 2048, [128, 512], BF16) for s in range(2)]
        self.aux = [c(BT + 8192 + s * 1024, [128, 512], BF16) for s in range(2)]
        self.iqT = c(BT + 12288, [128, NB, 8, 128], BF16)
        self.qT_b, self.kT_b, self.vr_b, self.sgr_b = Buf("qT"), Buf("kT"), Buf("vr"), Buf("sgr")
        self.aqT_b, self.iqT_b = Buf("aqT"), Buf("iqT")
        self.ropeA_b, self.ropeB_b = Buf("ropeA"), Buf("ropeB")
        self.bout_b = [Buf("bout0"), Buf("bout1")]
        self.aux_b = [Buf("aux0"), Buf("aux1")]
        self.t0a = c(BT, [128, NB, 64], F32)
        self.t0b = c(BT + 2048, [128, NB, 64], F32)
        self.t0i = c(BT + 4096, [128, NB, 64], I32)
        self.t0c = c(BT + 6144, [128, NB, 64], F32)
        self.snap = c(R_W, [128, 2, 4, 1024], BF16)
        self.S = c(R_W + 16384, [128, 1024], F32)
        self.kvbuf = [c(R_W + 20480 + s * 4096, [128, 8, 128], F32) for s in range(2)]
        self.gnr = c(R_W + 28672, [128, 1024], F32)
        self.snap_b, self.S_b, self.gnr_b = Buf("snap"), Buf("S"), Buf("gnr")
        self.kvbuf_b = [Buf("kvb0"), Buf("kvb1")]
        self.dkv = [self.dsem("dkv%d" % s) for s in range(2)]
        self.t1 = c(BT, [128, 1024], F32)
        self.t2 = c(BT + 4096, [128, 1024], F32)
        self.ybf = c(BT + 8192, [128, 1024], BF16)
        self.sTm = [c(BT + 10240 + s * 1024, [128, 512], BF16) for s in range(2)]
        self.t1_b, self.t2_b, self.ybf_b = Buf("t1"), Buf("t2"), Buf("ybf")
        self.sTm_b = [Buf("sTm0"), Buf("sTm1")]
        self.KT = c(R_X, [128, 2, 4096], BF16)
        self.V = c(R_X + 16384, [128, 32, 2, 128], BF16)
        self.ikT = c(R_X + 32768, [128, 4096], BF16)
        self.rb = [c(R_X + 40960 + s * 2048, [128, 512], F32) for s in range(4)]
        self.pT = [c(R_X + 49152 + s * 1024, [128, 512], BF16) for s in range(2)]
        self.rcp = c(R_X + 51200, [128, 512], F32)
        self.KT_b, self.V_b, self.ikT_b = Buf("KT"), Buf("V"), Buf("ikT")
        self.rb_b = [Buf("rb%d" % s) for s in range(4)]
        self.pT_b = [Buf("pT0"), Buf("pT1")]
        self.rcp_b = Buf("rcp")
        self.rbb = [c(R_X + 40960 + s * 2048, [128, 512], BF16) for s in range(4)]
        self.dg = c(R_X + 61440, [128, 16, 128], BF16)
        self.dg_b = Buf("dg")
        self.score = [c(R_W + i * 16384, [128, 4096], F32) for i in range(2)]
        self.mb = [c(R_X + 53248, [128, 4096], BF16), c(BT, [128, 4096], BF16)]
        self.score_b, self.mb_b = [Buf("score0"), Buf("score1")], [Buf("mb0"), Buf("mb1")]
        self.mixT = self.hT
        self.xspill_b = [Buf("xsp%d" % i) for i in range(NB)]
        self.kv_in_b, self.kb_in_b = Buf("kv_in"), Buf("kb_in")
        self.kv_all_b, self.kb_all_b = Buf("kv_all"), Buf("kb_all")
        self.d_ld = self.dsem("d_ld")
        self.d_x = [self.dsem("d_x%d" % i) for i in range(NB)]
        self.d_st = self.dsem("d_st")
        self.d_g = self.dsem("d_g")
        self.d_kt, self.d_v, self.d_ik = self.dsem("d_kt"), self.dsem("d_v"), self.dsem("d_ik")
        self.d_stg = [self.dsem("d_stg%d" % i) for i in range(2)]

    def load_x(self, src):
        for lb in range(NB):
            self.sp.dma(self.d_x[lb], self.xs[:, lb, :], src[lb], writes=[self.xs_b[lb]])

    def sincos(self, ang_fn, shape, out_sin, shift, n):
        dve, act = self.dve, self.act
        tb = self.t0b[:, :, 0:n]
        ti = self.t0i[:, :, 0:n]
        tc_ = self.t0c[:, :, 0:n]
        B1, B2, B3 = self.ropeB_b, self.bout_b[0], self.bout_b[1]
        ang = ang_fn
        dve.op(lambda e: e.tensor_scalar(out=tc_, in0=ang, scalar1=float(shift), scalar2=None, op0=ALU.add), reads=[self.ropeA_b], writes=[B3])
        dve.op(lambda e: e.tensor_scalar(out=tb, in0=tc_, scalar1=float(1.0 / (2 * PI)), scalar2=None, op0=ALU.mult), reads=[B3], writes=[B1])
        dve.op(lambda e: e.tensor_copy(out=ti, in_=tb), reads=[B1], writes=[B2])
        dve.op(lambda e: e.tensor_copy(out=tb, in_=ti), reads=[B2], writes=[B1])
        dve.op(lambda e: e.scalar_tensor_tensor(out=tc_, in0=tb, scalar=float(-2 * PI), in1=tc_, op0=ALU.mult, op1=ALU.add), reads=[B1, B3], writes=[B3])
        dve.op(lambda e: e.tensor_scalar(out=tb, in0=tc_, scalar1=PI, scalar2=float(-2 * PI), op0=ALU.is_gt, op1=ALU.mult), reads=[B3], writes=[B1])
        dve.op(lambda e: e.tensor_tensor(out=tc_, in0=tc_, in1=tb, op=ALU.add), reads=[B1, B3], writes=[B3])
        dve.op(lambda e: e.tensor_scalar(out=tb, in0=tc_, scalar1=-PI, scalar2=float(2 * PI), op0=ALU.is_lt, op1=ALU.mult), reads=[B3], writes=[B1])
        dve.op(lambda e: e.tensor_tensor(out=tc_, in0=tc_, in1=tb, op=ALU.add), reads=[B1, B3], writes=[B3])
        act.op(lambda e: e.activation(out=out_sin, in_=tc_, func=AF.Sin), reads=[B3], writes=[self.rope_b])

    def phase0(self):
        sp, dve = self.sp, self.dve
        sp.dma(self.d_ld, self.cstf, self.cstf_d, writes=[self.cst_b])
        sp.dma(self.d_ld, self.cstc, self.cstc_d, writes=[self.cst_b])
        sp.dma(self.d_ld, self.cstb, self.cstb_d, writes=[self.cst_b])
        sp.dma(self.d_ld, self.posi, self.pos_d, writes=[self.cst_b])
        self.load_x(self.x_d)
        posf = self.stat[:, 0:NB]
        dve.op(lambda e: e.tensor_copy(out=posf, in_=self.posi), reads=[self.cst_b], writes=[self.stat_b])
        for n, inv_off, cs, sn in ((64, C_INV128, self.cs128, self.sn128), (32, C_INV64, self.cs64, self.sn64)):
            ang = self.t0a[:, :, 0:n]
            inv = self.cstf[:, inv_off:inv_off + n]
            dve.op(lambda e, ang=ang, inv=inv, n=n: e.tensor_tensor(
                out=ang, in0=inv.unsqueeze(1).to_broadcast([128, NB, n]),
                in1=posf.unsqueeze(2).to_broadcast([128, NB, n]), op=ALU.mult),
                reads=[self.cst_b, self.stat_b], writes=[self.ropeA_b])
            self.sincos(ang, None, sn, 0.0, n)
            self.sincos(ang, None, cs, PI / 2, n)

    def norm_stats(self, row):
        sp, act, dve = self.sp, self.act, self.dve
        sp.dma(self.d_g, self.gbc, self.norms_d[row:row + 1, :].partition_broadcast(128), writes=[self.gbc_b])
        ss = self.stat[:, 16:16 + NB]
        for lb in range(NB):
            act.op(lambda e, lb=lb: e.activation(out=self.hb[1], in_=self.xs[:, lb, :], func=AF.Square,
                                                 accum_out=ss[:, lb:lb + 1]),
                   reads=[self.xs_b[lb]], writes=[self.hb_b[1], self.stat_b])
        rstd = self.stat[:, 32:32 + NB]
        act.op(lambda e: e.activation(out=rstd, in_=ss, func=AF.Sqrt, scale=1.0 / D,
                                      bias=self.cstf[:, C_EPS:C_EPS + 1]),
               reads=[self.stat_b, self.cst_b], writes=[self.stat_b])
        dve.op(lambda e: e.reciprocal(out=rstd, in_=rstd), reads=[self.stat_b], writes=[self.stat_b])
        return rstd

    def norm_T(self, row):
        pe, act, dve = self.pe, self.act, self.dve
        rstd = self.norm_stats(row)
        for lb in range(NB):
            s = lb % 2
            hb = self.hb[s]
            dve.op(lambda e, lb=lb, hb=hb: e.scalar_tensor_tensor(
                out=hb, in0=self.xs[:, lb, :], scalar=rstd[:, lb:lb + 1], in1=self.gbc,
                op0=ALU.mult, op1=ALU.mult),
                reads=[self.xs_b[lb], self.stat_b, self.gbc_b], writes=[self.hb_b[s]])
            for half in range(2):
                bk = 4 + 2 * s + half
                for i in range(8):
                    kc = half * 8 + i
                    pe.op(lambda e, bk=bk, i=i, kc=kc, hb=hb: e.transpose(
                        out=self.bankb(bk, 128, i * 128), in_=hb[:, kc * 128:(kc + 1) * 128], identity=self.ident),
                        reads=[self.hb_b[s], self.cst_b], writes=[self.pbuf[bk]])
                dst = self.hT[:, half * 8:(half + 1) * 8, lb * 128:(lb + 1) * 128]
                src = self.bankb(bk).rearrange("p (a b) -> p a b", a=8, b=128)
                eng = act if half == 0 else dve
                if eng is act:
                    eng.op(lambda e, dst=dst, src=src: e.activation(out=dst, in_=src, func=AF.Copy),
                           reads=[self.pbuf[bk]], writes=[self.hT_b[lb // 4]])
                else:
                    eng.op(lambda e, dst=dst, src=src: e.tensor_copy(out=dst, in_=src),
                           reads=[self.pbuf[bk]], writes=[self.hT_b[lb // 4]])

    def final_norm(self):
        dve, sp = self.dve, self.sp
        rstd = self.norm_stats(3)
        last = None
        for lb in range(NB):
            s = lb % 2
            o = self.sg[s]
            dve.op(lambda e, lb=lb: e.scalar_tensor_tensor(
                out=self.xs[:, lb, :], in0=self.xs[:, lb, :], scalar=rstd[:, lb:lb + 1], in1=self.gbc,
                op0=ALU.mult, op1=ALU.mult),
                reads=[self.xs_b[lb], self.stat_b, self.gbc_b], writes=[self.xs_b[lb]])
            last = sp.dma(self.d_st, self.out_d[lb], self.xs[:, lb, :], reads=[self.xs_b[lb]])
        return last

    def ffn(self, idx):
        pe, act, dve, pool = self.pe, self.act, self.dve, self.pool
        wg_d, wu_d, wd_d = self.wg_d[idx], self.wu_d[idx], self.wd_d[idx]

        def load(fg):
            s = fg % 2
            pool.dma(self.dw[s], self.wgu[s][:, 0].rearrange("p a b -> p (a b)"), wg_d[fg], writes=[self.wg_b[s]])
            pool.dma(self.dwu[s], self.wgu[s][:, 1].rearrange("p a b -> p (a b)"), wu_d[fg], writes=[self.wu_b[s]])
            pool.dma(self.dwd[s], self.wdr[s].rearrange("p a b -> p (a b)"), wd_d[fg], writes=[self.wd_b[s]])

        def gate_up(fg):
            s = fg % 2
            for f in range(2):
                for half in range(2):
                    u = f * 2 + half
                    pg, pu = (u % 2) * 2, (u % 2) * 2 + 1
                    rhs_sl = slice(half * 512, (half + 1) * 512)
                    for (bk, wi, wb) in ((pg, 0, self.wg_b[s]), (pu, 1, self.wu_b[s])):
                        for kc in range(16):
                            pe.op(lambda e, bk=bk, wi=wi, kc=kc, f=f, s=s, rhs_sl=rhs_sl: e.matmul(
                                self.bank(bk), lhsT=self.wgu[s][:, wi, kc, f * 128:(f + 1) * 128],
                                rhs=self.hT[:, kc, rhs_sl], start=(kc == 0), stop=(kc == 15)),
                                reads=[wb, self.hT_b[half]], writes=[self.pbuf[bk]])
                    sgs = u % 2
                    act.op(lambda e, pg=pg, sgs=sgs: e.activation(out=self.sg[sgs], in_=self.bank(pg), func=AF.Silu),
                           reads=[self.pbuf[pg]], writes=[self.sg_b[sgs]])
                    dve.op(lambda e, pu=pu, sgs=sgs, fg=fg, f=f, rhs_sl=rhs_sl: e.tensor_tensor(
                        out=self.aT[fg % 2][:, f, rhs_sl], in0=self.sg[sgs], in1=self.bank(pu), op=ALU.mult),
                        reads=[self.sg_b[sgs], self.pbuf[pu]], writes=[self.aT_b[fg % 2]])

        def down(fg):
            s = fg % 2
            for tb in range(NB):
                for ch in range(2):
                    par = (tb * 2 + ch) % 2
                    for cbk in range(2):
                        bk = 4 + par * 2 + cbk
                        for f in range(2):
                            c0 = ch * 1024 + cbk * 512
                            pe.op(lambda e, bk=bk, f=f, tb=tb, c0=c0, fg=fg, s=s: e.matmul(
                                self.bank(bk), lhsT=self.aT[fg % 2][:, f, tb * 128:(tb + 1) * 128],
                                rhs=self.wdr[s][:, f, c0:c0 + 512], start=(f == 0), stop=(f == 1)),
                                reads=[self.aT_b[fg % 2], self.wd_b[s]], writes=[self.pbuf[bk]])
                    b0 = 4 + par * 2
                    xsl = self.xs[:, tb, ch * 1024:(ch + 1) * 1024]
                    dve.op(lambda e, b0=b0, xsl=xsl: e.scalar_tensor_tensor(
                        out=xsl, in0=self.bank(b0, 1024), scalar=0.5, in1=xsl, op0=ALU.mult, op1=ALU.add),
                        reads=[self.pbuf[b0], self.pbuf[b0 + 1], self.xs_b[tb]], writes=[self.xs_b[tb]])

        nfg = NFG
        load(0)
        load(1)
        for fg in range(nfg + 1):
            if fg < nfg:
                gate_up(fg)
            if fg >= 1:
                down(fg - 1)
                if fg + 1 < nfg:
                    load(fg + 1)

    def rope(self, bk, col0, H, dh, cos, sin, out, out_b, extra_reads=()):
        dve = self.dve
        hd = dh // 2
        n = H * dh
        v = self.bank(bk, n, col0).rearrange("p (h t d) -> p h t d", h=H, t=2, d=hd)
        A = self.ropeA[:, 0:n].rearrange("p (h t d) -> p h t d", h=H, t=2, d=hd)
        Bt = self.ropeB[:, 0:n].rearrange("p (h t d) -> p h t d", h=H, t=2, d=hd)
        cos_b4 = cos.unsqueeze(1).unsqueeze(1).to_broadcast([128, H, 2, hd])
        sin_b3 = sin.unsqueeze(1).to_broadcast([128, H, hd])
        rd = [self.pbuf[bk], self.rope_b] + list(extra_reads)
        dve.op(lambda e: e.tensor_tensor(out=A, in0=v, in1=cos_b4, op=ALU.mult), reads=rd, writes=[self.ropeA_b])
        dve.op(lambda e: e.scalar_tensor_tensor(out=Bt[:, :, 0, :], in0=v[:, :, 1, :], scalar=-1.0, in1=sin_b3,
                                                op0=ALU.mult, op1=ALU.mult), reads=rd, writes=[self.ropeB_b])
        self.rope_ps_tok = dve.op(lambda e: e.tensor_tensor(out=Bt[:, :, 1, :], in0=v[:, :, 0, :], in1=sin_b3, op=ALU.mult),
                                  reads=rd, writes=[self.ropeB_b])
        return dve.op(lambda e: e.tensor_tensor(out=out, in0=self.ropeA[:, 0:n], in1=self.ropeB[:, 0:n], op=ALU.add),
                      reads=[self.ropeA_b, self.ropeB_b], writes=[out_b])

    def phaseB(self):
        pe, act, dve, pool, sp = self.pe, self.act, self.dve, self.pool, self.sp
        self.norm_T(1)
        for lb in range(NB):
            sp.dma(self.d_st, self.xspill[lb], self.xs[:, lb, :], reads=[self.xs_b[lb]], writes=[self.xspill_b[lb]])
        self.barrier()

        def load(cg):
            s = cg % 2
            pool.dma(self.dw[s], self.win[s].rearrange("p a b -> p (a b)"), self.win_d[cg],
                     writes=[self.wg_b[s], self.wu_b[s]])

        fams = ["rv", "rv", "rk", "rk", "rq", "rq", "rg", "rg", "aq", "aq", "akav", "ikiw", "iq", "iq"]
        load(0)
        load(1)
        self.tpc = 0
        self.kvc = 0
        pending = []
        u = 0
        kvrows = self.kv_in.rearrange("(l h d) e -> d l h e", l=NB, h=8, d=128)
        for cg in range(14):
            s = cg % 2
            fam = fams[cg]
            hs = cg % 2 if fam != "akav" and fam != "ikiw" else 0
            if fam in ("rv", "rk", "rq", "rg", "aq", "iq"):
                hs = [c for c in range(14) if fams[c] == fam].index(cg)
            ncols = 144 if fam == "ikiw" else 512
            for lb in range(NB):
                bk = u % 4
                for kc in range(16):
                    pe.op(lambda e, bk=bk, kc=kc, lb=lb, s=s, ncols=ncols: e.matmul(
                        self.bank(bk, ncols), lhsT=self.hT[:, kc, lb * 128:(lb + 1) * 128],
                        rhs=self.win[s][:, kc, 0:ncols], start=(kc == 0), stop=(kc == 15)),
                        reads=[self.hT_b[lb // 4], self.wg_b[s], self.wu_b[s]], writes=[self.pbuf[bk]])
                for fn in pending:
                    fn()
                pending = []
                pending.append(self.postB(fam, hs, lb, bk, u, kvrows))
                u += 1
            if cg + 2 < 14:
                load(cg + 2)
            if cg == 4:
                self.issue_ag(self.kv_in, self.kv_all)
            if cg == 12:
                self.issue_ag(self.kb_in, self.kb_all)
        for fn in pending:
            fn()

    def postB(self, fam, hs, lb, bk, u, kvrows):
        pe, act, dve, sp = self.pe, self.act, self.dve, self.sp
        pb = self.pbuf[bk]
        so = u % 2
        h0 = hs * 4

        def transposes(src_ap, src_b, nblk):
            tb = 4 + (self.tpc % 2)
            self.tpc += 1
            for i in range(nblk):
                pe.op(lambda e, i=i, tb=tb: e.transpose(out=self.bankb(tb, 128, i * 128),
                                                        in_=src_ap[:, i * 128:(i + 1) * 128], identity=self.ident),
                      reads=[src_b, self.cst_b], writes=[self.pbuf[tb]])
            return tb

        if fam == "rv":
            act.op(lambda e: e.activation(out=self.vr[:, lb, hs * 512:(hs + 1) * 512], in_=self.bank(bk), func=AF.Copy),
                   reads=[pb], writes=[self.vr_b])
            return lambda: None
        if fam == "rg":
            act.op(lambda e: e.activation(out=self.sgr[:, lb, hs * 512:(hs + 1) * 512], in_=self.bank(bk), func=AF.Silu),
                   reads=[pb], writes=[self.sgr_b])
            return lambda: None
        if fam in ("rk", "rq", "aq"):
            ob = self.boutb[so]
            self.rope(bk, 0, 4, 128, self.cs128[:, lb, :], self.sn128[:, lb, :], ob, self.bout_b[so])
            if fam == "rk":
                gk_b = self.cstf[:, C_GK + h0:C_GK + h0 + 4].unsqueeze(2).to_broadcast([128, 4, 128])
                dve.op(lambda e: e.tensor_tensor(out=self.aux[so].rearrange("p (h d) -> p h d", h=4, d=128),
                                                 in0=ob.rearrange("p (h d) -> p h d", h=4, d=128), in1=gk_b, op=ALU.mult),
                       reads=[self.bout_b[so], self.cst_b], writes=[self.aux_b[so]])

            def deferred():
                tb = transposes(ob, self.bout_b[so], 4)
                if fam == "rk":
                    dst = self.kT[:, lb, h0:h0 + 4, :].rearrange("p h t -> p (h t)")
                    tab = self.cstf[:, C_GKT + h0 * 128:C_GKT + h0 * 128 + 512]
                    dve.op(lambda e: e.tensor_tensor(out=dst, in0=self.bankb(tb, 512), in1=tab, op=ALU.mult),
                           reads=[self.pbuf[tb], self.cst_b], writes=[self.kT_b])
                    kb = 6 + (self.kvc % 2)
                    self.kvc += 1
                    for h in range(4):
                        pe.op(lambda e, h=h, kb=kb: e.matmul(
                            self.bank(kb, 128, h * 128), lhsT=self.aux[so][:, h * 128:(h + 1) * 128],
                            rhs=self.vr[:, lb, (h0 + h) * 128:(h0 + h + 1) * 128], start=True, stop=True),
                            reads=[self.aux_b[so], self.vr_b], writes=[self.pbuf[kb]])
                    act.op(lambda e, kb=kb: e.activation(out=self.stg[so], in_=self.bank(kb), func=AF.Copy),
                           reads=[self.pbuf[kb]], writes=[self.stg_b[so]])
                    sp.dma(self.d_stg[so], kvrows[:, lb, h0:h0 + 4, :],
                           self.stg[so].rearrange("p (h e) -> p h e", h=4, e=128),
                           reads=[self.stg_b[so]])
                elif fam == "rq":
                    dst = self.qT[:, lb, h0:h0 + 4, :].rearrange("p h t -> p (h t)")
                    tab = self.cstf[:, C_GQT + h0 * 128:C_GQT + h0 * 128 + 512]
                    dve.op(lambda e: e.tensor_tensor(out=dst, in0=self.bankb(tb, 512), in1=tab, op=ALU.mult),
                           reads=[self.pbuf[tb], self.cst_b], writes=[self.qT_b])
                else:
                    dst = self.aqT[:, lb, h0:h0 + 4, :].rearrange("p h t -> p (h t)")
                    act.op(lambda e: e.activation(out=dst, in_=self.bankb(tb, 512), func=AF.Copy),
                           reads=[self.pbuf[tb]], writes=[self.aqT_b])
            return deferred
        if fam == "akav":
            ob = self.boutb[so][:, 0:256]
            self.rope(bk, 0, 2, 128, self.cs128[:, lb, :], self.sn128[:, lb, :], ob, self.bout_b[so])
            avst = self.stgb[so][:, 0:256]
            act.op(lambda e: e.activation(out=avst, in_=self.bank(bk, 256, 256), func=AF.Copy),
                   reads=[pb], writes=[self.stg_b[so]], extra=[self.rope_ps_tok])

            def deferred():
                tb = transposes(ob, self.bout_b[so], 2)
                akst = self.stgb[so][:, 256:512]
                act.op(lambda e: e.activation(out=akst, in_=self.bankb(tb, 256), func=AF.Copy),
                       reads=[self.pbuf[tb]], writes=[self.stg_b[so]])
                sp.dma(self.d_stg[so], self.kb_in[:, lb * 256:(lb + 1) * 256], akst, reads=[self.stg_b[so]])
                sp.dma(self.d_stg[so], self.kb_in[:, 3072 + lb * 256:3072 + (lb + 1) * 256], avst,
                       reads=[self.stg_b[so]])
            return deferred
        if fam == "ikiw":
            ob = self.boutb[so][:, 0:128]
            self.rope(bk, 0, 2, 64, self.cs64[:, lb, :], self.sn64[:, lb, :], ob, self.bout_b[so])
            act.op(lambda e: e.activation(out=self.wabs[:, lb, :], in_=self.bank(bk, 16, 128), func=AF.Abs, scale=1.0 / 32),
                   reads=[pb], writes=[self.wabs_b], extra=[self.rope_ps_tok])
            act.op(lambda e: e.activation(out=self.wsgn[:, lb, :], in_=self.bank(bk, 16, 128), func=AF.Sign),
                   reads=[pb], writes=[self.wabs_b])
            act.op(lambda e: e.activation(out=self.wsgnb[:, lb, :], in_=self.bank(bk, 16, 128), func=AF.Sign),
                   reads=[pb], writes=[self.wabs_b])

            def deferred():
                tb = transposes(ob, self.bout_b[so], 1)
                ikst = self.stgb[so][:, 0:128]
                act.op(lambda e: e.activation(out=ikst, in_=self.bankb(tb, 128), func=AF.Copy),
                       reads=[self.pbuf[tb]], writes=[self.stg_b[so]])
                sp.dma(self.d_stg[so], self.kb_in[:, 2048 + lb * 128:2048 + (lb + 1) * 128], ikst,
                       reads=[self.stg_b[so]])
            return deferred
        if fam == "iq":
            of = self.bout[so]
            self.rope(bk, 0, 8, 64, self.cs64[:, lb, :], self.sn64[:, lb, :], of, self.bout_b[so])
            wb_ = self.wabs[:, lb, hs * 8:(hs + 1) * 8].unsqueeze(2).to_broadcast([128, 8, 64])
            dve.op(lambda e: e.tensor_tensor(out=self.aux[so].rearrange("p (h d) -> p h d", h=8, d=64),
                                             in0=of.rearrange("p (h d) -> p h d", h=8, d=64), in1=wb_, op=ALU.mult),
                   reads=[self.bout_b[so], self.wabs_b], writes=[self.aux_b[so]])

            def deferred():
                tb = transposes(self.aux[so], self.aux_b[so], 4)
                dst = self.iqT[:, lb, hs * 4:(hs + 1) * 4, :].rearrange("p h t -> p (h t)")
                act.op(lambda e: e.activation(out=dst, in_=self.bankb(tb, 512), func=AF.Copy),
                       reads=[self.pbuf[tb]], writes=[self.iqT_b])
            return deferred
        raise ValueError(fam)

    def issue_ag(self, src, dst):
        pool = self.pool
        toks = [Tok(d.sem, d.n, d) for d in self.d_stg if d.n > 0]
        pool.wait(toks)
        self.cc_n += 1
        sem = self.cc_sem
        pool.ops.append(lambda e, src=src, dst=dst: e.collective_compute(
            "AllGather", ALU.bypass, replica_groups=[list(range(8))], ins=[src], outs=[dst]).then_inc(sem, 1))

    def allgather(self):
        assert self.cc_n == 2
        self.barrier()

    def phaseC1(self):
        pe, act, dve, pool, sp = self.pe, self.act, self.dve, self.pool, self.sp
        sp.dma(self.d_g, self.gnr, self.norms_d[4:5, 0:1024].partition_broadcast(128), writes=[self.gnr_b])
        dve.op(lambda e: e.memset(self.snap.rearrange("p a b c -> p (a b c)"), 0.0), writes=[self.snap_b])
        kvsrc = self.kv_all.rearrange("(r l h d) e -> d r l h e", r=8, l=NB, h=8, d=128)
        gC = self.cstf[:, C_GC:C_GC + 1024]
        step = 0
        for b in range(2):
            dve.op(lambda e: e.memset(self.S, 0.0), writes=[self.S_b])
            for n in range(32):
                k, jj = divmod(n, 8)
                s = step % 2
                if n < 31:
                    sp.dma(self.dkv[s], self.kvbuf[s], kvsrc[:, jj, b * 4 + k, :, :], reads=[self.kv_all_b],
                           writes=[self.kvbuf_b[s]])
                sn = self.snap[:, b, k, :]
                dve.op(lambda e, sn=sn, jj=jj: e.scalar_tensor_tensor(
                    out=sn, in0=self.S, scalar=self.selc[:, jj:jj + 1], in1=sn, op0=ALU.mult, op1=ALU.add),
                    reads=[self.S_b, self.cst_b, self.snap_b], writes=[self.snap_b])
                if n < 31:
                    dve.op(lambda e, s=s: e.tensor_tensor(out=self.S, in0=self.S,
                                                          in1=self.kvbuf[s].rearrange("p h e -> p (h e)"), op=ALU.add),
                           reads=[self.S_b, self.kvbuf_b[s]], writes=[self.S_b])
                    dve.op(lambda e: e.tensor_tensor(out=self.S, in0=self.S, in1=gC, op=ALU.mult),
                           reads=[self.S_b, self.cst_b], writes=[self.S_b])
                    step += 1
        def block(lb):
            b, k = divmod(lb, 4)
            for hg in range(2):
                sbk, obk = hg, 2 + hg
                for h in range(4):
                    H = hg * 4 + h
                    pe.op(lambda e, h=h, H=H, sbk=sbk: e.matmul(
                        self.bank(sbk, 128, h * 128), lhsT=self.kT[:, lb, H, :], rhs=self.qT[:, lb, H, :],
                        start=True, stop=True), reads=[self.kT_b, self.qT_b], writes=[self.pbuf[sbk]])
                dve.op(lambda e, sbk=sbk, hg=hg: e.tensor_tensor(out=self.sTm[hg], in0=self.bank(sbk), in1=self.tri4,
                                                                 op=ALU.mult),
                       reads=[self.pbuf[sbk], self.cst_b], writes=[self.sTm_b[hg]])
                for h in range(4):
                    H = hg * 4 + h
                    pe.op(lambda e, h=h, H=H, obk=obk, hg=hg: e.matmul(
                        self.bank(obk, 128, h * 128), lhsT=self.sTm[hg][:, h * 128:(h + 1) * 128],
                        rhs=self.vr[:, lb, H * 128:(H + 1) * 128], start=True, stop=False),
                        reads=[self.sTm_b[hg], self.vr_b], writes=[self.pbuf[obk]])
                    pe.op(lambda e, h=h, H=H, obk=obk: e.matmul(
                        self.bank(obk, 128, h * 128), lhsT=self.qT[:, lb, H, :],
                        rhs=self.snap[:, b, k, H * 128:(H + 1) * 128], start=False, stop=True),
                        reads=[self.qT_b, self.snap_b], writes=[self.pbuf[obk]])
                act.op(lambda e, obk=obk, hg=hg: e.activation(out=self.t1[:, hg * 512:(hg + 1) * 512],
                                                              in_=self.bank(obk), func=AF.Copy),
                       reads=[self.pbuf[obk]], writes=[self.t1_b])
            t1v = self.t1.rearrange("p (h e) -> p h e", h=8, e=128)
            t2v = self.t2.rearrange("p (h e) -> p h e", h=8, e=128)
            s1 = self.stat[:, 64:72]
            s2 = self.stat[:, 72:80]
            dve.op(lambda e: e.tensor_reduce(out=s1, in_=t1v, axis=AX.X, op=ALU.add), reads=[self.t1_b], writes=[self.stat_b])
            dve.op(lambda e: e.tensor_scalar(out=s1, in0=s1, scalar1=-1.0 / 128, scalar2=None, op0=ALU.mult),
                   reads=[self.stat_b], writes=[self.stat_b])
            dve.op(lambda e: e.tensor_tensor(out=t2v, in0=t1v, in1=s1.unsqueeze(2).to_broadcast([128, 8, 128]), op=ALU.add),
                   reads=[self.t1_b, self.stat_b], writes=[self.t2_b])
            dve.op(lambda e: e.tensor_tensor(out=self.t1, in0=self.t2, in1=self.t2, op=ALU.mult),
                   reads=[self.t2_b], writes=[self.t1_b])
            dve.op(lambda e: e.tensor_reduce(out=s2, in_=t1v, axis=AX.X, op=ALU.add), reads=[self.t1_b], writes=[self.stat_b])
            act.op(lambda e: e.activation(out=s2, in_=s2, func=AF.Sqrt, scale=1.0 / 128, bias=self.cstf[:, C_EPS:C_EPS + 1]),
                   reads=[self.stat_b, self.cst_b], writes=[self.stat_b])
            dve.op(lambda e: e.reciprocal(out=s2, in_=s2), reads=[self.stat_b], writes=[self.stat_b])
            dve.op(lambda e: e.tensor_tensor(out=t1v, in0=t2v, in1=s2.unsqueeze(2).to_broadcast([128, 8, 128]), op=ALU.mult),
                   reads=[self.t2_b, self.stat_b], writes=[self.t1_b])
            dve.op(lambda e: e.tensor_tensor(out=self.t2, in0=self.t1, in1=self.gnr, op=ALU.mult),
                   reads=[self.t1_b, self.gnr_b], writes=[self.t2_b])
            dve.op(lambda e: e.tensor_tensor(out=self.ybf, in0=self.t2, in1=self.sgr[:, lb, :], op=ALU.mult),
                   reads=[self.t2_b, self.sgr_b], writes=[self.ybf_b])
            tb = 4 + lb % 2
            for h in range(8):
                pe.op(lambda e, h=h, tb=tb: e.transpose(out=self.bankb(tb, 128, h * 128),
                                                        in_=self.ybf[:, h * 128:(h + 1) * 128], identity=self.ident),
                      reads=[self.ybf_b, self.cst_b], writes=[self.pbuf[tb]])
            dst = self.mixT[:, 0:8, lb * 128:(lb + 1) * 128]
            act.op(lambda e, tb=tb, dst=dst: e.activation(out=dst, in_=self.bankb(tb).rearrange("p (a b) -> p a b", a=8, b=128),
                                                          func=AF.Copy),
                   reads=[self.pbuf[tb]], writes=[self.hT_b[lb // 4]])

        for lb in range(NB):
            block(lb)

    def phaseC2(self):
        pe, act, dve, pool, sp = self.pe, self.act, self.dve, self.pool, self.sp
        kb = self.kb_all
        ksrc = kb[:, 0:2048].rearrange("(j p) (l g t) -> p g l j t", p=128, l=NB, g=2, t=128)
        isrc = kb[:, 2048:3072].rearrange("(j p) (l t) -> p l j t", p=128, l=NB, t=128)
        vsrc = kb[:, 3072:5120].rearrange("(j p) (l g d) -> p g l j d", p=128, l=NB, g=2, d=128)
        self.iu = 0
        self.au = 0
        scale = float(128 ** -0.5)
        st = self.stat
        rmax, rmin, w0, mid, cnt, tmp, tau = (st[:, 96:97], st[:, 97:98], st[:, 98:99], st[:, 99:100],
                                              st[:, 100:101], st[:, 101:102], st[:, 102:103])
        wtab = st[:, 128:128 + NIT + 1]
        SB = self.stat_b

        def load_ik(b):
            pi_ = [(self.ikT[:, k * 1024:(k + 1) * 1024].rearrange("p (j t) -> p j t", j=8, t=128),
                    isrc[:, b * 4 + k, :, :]) for k in range(4)]
            sp.dma_group(self.d_ik, pi_, reads=[self.kb_all_b], writes=[self.ikT_b])

        def load_kv(b):
            pk, pv = [], []
            for k in range(4):
                for g in range(2):
                    pk.append((self.KT[:, g, k * 1024:(k + 1) * 1024].rearrange("p (j t) -> p j t", j=8, t=128),
                               ksrc[:, g, b * 4 + k, :, :]))
                    pv.append((self.V[:, k * 8:(k + 1) * 8, g, :], vsrc[:, g, b * 4 + k, :, :]))
            sp.dma_group(self.d_kt, pk, reads=[self.kb_all_b], writes=[self.KT_b])
            sp.dma_group(self.d_v, pv, reads=[self.kb_all_b], writes=[self.V_b])

        def X(lb):
            S = 1024 * (lb % 4 + 1)
            pool.op(lambda e: e.tensor_tensor(
                out=self.dg, in0=self.ident.unsqueeze(1).to_broadcast([128, 16, 128]),
                in1=self.wsgnb[:, lb, :].unsqueeze(2).to_broadcast([128, 16, 128]), op=ALU.mult),
                reads=[self.cst_b, self.wabs_b], writes=[self.dg_b])
            units = [(kg, h) for kg in range(S // 512) for h in range(16)]
            prev = None

            def headsum(kg, h, r):
                pe.op(lambda e: e.matmul(self.bank(2), lhsT=self.dg[:, h, :], rhs=self.rbb[r],
                                         start=(h == 0), stop=(h == 15)),
                      reads=[self.dg_b, self.rb_b[r]], writes=[self.pbuf[2]])
                if h == 15:
                    sc = self.score[lb % 2][:, kg * 512:(kg + 1) * 512]
                    act.op(lambda e: e.activation(out=sc, in_=self.bank(2), func=AF.Copy), reads=[self.pbuf[2]],
                           writes=[self.score_b[lb % 2]])
            for (kg, h) in units:
                hp, half = divmod(h, 2)
                bk = self.iu % 2
                r = self.iu % 4
                self.iu += 1
                pr = slice(half * 64, (half + 1) * 64)
                pe.op(lambda e, bk=bk, pr=pr, hp=hp, kg=kg: e.matmul(
                    self.bank(bk), lhsT=self.iqT[pr, lb, hp, :], rhs=self.ikT[pr, kg * 512:(kg + 1) * 512],
                    start=True, stop=True), reads=[self.iqT_b, self.ikT_b], writes=[self.pbuf[bk]])
                act.op(lambda e, bk=bk, r=r: e.activation(out=self.rbb[r], in_=self.bank(bk), func=AF.Relu),
                       reads=[self.pbuf[bk]], writes=[self.rb_b[r]])
                if prev is not None:
                    headsum(*prev)
                prev = (kg, h, r)
            headsum(*prev)

        def Y(lb):
            S = 1024 * (lb % 4 + 1)
            m = lb % 2
            scb = self.score_b[m]
            scv = self.score[m][:, 0:S]
            dve.op(lambda e: e.tensor_reduce(out=rmax, in_=scv, axis=AX.X, op=ALU.max), reads=[scb], writes=[SB])
            dve.op(lambda e: e.tensor_reduce(out=rmin, in_=scv, axis=AX.X, op=ALU.min), reads=[scb], writes=[SB])
            dve.op(lambda e: e.tensor_tensor(out=self.score[m][:, S - 1024:S], in0=self.score[m][:, S - 1024:S], in1=self.cb,
                                             op=ALU.add), reads=[scb, self.cst_b], writes=[scb])
            dve.op(lambda e: e.tensor_scalar(out=w0, in0=rmax, scalar1=rmin, scalar2=0.5, op0=ALU.subtract, op1=ALU.mult),
                   reads=[SB], writes=[SB])
            dve.op(lambda e: e.tensor_scalar(out=wtab, in0=self.cstf[:, C_POW2:C_POW2 + NIT + 1], scalar1=w0, scalar2=None,
                                             op0=ALU.mult), reads=[SB, self.cst_b], writes=[SB])
            dve.op(lambda e: e.tensor_tensor(out=mid, in0=rmin, in1=wtab[:, 0:1], op=ALU.add), reads=[SB], writes=[SB])
            for i in range(NIT):
                dve.op(lambda e: e.tensor_scalar(out=self.mb[m][:, 0:S], in0=scv, scalar1=mid, scalar2=0.0,
                                                 op0=ALU.is_ge, op1=ALU.add, accum_out=cnt),
                       reads=[scb, SB], writes=[self.mb_b[m], SB])
                dve.op(lambda e, i=i: e.tensor_scalar(out=tmp, in0=cnt, scalar1=256.0, scalar2=wtab[:, i:i + 1],
                                                      op0=ALU.is_ge, op1=ALU.mult), reads=[SB], writes=[SB])
                dve.op(lambda e, i=i: e.scalar_tensor_tensor(out=mid, in0=tmp, scalar=wtab[:, i + 1:i + 2], in1=mid,
                                                             op0=ALU.subtract, op1=ALU.add), reads=[SB], writes=[SB])
            dve.op(lambda e: e.tensor_tensor(out=tau, in0=mid, in1=wtab[:, NIT:NIT + 1], op=ALU.subtract),
                   reads=[SB], writes=[SB])
            dve.op(lambda e: e.tensor_scalar(out=self.mb[m][:, 0:S], in0=scv, scalar1=tau, scalar2=NEG_MASK,
                                             op0=ALU.is_lt, op1=ALU.mult), reads=[scb, SB], writes=[self.mb_b[m]])

        def Zmain(lb):
            S = 1024 * (lb % 4 + 1)
            m = lb % 2
            nch = S // 128
            prev = None

            def pv(c, g, ps_):
                pe.op(lambda e: e.matmul(
                    self.bank(4 + g), lhsT=self.V[:, c, g, :], rhs=self.pT[ps_], start=(c == 0), stop=(c == nch - 1)),
                    reads=[self.V_b, self.pT_b[ps_]], writes=[self.pbuf[4 + g]])
                pe.op(lambda e: e.matmul(
                    self.bank(6 + g), lhsT=self.ones, rhs=self.pT[ps_], start=(c == 0), stop=(c == nch - 1)),
                    reads=[self.cst_b, self.pT_b[ps_]], writes=[self.pbuf[6 + g]])
            for c in range(nch):
                for g in range(2):
                    ps_ = self.au % 2
                    self.au += 1
                    pe.op(lambda e, c=c, g=g: e.matmul(
                        self.bank(3), lhsT=self.KT[:, g, c * 128:(c + 1) * 128],
                        rhs=self.aqT[:, lb, g * 4:(g + 1) * 4, :].rearrange("p h t -> p (h t)"), start=True, stop=False),
                        reads=[self.KT_b, self.aqT_b], writes=[self.pbuf[3]])
                    pe.op(lambda e, c=c: e.matmul(
                        self.bank(3), lhsT=self.mb[m][:, c * 128:(c + 1) * 128], rhs=self.ident4, start=False, stop=True),
                        reads=[self.mb_b[m], self.cst_b], writes=[self.pbuf[3]])
                    if prev is not None:
                        pv(*prev)
                    act.op(lambda e, ps_=ps_: e.activation(out=self.pT[ps_], in_=self.bank(3), func=AF.Exp, scale=scale),
                           reads=[self.pbuf[3]], writes=[self.pT_b[ps_]])
                    prev = (c, g, ps_)
            pv(*prev)

        def Zfin(lb):
            for g in range(2):
                dve.op(lambda e, g=g: e.reciprocal(out=self.rcp, in_=self.bank(6 + g)), reads=[self.pbuf[6 + g]],
                       writes=[self.rcp_b])
                dst = self.mixT[:, 8 + g * 4:8 + (g + 1) * 4, lb * 128:(lb + 1) * 128]
                dve.op(lambda e, g=g, dst=dst: e.tensor_tensor(
                    out=dst, in0=self.bank(4 + g).rearrange("p (h t) -> p h t", h=4, t=128),
                    in1=self.rcp.rearrange("p (h t) -> p h t", h=4, t=128), op=ALU.mult),
                    reads=[self.pbuf[4 + g], self.rcp_b], writes=[self.hT_b[lb // 4]])

        load_ik(0)
        X(0)
        Y(0)
        for lb in range(1, NB):
            if lb == 4:
                load_ik(1)
            X(lb)
            if lb - 1 in (0, 4):
                load_kv((lb - 1) // 4)
            Zmain(lb - 1)
            Y(lb)
            Zfin(lb - 1)
        Zmain(NB - 1)
        Zfin(NB - 1)

    def phaseD(self):
        pe, dve, pool = self.pe, self.dve, self.pool
        self.barrier()
        self.load_x(self.xspill)
        for cg in range(4):
            s = cg % 2
            pool.dma(self.dw[s], self.win[s].rearrange("p a b -> p (a b)"), self.wout_d[cg],
                     writes=[self.wg_b[s], self.wu_b[s]])
            for lb in range(NB):
                bk = (cg * NB + lb) % 4
                for kc in range(16):
                    pe.op(lambda e, bk=bk, kc=kc, lb=lb, s=s: e.matmul(
                        self.bank(bk), lhsT=self.mixT[:, kc, lb * 128:(lb + 1) * 128], rhs=self.win[s][:, kc, :],
                        start=(kc == 0), stop=(kc == 15)),
                        reads=[self.hT_b[lb // 4], self.wg_b[s], self.wu_b[s]], writes=[self.pbuf[bk]])
                xsl = self.xs[:, lb, cg * 512:(cg + 1) * 512]
                dve.op(lambda e, bk=bk, xsl=xsl: e.tensor_tensor(out=xsl, in0=self.bank(bk), in1=xsl, op=ALU.add),
                       reads=[self.pbuf[bk], self.xs_b[lb]], writes=[self.xs_b[lb]])
        self.barrier()

    def build(self):
        stop = self.stop
        self.phase0()
        self.barrier()
        self.norm_T(0)
        self.ffn(0)

        def finish_reload():
            self.barrier()
            self.load_x(self.xspill)
            self.final_norm()
            self.barrier()
        if stop == "ffn1":
            self.final_norm()
            self.barrier()
            return
        self.phaseB()
        if stop == "B":
            return finish_reload()
        self.allgather()
        if stop == "AG":
            return finish_reload()
        self.phaseC1()
        self.barrier()
        if stop == "C1":
            if self.debug:
                self.barrier()
                self.sp.dma(self.d_st, self.dbg_mix, self.mixT.rearrange("p a b -> p (a b)"), reads=self.hT_b)
            return finish_reload()
        self.phaseC2()
        if self.debug:
            self.barrier()
            self.sp.dma(self.d_st, self.dbg_mix, self.mixT.rearrange("p a b -> p (a b)"), reads=self.hT_b)
        if stop == "C2":
            return finish_reload()
        self.phaseD()
        if self.debug:
            for lb in range(NB):
                self.sp.dma(self.d_st, self.dbg_x[lb], self.xs[:, lb, :], reads=[self.xs_b[lb]])
        if stop != "D":
            self.norm_T(2)
            self.ffn(1)
        self.final_norm()
        self.barrier()


_CACHE = {}


def _consts():
    f = np.zeros((128, NF), np.float32)
    f[:, C_INV128:C_INV128 + 64] = (np.float32(10000.0) ** (-(np.arange(0, 128, 2, dtype=np.float32) / np.float32(128)))).astype(np.float32)[None]
    f[:, C_INV64:C_INV64 + 32] = (np.float32(10000.0) ** (-(np.arange(0, 64, 2, dtype=np.float32) / np.float32(64)))).astype(np.float32)[None]
    h = np.arange(8, dtype=np.float64)
    lg = np.log1p(-np.exp2(-5.0 - h))
    p1 = np.arange(1, 129, dtype=np.float64)
    f[:, C_GQ:C_GQ + 8] = np.exp(p1[:, None] * lg[None, :])
    f[:, C_GK:C_GK + 8] = np.exp(-p1[:, None] * lg[None, :]) * 128 ** -0.5
    f[:, C_GQT:C_GQT + 1024] = np.exp(lg[:, None] * p1[None, :]).reshape(1, 1024)
    f[:, C_GKT:C_GKT + 1024] = (np.exp(-lg[:, None] * p1[None, :]) * 128 ** -0.5).reshape(1, 1024)
    f[:, C_GC:C_GC + 1024] = np.repeat(np.exp(128 * lg), 128)[None]
    f[:, C_POW2:C_POW2 + 24] = (2.0 ** -np.arange(24))[None]
    f[:, C_EPS] = 1e-6
    cb = np.zeros((128, NCB), np.float32)
    eye = np.eye(128, dtype=np.float32)
    cb[:, 0:512] = np.tile(eye, (1, 4))
    tri = (np.arange(128)[None, :] >= np.arange(128)[:, None]).astype(np.float32)
    cb[:, 512:1024] = np.tile(tri, (1, 4))
    cb[:, 1024:1152] = 1.0
    return f, cb.astype(ml_dtypes.bfloat16)


def _percore_consts(j):
    c = np.zeros((128, NCC), np.float32)
    qrel = 128 * j + np.arange(128)
    c[:, 0:1024] = np.where(np.arange(1024)[None, :] > qrel[:, None], np.float32(-1e30), np.float32(0.0))
    c[:, 1024 + j] = 1.0
    return c


def _win_layout(w_in):
    O = dict(rq=0, rk=1024, rv=2048, rg=3072, aq=4096, ak=5120, av=5376, iq=5632, ik=6656, iw=6720)
    cols = []

    def rng(a, n):
        return list(range(a, a + n))
    cols += rng(O["rv"], 1024) + rng(O["rk"], 1024) + rng(O["rq"], 1024) + rng(O["rg"], 1024) + rng(O["aq"], 1024)
    cols += rng(O["ak"], 256) + rng(O["av"], 256)
    cols += rng(O["ik"], 64) + rng(O["ik"], 64) + rng(O["iw"], 16) + [-1] * 368
    cols += rng(O["iq"], 1024)
    cols = np.array(cols)
    W = np.zeros((D, 14 * 512), np.float32)
    m = cols >= 0
    W[:, m] = w_in[:, cols[m]]
    return np.ascontiguousarray(W.reshape(16, 128, 14, 512).transpose(2, 1, 0, 3)).reshape(14, 128, 16 * 512)


def _gu_layout(W):
    return np.ascontiguousarray(W.reshape(16, 128, NFG, 256).transpose(2, 1, 0, 3)).reshape(NFG, 128, 16 * 256)


def _wd_layout(W):
    return np.ascontiguousarray(W.reshape(NFG, 2, 128, D).transpose(0, 2, 1, 3)).reshape(NFG, 128, 2 * D)


def _wout_layout(W):
    return np.ascontiguousarray(W.reshape(16, 128, 4, 512).transpose(2, 1, 0, 3)).reshape(4, 128, 16 * 512)


def _chunks(j):
    return [(b, 8 * k + j) for b in range(2) for k in range(4)]


def kernel(x, positions, ffn1_norm, ffn1_w_gate, ffn1_w_up, ffn1_w_down, mix_norm, w_in, ret_norm, w_out,
           ffn2_norm, ffn2_w_gate, ffn2_w_up, ffn2_w_down, final_norm, _stop="full", _debug=False):
    x = np.asarray(x, np.float32)
    positions = np.asarray(positions, np.int32)
    key = (_stop, _debug)
    if key not in _CACHE:
        _CACHE[key] = Prog(_stop, _debug)
    prog = _CACHE[key]
    cf, cbb = _consts()
    norms = np.zeros((5, D), np.float32)
    norms[0] = np.asarray(ffn1_norm)[0]
    norms[1] = np.asarray(mix_norm)[0]
    norms[2] = np.asarray(ffn2_norm)[0]
    norms[3] = np.asarray(final_norm)
    norms[4, :1024] = np.asarray(ret_norm)[0]
    shared = dict(
        norms=norms,
        wg1=_gu_layout(np.asarray(ffn1_w_gate, np.float32)[0]), wu1=_gu_layout(np.asarray(ffn1_w_up, np.float32)[0]),
        wd1=_wd_layout(np.asarray(ffn1_w_down, np.float32)[0]),
        wg2=_gu_layout(np.asarray(ffn2_w_gate, np.float32)[0]), wu2=_gu_layout(np.asarray(ffn2_w_up, np.float32)[0]),
        wd2=_wd_layout(np.asarray(ffn2_w_down, np.float32)[0]),
        win=_win_layout(np.asarray(w_in, np.float32)[0]), wout=_wout_layout(np.asarray(w_out, np.float32)[0]),
        cstf=cf, cstb=cbb,
    )
    in_maps = []
    for j in range(8):
        ch = _chunks(j)
        xm = np.stack([x[b, n * 128:(n + 1) * 128] for (b, n) in ch])
        pm = np.stack([positions[b, n * 128:(n + 1) * 128] for (b, n) in ch], axis=1)
        m = dict(shared)
        m["x"] = np.ascontiguousarray(xm)
        m["pos"] = np.ascontiguousarray(pm.astype(np.int32))
        m["cstc"] = _percore_consts(j)
        in_maps.append(m)
    res = run_bass_kernel_spmd(prog.nc, in_maps, core_ids=list(range(8)))
    if _debug:
        kernel.dbg = [(res.results[j]["dbg_mix"], res.results[j]["dbg_x"]) for j in range(8)]
    out = np.zeros((2, 4096, D), np.float32)
    for j in range(8):
        o = res.results[j]["out"]
        for lb, (b, n) in enumerate(_chunks(j)):
            out[b, n * 128:(n + 1) * 128] = o[lb]
    return out
```
